# Optimizing a Trainium2 kernel written in Bass

```python
import math
import jax
import jax.numpy as jnp
from jax import lax
import numpy as np

D_MODEL = 1024
BATCH = 2
SEQ = 16384
DEPTH = 4

GRID_W = 64
CTX_LEN = 256
N_MIXERS = 3
N_MOD = 6
EPS = 1e-6

CONV_WIDTH = 31

HY_ORDER = 2
HY_SHORT = 3
HY_EMB = 33
HY_FILTER_WIDTH = 64
HY_INNER = 2
HY_TARGET = 1e-2
HY_DECAY_MIN = math.log(HY_TARGET) / 1.5
HY_DECAY_MAX = math.log(HY_TARGET) / 0.3

HG_EXPAND = 128
HG_HEADS = D_MODEL // HG_EXPAND
HG_HEAD_I = D_MODEL // HG_HEADS
HG_CHUNK = 64

D_FF = 7 * D_MODEL // 2
N_EXPERTS = 8
TOP_K = 2
MOE_BLOCK = 256

N_A = (DEPTH + 2) // 3
N_B = (DEPTH + 1) // 3
N_C = DEPTH // 3
N_DENSE = (DEPTH + 1) // 2
N_MOE = DEPTH // 2

kernel_name = "hybrid_conformer_hyena_hgrn2_moe_trunk"


def rms_norm(x, g):
    xf = x.astype(jnp.float32)
    y = xf * lax.rsqrt(jnp.mean(xf * xf, axis=-1, keepdims=True) + EPS)
    return (y * g.astype(jnp.float32)).astype(x.dtype)


def layer_norm(x, g, b):
    xf = x.astype(jnp.float32)
    mu = jnp.mean(xf, axis=-1, keepdims=True)
    xc = xf - mu
    var = jnp.mean(xc * xc, axis=-1, keepdims=True)
    return (xc * lax.rsqrt(var + EPS) * g.astype(jnp.float32) + b.astype(jnp.float32)).astype(x.dtype)


def pos_embed_2d(rows, dim):
    quarter = dim // 4
    omega = 1.0 / (10000.0 ** (jnp.arange(quarter, dtype=jnp.float32) / quarter))
    t = jnp.arange(rows * GRID_W)
    r = (t // GRID_W).astype(jnp.float32)[:, None] * omega
    col = (t % GRID_W).astype(jnp.float32)[:, None] * omega
    return jnp.concatenate([jnp.sin(r), jnp.cos(r), jnp.sin(col), jnp.cos(col)], axis=-1)


def depthwise_conv(x, w, b):
    width = w.shape[0]
    pad = (width - 1) // 2
    y = lax.conv_general_dilated(
        x, w[:, None, :].astype(x.dtype), window_strides=(1,),
        padding=[(pad, width - 1 - pad)],
        dimension_numbers=("NWC", "WIO", "NWC"),
        feature_group_count=x.shape[-1])
    return y + b


def conformer_conv(h, w_pw1, b_pw1, w_dw, b_dw, ln_g, ln_b, w_pw2, b_pw2):
    a, g = jnp.split(h @ w_pw1 + b_pw1, 2, axis=-1)
    u = a * jax.nn.sigmoid(g)
    u = depthwise_conv(u, w_dw, b_dw)
    u = jax.nn.silu(layer_norm(u, ln_g, ln_b))
    return u @ w_pw2 + b_pw2


def hyena_filters(L, w_f1, b_f1, freq, w_f2, b_f2, w_f3):
    f32 = jnp.float32
    t = jnp.linspace(0.0, 1.0, L, dtype=f32)[:, None]
    bands = (HY_EMB - 1) // 2
    w = 2.0 * math.pi * jnp.arange(L, dtype=f32)[:, None] / L
    fb = jnp.linspace(1e-4, bands - 1, bands, dtype=f32)[None, :]
    z = jnp.concatenate([t, jnp.cos(fb * w), -jnp.sin(fb * w)], axis=-1)
    fr = freq.astype(f32)
    hid = jnp.sin(fr * (z @ w_f1.astype(f32) + b_f1.astype(f32)))
    for j in range(HY_INNER):
        hid = jnp.sin(fr * (hid @ w_f2[j].astype(f32) + b_f2[j].astype(f32)))
    filt = hid @ w_f3.astype(f32)
    d = filt.shape[-1] // (2 * HY_ORDER)
    deltas = jnp.abs(jnp.linspace(HY_DECAY_MIN, HY_DECAY_MAX, d, dtype=f32))
    decay = jnp.exp(-t * deltas)
    filt = filt.reshape(L, HY_ORDER, 2, d) * decay[:, None, None, :]
    fwd, bwd = filt[:, :, 0], filt[:, :, 1]
    two_sided = jnp.concatenate([fwd, jnp.zeros((1, HY_ORDER, d), f32), bwd[:0:-1]], axis=0)
    return two_sided / jnp.sum(jnp.abs(two_sided), axis=0, keepdims=True)


def hyena(h, w_in, b_in, w_short, b_short, w_f1, b_f1, freq, w_f2, b_f2, w_f3, bias, w_out, b_out):
    L = h.shape[1]
    u = depthwise_conv(h @ w_in + b_in, w_short, b_short)
    v, x1, x2 = jnp.split(u, 3, axis=-1)
    filt_f = jnp.fft.rfft(hyena_filters(L, w_f1, b_f1, freq, w_f2, b_f2, w_f3), axis=0)
    z = v.astype(jnp.float32)
    for n, gate in enumerate((x1, x2)):
        z_f = jnp.fft.rfft(z, n=2 * L, axis=1)
        conv = jnp.fft.irfft(z_f * filt_f[None, :, n], n=2 * L, axis=1)[:, :L]
        z = gate.astype(jnp.float32) * (conv + bias[n].astype(jnp.float32) * z)
    return z.astype(h.dtype) @ w_out + b_out


def gla_chunk_scan(q, k, v, g, s0):
    Bn, H, L, dk = q.shape
    dv = v.shape[-1]
    n = L // HG_CHUNK
    q, k, g = (a.reshape(Bn, H, n, HG_CHUNK, dk) for a in (q, k, g))
    v = v.reshape(Bn, H, n, HG_CHUNK, dv)
    b = jnp.cumsum(g, axis=3)
    b_last = b[:, :, :, -1:]
    q_dec = q * jnp.exp(b)
    k_inv = k * jnp.exp(-b)
    k_end = k * jnp.exp(b_last - b)
    mask = jnp.tril(jnp.ones((HG_CHUNK, HG_CHUNK), dtype=bool))
    att = jnp.where(mask, jnp.einsum("bhntk,bhnsk->bhnts", q_dec, k_inv), 0.0)
    o_intra = jnp.einsum("bhnts,bhnsv->bhntv", att, v)
    ds = jnp.einsum("bhnsk,bhnsv->bhnkv", k_end, v)
    chunk_decay = jnp.exp(b_last[:, :, :, 0])

    def step(s, inp):
        dec, d = inp
        return dec[..., None] * s + d, s

    s_final, s_prev = lax.scan(step, s0, (jnp.moveaxis(chunk_decay, 2, 0), jnp.moveaxis(ds, 2, 0)))
    s_prev = jnp.moveaxis(s_prev, 0, 2)
    o_inter = jnp.einsum("bhntk,bhnkv->bhntv", q_dec, s_prev)
    return (o_intra + o_inter).reshape(Bn, H, L, dv), s_final


def hgrn2_mixer(h, hc, lb, w_in, gn_g, w_out):
    f32 = jnp.float32
    lb = lb.astype(f32)

    def flip(a):
        return jnp.flip(a, axis=2)

    def mix(u, states):
        Bn, L, D = u.shape
        q, i, og, zf, zb = jnp.split(u @ w_in, 5, axis=-1)

        def heads(a):
            return a.astype(f32).reshape(Bn, L, HG_HEADS, -1).transpose(0, 2, 1, 3)

        qh, vh = heads(jax.nn.silu(q)), heads(i)
        f_fw = lb + (1.0 - lb) * jax.nn.sigmoid(zf.astype(f32))
        f_bw = lb + (1.0 - lb) * jax.nn.sigmoid(zb.astype(f32))
        o_f, s_f = gla_chunk_scan(qh, heads(1.0 - f_fw), vh, heads(jnp.log(f_fw)), states[0])
        o_b, s_b = gla_chunk_scan(flip(qh), flip(heads(1.0 - f_bw)), flip(vh), flip(heads(jnp.log(f_bw))), states[1])
        o = (o_f + flip(o_b)).transpose(0, 2, 1, 3)
        o = o * lax.rsqrt(jnp.mean(o * o, axis=-1, keepdims=True) + EPS)
        o = o * gn_g.astype(f32).reshape(HG_HEADS, HG_HEAD_I)
        y = (o.reshape(Bn, L, D).astype(u.dtype) * jax.nn.silu(og)) @ w_out
        return y, (s_f, s_b)

    zero = jnp.zeros((hc.shape[0], HG_HEADS, HG_EXPAND, HG_HEAD_I), f32)
    yc, ctx_states = mix(hc, (zero, zero))
    y, _ = mix(h, ctx_states)
    return y, yc


def swiglu(t, w_gate, w_up, w_down):
    return (jax.nn.silu(t @ w_gate) * (t @ w_up)) @ w_down


def moe_swiglu(tok, w_router, w_gate, w_up, w_down):
    T, D = tok.shape
    logits = (tok @ w_router).astype(jnp.float32)
    top_logit, top_e = lax.top_k(logits, TOP_K)
    gate = jax.nn.softmax(top_logit, axis=-1)
    flat_e = top_e.reshape(-1)
    order = jnp.argsort(flat_e)
    sorted_e = flat_e[order]
    sorted_tok = (order // TOP_K).astype(jnp.int32)
    sorted_gate = gate.reshape(-1)[order]
    counts = jnp.bincount(flat_e, length=N_EXPERTS)
    padded = (counts + MOE_BLOCK - 1) // MOE_BLOCK * MOE_BLOCK
    start = jnp.cumsum(counts) - counts
    pad_end = jnp.cumsum(padded)
    pad_start = pad_end - padded
    dest = pad_start[sorted_e] + jnp.arange(T * TOP_K) - start[sorted_e]
    n_blocks = -(-(T * TOP_K) // MOE_BLOCK) + N_EXPERTS
    n_slots = n_blocks * MOE_BLOCK
    slot_tok = jnp.zeros((n_slots,), jnp.int32).at[dest].set(sorted_tok)
    slot_gate = jnp.zeros((n_slots,), jnp.float32).at[dest].set(sorted_gate)
    block_e = jnp.minimum(
        jnp.searchsorted(pad_end, jnp.arange(n_blocks) * MOE_BLOCK, side="right"), N_EXPERTS - 1)
    xb = tok[slot_tok].reshape(n_blocks, MOE_BLOCK, D)

    def expert_block(args):
        xe, e = args
        return swiglu(xe, w_gate[e], w_up[e], w_down[e])

    yb = lax.map(expert_block, (xb, block_e)).reshape(n_slots, D)
    yb = yb * slot_gate[:, None].astype(yb.dtype)
    return jnp.zeros_like(tok).at[slot_tok].add(yb)


def setup_inputs(seed: int = 0) -> dict:
    key = jax.random.key(seed)
    ks = iter(jax.random.split(key, 48))
    f32 = jnp.float32
    D = D_MODEL

    def w(shape, fan_in, gain=1.0):
        return jax.random.normal(next(ks), shape, f32) * (gain * fan_in ** -0.5)

    def small(shape, s=0.02):
        return jax.random.normal(next(ks), shape, f32) * s

    def ones_noise(shape):
        return 1.0 + small(shape)

    return {
        "x": jax.random.normal(next(ks), (BATCH, SEQ, D), f32),
        "c": jax.random.normal(next(ks), (BATCH, D), f32),
        "ctx": jax.random.normal(next(ks), (BATCH, CTX_LEN, D), f32),
        "c_ctx": jax.random.normal(next(ks), (D,), f32),
        "ada_w": w((DEPTH, D, N_MOD * D), D, 0.5),
        "ada_b": small((DEPTH, N_MOD * D)),
        "norm1_g": ones_noise((DEPTH, D)),
        "norm2_g": ones_noise((DEPTH, D)),
        "normf_g": ones_noise((D,)),
        "cf_w_pw1": w((N_A, D, 2 * D), D),
        "cf_b_pw1": small((N_A, 2 * D)),
        "cf_w_dw": w((N_A, CONV_WIDTH, D), CONV_WIDTH),
        "cf_b_dw": small((N_A, D)),
        "cf_ln_g": ones_noise((N_A, D)),
        "cf_ln_b": small((N_A, D)),
        "cf_w_pw2": w((N_A, D, D), D),
        "cf_b_pw2": small((N_A, D)),
        "hy_w_in": w((N_B, D, 3 * D), D),
        "hy_b_in": small((N_B, 3 * D)),
        "hy_w_short": w((N_B, HY_SHORT, 3 * D), HY_SHORT),
        "hy_b_short": small((N_B, 3 * D)),
        "hy_w_f1": w((N_B, HY_EMB, HY_FILTER_WIDTH), HY_EMB),
        "hy_b_f1": small((N_B, HY_FILTER_WIDTH)),
        "hy_freq": ones_noise((N_B, HY_FILTER_WIDTH)),
        "hy_w_f2": w((N_B, HY_INNER, HY_FILTER_WIDTH, HY_FILTER_WIDTH), HY_FILTER_WIDTH),
        "hy_b_f2": small((N_B, HY_INNER, HY_FILTER_WIDTH)),
        "hy_w_f3": w((N_B, HY_FILTER_WIDTH, HY_ORDER * 2 * D), HY_FILTER_WIDTH),
        "hy_bias": small((N_B, HY_ORDER, D), 0.5),
        "hy_w_out": w((N_B, D, D), D),
        "hy_b_out": small((N_B, D)),
        "hg_lb_logits": small((DEPTH, D), 0.1),
        "hg_w_in": w((N_C, D, 5 * D), D),
        "hg_gn_g": ones_noise((N_C, D)),
        "hg_w_out": w((N_C, D, D), D),
        "ffn_w_gate": w((N_DENSE, D, D_FF), D),
        "ffn_w_up": w((N_DENSE, D, D_FF), D),
        "ffn_w_down": w((N_DENSE, D_FF, D), D_FF),
        "moe_w_router": w((N_MOE, D, N_EXPERTS), D),
        "moe_w_gate": w((N_MOE, N_EXPERTS, D, D_FF), D),
        "moe_w_up": w((N_MOE, N_EXPERTS, D, D_FF), D),
        "moe_w_down": w((N_MOE, N_EXPERTS, D_FF, D), D_FF),
    }


def reference(x, c, ctx, c_ctx, ada_w, ada_b, norm1_g, norm2_g, normf_g,
              cf_w_pw1, cf_b_pw1, cf_w_dw, cf_b_dw, cf_ln_g, cf_ln_b, cf_w_pw2, cf_b_pw2,
              hy_w_in, hy_b_in, hy_w_short, hy_b_short, hy_w_f1, hy_b_f1, hy_freq, hy_w_f2, hy_b_f2,
              hy_w_f3, hy_bias, hy_w_out, hy_b_out,
              hg_lb_logits, hg_w_in, hg_gn_g, hg_w_out,
              ffn_w_gate, ffn_w_up, ffn_w_down,
              moe_w_router, moe_w_gate, moe_w_up, moe_w_down):
    Bn, S, D = x.shape
    rows = S // GRID_W
    x = x + pos_embed_2d(rows, D).astype(x.dtype)[None]
    xc = ctx
    p = jax.nn.softmax(hg_lb_logits.astype(jnp.float32), axis=0)
    lb_all = jnp.cumsum(p, axis=0) - p[0]
    silu_c = jax.nn.silu(c)
    silu_cc = jax.nn.silu(c_ctx)

    for i in range(DEPTH):
        last = i == DEPTH - 1
        kind, j = i % N_MIXERS, i // N_MIXERS
        sh1, sc1, g1, sh2, sc2, g2 = jnp.split((silu_c @ ada_w[i] + ada_b[i])[:, None, :], N_MOD, axis=-1)
        csh1, csc1, cg1, csh2, csc2, cg2 = jnp.split(silu_cc @ ada_w[i] + ada_b[i], N_MOD, axis=-1)

        h = rms_norm(x, norm1_g[i]) * (1.0 + sc1) + sh1
        if (not last) or kind == 2:
            hc = rms_norm(xc, norm1_g[i]) * (1.0 + csc1) + csh1
        if kind == 0:
            prm = (cf_w_pw1[j], cf_b_pw1[j], cf_w_dw[j], cf_b_dw[j], cf_ln_g[j], cf_ln_b[j], cf_w_pw2[j], cf_b_pw2[j])
            y = conformer_conv(h, *prm)
            yc = None if last else conformer_conv(hc, *prm)
        elif kind == 1:
            prm = (hy_w_in[j], hy_b_in[j], hy_w_short[j], hy_b_short[j], hy_w_f1[j], hy_b_f1[j], hy_freq[j],
                   hy_w_f2[j], hy_b_f2[j], hy_w_f3[j], hy_bias[j], hy_w_out[j], hy_b_out[j])
            y = hyena(h, *prm)
            yc = None if last else hyena(hc, *prm)
        else:
            y, yc = hgrn2_mixer(h, hc, lb_all[i], hg_w_in[j], hg_gn_g[j], hg_w_out[j])
        x = x + g1 * y
        if not last:
            xc = xc + cg1 * yc

        tok = (rms_norm(x, norm2_g[i]) * (1.0 + sc2) + sh2).reshape(-1, D)
        if not last:
            tok_c = (rms_norm(xc, norm2_g[i]) * (1.0 + csc2) + csh2).reshape(-1, D)
            tok = jnp.concatenate([tok_c, tok], axis=0)
        if i % 2 == 0:
            out = swiglu(tok, ffn_w_gate[i // 2], ffn_w_up[i // 2], ffn_w_down[i // 2])
        else:
            out = moe_swiglu(tok, moe_w_router[i // 2], moe_w_gate[i // 2], moe_w_up[i // 2], moe_w_down[i // 2])
        n_ctx = tok.shape[0] - Bn * S
        x = x + g2 * out[n_ctx:].reshape(Bn, S, D)
        if not last:
            xc = xc + cg2 * out[:n_ctx].reshape(xc.shape)

    return rms_norm(x, normf_g)
```

```python
import math
import contextlib
import numpy as np
import concourse.bass as bass
import concourse.mybir as mybir
from concourse.bass_utils import run_bass_kernel_spmd

F32 = mybir.dt.float32
BF16 = mybir.dt.bfloat16
ALU = mybir.AluOpType
AF = mybir.ActivationFunctionType
AX = mybir.AxisListType

D = 1024
KC = 8
DFF = 3584
FC = 28
SEQ = 16384
BATCH = 2
CTX = 256
NCORE = 8
TPC = 4096
EPS = 1e-6
GRID_W = 64
NE = 8

ENGS = ("sync", "scalar", "vector", "gpsimd", "tensor")
DMA_SLOTS = {"sync": 24, "scalar": 8, "gpsimd": 24}


class Op:
    __slots__ = ("eng", "fn", "waits", "signal", "semkey", "semval", "dma")

    def __init__(self, eng, fn, dma):
        self.eng = eng
        self.fn = fn
        self.waits = []
        self.signal = False
        self.semkey = None
        self.semval = 0
        self.dma = dma


class Region:
    __slots__ = ("w", "rs")

    def __init__(self):
        self.w = None
        self.rs = []


class Prog:
    def __init__(self, nc):
        self.nc = nc
        self.ops = {e: [] for e in ENGS}
        self.stack = contextlib.ExitStack()
        self.dma_slot = {e: [None] * n for e, n in DMA_SLOTS.items()}
        self.dma_rr = {e: 0 for e in DMA_SLOTS}
        self.out_dmas = []
        self.big = self.stack.enter_context(nc.sbuf_tensor("big", [128, SB_WORDS], F32))
        self.off = 0
        self.marks = []
        self.psum = [self.stack.enter_context(nc.psum_tensor(f"ps{i}", [128, 512], F32)) for i in range(8)]
        self.psr = [Region() for _ in range(8)]
        self.ps_i = 0

    def alloc(self, free_shape, dt=F32, parts=128):
        n = int(np.prod(free_shape))
        words = n if dt == F32 else (n + 1) // 2
        words = (words + 7) // 8 * 8
        assert self.off + words <= SB_WORDS, f"SBUF overflow {self.off}+{words}"
        ap = self.big[:, self.off:self.off + words]
        self.off += words
        if dt != F32:
            ap = ap.bitcast(dt)
        ap = ap[:, 0:n]
        if len(free_shape) == 2:
            ap = ap.rearrange("p (a b) -> p a b", a=free_shape[0])
        elif len(free_shape) == 3:
            ap = ap.rearrange("p (a b c) -> p a b c", a=free_shape[0], b=free_shape[1])
        if parts != 128:
            ap = ap[0:parts]
        return ap

    def mark(self):
        self.marks.append(self.off)

    def release(self):
        self.barrier()
        self.off = self.marks.pop()

    def bank(self):
        i = self.ps_i
        self.ps_i = (i + 1) % 8
        return self.psum[i], self.psr[i]

    def R(self, n=None):
        if n is None:
            return Region()
        return [Region() for _ in range(n)]

    def op(self, eng, fn, reads=(), writes=(), dma=False, is_out=False):
        o = Op(eng, fn, dma)
        deps = []
        seen = set()

        def add(d):
            if d is None or id(d) in seen:
                return
            seen.add(id(d))
            deps.append(d)

        for r in reads:
            add(r.w)
        for r in writes:
            add(r.w)
            for x in r.rs:
                add(x)
        if dma:
            slots = self.dma_slot[eng]
            i = self.dma_rr[eng]
            self.dma_rr[eng] = (i + 1) % len(slots)
            add(slots[i])
            o.semkey = (eng, i)
            slots[i] = o
            o.signal = True
        else:
            o.semkey = eng
        for d in deps:
            if d.eng == "tensor" and eng == "tensor" and not d.dma and not dma:
                continue
            if SAME_ENGINE_FREE and d.eng == eng and not d.dma and not dma:
                continue
            d.signal = True
            o.waits.append(d)
        for r in reads:
            if not dma:
                r.rs = [x for x in r.rs if x.dma or x.eng != eng]
            r.rs.append(o)
        for r in writes:
            r.w = o
            r.rs = []
        self.ops[eng].append(o)
        if is_out:
            self.out_dmas.append(o)
        return o

    def dma(self, eng, out, in_, reads=(), writes=(), is_out=False, **kw):
        return self.op(eng, lambda e: e.dma_start(out=out, in_=in_, **kw), reads, writes, dma=True, is_out=is_out)

    def V(self, fn, reads=(), writes=()):
        return self.op("vector", fn, reads, writes)

    def S(self, fn, reads=(), writes=()):
        return self.op("scalar", fn, reads, writes)

    def G(self, fn, reads=(), writes=()):
        return self.op("gpsimd", fn, reads, writes)

    def T(self, fn, reads=(), writes=()):
        return self.op("tensor", fn, reads, writes)

    def mm(self, out, outr, pairs, reads):
        def fn(e):
            n = len(pairs)
            ins = None
            for i, (l, r) in enumerate(pairs):
                ins = e.matmul(out, lhsT=l, rhs=r, start=(i == 0), stop=(i == n - 1))
            return ins
        return self.op("tensor", fn, reads, [outr])

    def barrier(self):
        lasts = []
        for e in ENGS:
            for o in reversed(self.ops[e]):
                if not o.dma and o.fn is not None:
                    lasts.append(o)
                    break
        for e in DMA_SLOTS:
            for o in self.dma_slot[e]:
                if o is not None:
                    lasts.append(o)
        for e in ENGS:
            o = Op(e, None, False)
            o.semkey = e
            for d in lasts:
                if d.eng == e and not d.dma:
                    continue
                d.signal = True
                o.waits.append(d)
            self.ops[e].append(o)

    def finish(self):
        o = Op("sync", None, False)
        o.semkey = "sync"
        o.waits = list(self.out_dmas)
        self.ops["sync"].append(o)

    def emit(self):
        nc = self.nc
        sems = {}
        self.maxcnt = {}
        for e in ENGS:
            cnt = 0
            slotcnt = {}
            for o in self.ops[e]:
                if o.dma:
                    slotcnt[o.semkey] = slotcnt.get(o.semkey, 0) + 16
                    o.semval = slotcnt[o.semkey]
                    sems.setdefault(o.semkey, None)
                elif o.signal:
                    cnt += 1
                    o.semval = cnt
            sems[e] = None
            self.maxcnt[e] = (cnt, len(self.ops[e]))
        for k in list(sems):
            nm = k if isinstance(k, str) else f"d_{k[0]}_{k[1]}"
            sems[k] = self.stack.enter_context(nc.semaphore("s_" + nm))

        for k, h in sems.items():
            nc.sync.sem_clear(h)
        nc.all_engine_barrier()

        def run(e, eng):
            waited = {}
            for o in self.ops[e]:
                for d in o.waits:
                    if waited.get(d.semkey, 0) >= d.semval:
                        continue
                    eng.wait_ge(sems[d.semkey], d.semval)
                    waited[d.semkey] = d.semval
                if o.fn is None:
                    continue
                ins = o.fn(eng)
                if o.dma:
                    ins.then_inc(sems[o.semkey], 16)
                elif o.signal:
                    ins.then_inc(sems[o.semkey], 1)

        with nc.Block() as block:
            @block.sync
            def _(eng):
                run("sync", eng)

            @block.scalar
            def _(eng):
                run("scalar", eng)

            @block.vector
            def _(eng):
                run("vector", eng)

            @block.gpsimd
            def _(eng):
                run("gpsimd", eng)

            @block.tensor
            def _(eng):
                run("tensor", eng)
        self.stack.close()


SAME_ENGINE_FREE = False
SB_WORDS = 49152


class Env:
    def __init__(self, name):
        self.nc = bass.Bass("TRN2", target_bir_lowering=False)
        self.P = Prog(self.nc)
        self.ins = {}
        self.outs = {}

    def inp(self, name, shape, dt=F32):
        t = self.nc.dram_tensor(name, list(shape), dt, kind="ExternalInput").ap()
        self.ins[name] = t
        return t

    def out(self, name, shape, dt=F32):
        t = self.nc.dram_tensor(name, list(shape), dt, kind="ExternalOutput").ap()
        self.outs[name] = t
        return t

    def scratch(self, name, shape, dt=F32):
        return self.nc.dram_tensor(name, list(shape), dt).ap()

    def consts(self):
        P = self.P
        cin = self.inp("consts", [128, 384])
        self.cf = P.alloc([384])
        self.cr = P.R()
        P.dma("sync", self.cf, cin, writes=[self.cr])
        self.ident = self.cf[:, 0:128]
        self.ones = self.cf[:, 128:256]
        self.J = self.cf[:, 256:384]
        cb = P.alloc([256], BF16)
        P.V(lambda e: e.tensor_copy(out=cb, in_=self.cf[:, 0:256]), [self.cr], [self.cr])
        self.ident_b = cb[:, 0:128]
        self.ones_b = cb[:, 128:256]


def host_consts():
    return np.concatenate([np.eye(128, dtype=np.float32), np.ones((128, 128), np.float32),
                           np.eye(128, dtype=np.float32)[::-1]], axis=1)


def fm(v):
    v = np.asarray(v, np.float32)
    return np.ascontiguousarray(v.reshape(-1, 128).T)


def load_small(E, name, ncols):
    P = E.P
    d = E.inp(name, [128, ncols])
    t = P.alloc([ncols])
    r = P.R()
    P.dma("sync", t, d, writes=[r])
    return t, r


def load_w_bf16(E, dst, dst_r, src_ap, reads=()):
    return E.P.dma("gpsimd", dst, src_ap, reads=reads, writes=[dst_r])


def compute_mod(E, adaw, vt, vr, col_c, col_adab, modT, modr):
    P = E.P
    P.mark()
    sc = P.alloc([16])
    scr = P.R()
    P.S(lambda e: e.activation(out=sc, in_=vt[:, col_c:col_c + 16], func=AF.Silu), [vr], [scr])
    sc3 = sc.rearrange("p (k j) -> p k j", j=2)
    wb = [P.alloc([8, 384]) for _ in range(2)]
    wr = P.R(2)
    ps, psr = P.bank()
    adv = adaw.rearrange("(k p) n -> p k n", p=128)
    for mb in range(16):
        i = mb % 2
        P.dma("sync", wb[i], adv[:, :, mb * 384:(mb + 1) * 384], writes=[wr[i]])
        for j in range(3):
            m = mb * 3 + j
            P.mm(ps[:, m * 2:m * 2 + 2], psr,
                 [(wb[i][:, k, j * 128:(j + 1) * 128], sc3[:, k, :]) for k in range(8)], [wr[i], scr])
    ps3 = ps[:, 0:96].rearrange("p (m j) -> p m j", j=2)
    for j in range(2):
        P.V(lambda e, j=j: e.tensor_tensor(out=modT[:, :, j], in0=ps3[:, :, j], in1=vt[:, col_adab:col_adab + 48],
                                           op=ALU.add), [psr, vr], [modr])
    P.release()


def rms_ctx(E, Wmax):
    P = E.P
    return dict(sq=P.alloc([8, Wmax], BF16), sqr=P.R(), rstd=P.alloc([Wmax]), rr=P.R(),
                tmp=[P.alloc([Wmax]) for _ in range(2)], tr=P.R(2))


def rms_mod(E, rc, x, xr, W, gsc, sh, jcol, scalr, out, outr, out_f32=None, out_f32_r=None):
    P = E.P
    sq, sqr, rr, tr = rc["sq"], rc["sqr"], rc["rr"], rc["tr"]
    rstd = rc["rstd"][:, 0:W]
    tmp = [t[:, 0:W] for t in rc["tmp"]]
    for c in range(8):
        P.S(lambda e, c=c: e.activation(out=sq[:, c, 0:W], in_=x[:, c, 0:W], func=AF.Square), [xr], [sqr])
    for a in range(0, W, 512):
        b = min(a + 512, W)
        ps, psr = P.bank()
        P.mm(ps[:, 0:b - a], psr, [(E.ones_b, sq[:, c, a:b]) for c in range(8)], [sqr, E.cr])
        P.V(lambda e, a=a, b=b, ps=ps: e.tensor_scalar(out=rstd[:, a:b], in0=ps[:, 0:b - a], scalar1=float(D * EPS),
                                                       scalar2=None, op0=ALU.add), [psr], [rr])
        P.S(lambda e, a=a, b=b: e.sqrt(out=rstd[:, a:b], in_=rstd[:, a:b]), [rr], [rr])
        P.V(lambda e, a=a, b=b: e.reciprocal(out=rstd[:, a:b], in_=rstd[:, a:b]), [rr], [rr])
    for c in range(8):
        i = c % 2
        P.V(lambda e, c=c, i=i: e.tensor_tensor(out=tmp[i], in0=x[:, c, 0:W], in1=rstd, op=ALU.mult),
            [xr, rr], [tr[i]])
        P.S(lambda e, c=c, i=i: e.activation(out=out[:, c, 0:W], in_=tmp[i], func=AF.Identity,
                                             bias=sh[:, c, jcol:jcol + 1], scale=gsc[:, c, jcol:jcol + 1]),
            [tr[i], scalr], [outr])
        if out_f32 is not None:
            P.V(lambda e, c=c, i=i: e.tensor_scalar(out=out_f32[:, c, 0:W], in0=tmp[i],
                                                    scalar1=gsc[:, c, jcol:jcol + 1], scalar2=sh[:, c, jcol:jcol + 1],
                                                    op0=ALU.mult, op1=ALU.add),
                [tr[i], scalr], [out_f32_r])


def layer_scalars(E, modT, modr, vt, vr, col_n1, col_n2):
    P = E.P
    sc = {}
    r = P.R()
    m4 = modT.rearrange("p (s c) j -> p s c j", s=6)
    for nm, si, col in (("gsc1", 1, col_n1), ("gsc2", 4, col_n2)):
        t = P.alloc([8, 2])
        for j in range(2):
            P.V(lambda e, t=t, j=j, si=si, col=col: e.scalar_tensor_tensor(
                out=t[:, :, j], in0=m4[:, si, :, j], scalar=1.0, in1=vt[:, col:col + 8], op0=ALU.add, op1=ALU.mult),
                [modr, vr], [r])
        P.V(lambda e, t=t: e.tensor_scalar(out=t, in0=t, scalar1=float(math.sqrt(D)), scalar2=None, op0=ALU.mult),
            [r], [r])
        sc[nm] = t
    sc["sh1"] = m4[:, 0]
    sc["g1"] = m4[:, 2]
    sc["sh2"] = m4[:, 3]
    sc["g2"] = m4[:, 5]
    sc["r"] = r
    sc["modr"] = modr
    return sc


def stage_conformer(E, tiles, w_pw1, w_pw2, vt, vr, cols, sc, xout, xout_r):
    P = E.P
    P.mark()
    c_bpw1, c_bdw, c_lng, c_lnb, c_bpw2, c_wdw = cols
    w1 = P.alloc([8, 2048], BF16)
    w2 = P.alloc([8, 1024], BF16)
    w1r, w2r = P.R(), P.R()
    w1v = w_pw1.rearrange("(k p) n -> p k n", p=128)
    for k in range(8):
        P.dma("gpsimd", w1[:, k, :], w1v[:, k, :], writes=[w1r])
    P.dma("gpsimd", w2, w_pw2.rearrange("(k p) n -> p k n", p=128), writes=[w2r])
    dgb = [P.alloc([31, 128], BF16) for _ in range(2)]
    dgrs = P.R(2)
    g1b = P.alloc([8, 2])
    g1br = P.R()
    for j in range(2):
        P.V(lambda e, j=j: e.tensor_tensor(out=g1b[:, :, j], in0=sc["g1"][:, :, j], in1=vt[:, c_bpw2:c_bpw2 + 8],
                                           op=ALU.mult), [sc["modr"], vr], [g1br])
    rc = rms_ctx(E, 542)
    xe = P.alloc([8, 542])
    xer = P.R()
    vv = P.alloc([2 * 8 * 512])
    vvr = P.R()
    pe = vv[:, 0:8 * 542].rearrange("p (a b) -> p a b", a=8)
    per = vvr
    v = vv[:, 0:4096].rearrange("p (a b) -> p a b", a=8)
    v2 = vv[:, 4096:8192].rearrange("p (a b) -> p a b", a=8)
    v2r = vvr
    xo = v2
    xor_ = vvr
    he = P.alloc([8, 542], BF16)
    her = P.R()
    act = he
    actr = her
    ue = P.alloc([8, 542], BF16)
    uer = P.R()
    sg = [P.alloc([512]) for _ in range(2)]
    sgr = P.R(2)
    st = P.alloc([4, 512])
    str_ = P.R()
    tmp = [P.alloc([512]) for _ in range(2)]
    tmr = P.R(2)
    def tile_body(t):
        W = t["W"]
        We = W + 30
        j = t["j"]
        if t.get("zero_halo"):
            P.V(lambda e: e.memset(xe[:, :, :], 0.0), [], [xer])
            P.dma("sync", xe[:, :, 15:15 + W], t["src"], reads=t["in_regions"], writes=[xer])
        else:
            if isinstance(t["src"], list):
                for (o_, w_, ap_) in t["src"]:
                    P.dma("sync", xe[:, :, o_:o_ + w_], ap_, reads=t["in_regions"], writes=[xer])
            else:
                P.dma("sync", xe[:, :, 0:We], t["src"], reads=t["in_regions"], writes=[xer])
        if t.get("pos") is not None:
            P.dma("scalar", pe[:, :, 0:We], t["pos"], writes=[per])
            P.V(lambda e, We=We: e.tensor_tensor(out=xe[:, :, 0:We], in0=xe[:, :, 0:We], in1=pe[:, :, 0:We], op=ALU.add),
                [xer, per], [xer])
        if getattr(E, "dbgx", None) is not None and t["col"] == 512:
            P.dma("sync", E.dbgx[:, 0:8, 0:We], xe[:, :, 0:We], reads=[xer], is_out=True)
        rms_mod(E, rc, xe, xer, We, sc["gsc1"], sc["sh1"], j, sc["r"], he, her)
        if getattr(E, "dbgx", None) is not None and t["col"] == 512:
            P.dma("gpsimd", E.dbgx[:, 8:16, 0:We], he[:, :, 0:We], reads=[her], is_out=True)
        pieces = [(0, min(We, 512))] + ([(512, We)] if We > 512 else [])
        for c in range(8):
            for (a, b) in pieces:
                psa, psar = P.bank()
                psg, psgr = P.bank()
                P.mm(psa[:, 0:b - a], psar, [(w1[:, k, c * 128:(c + 1) * 128], he[:, k, a:b]) for k in range(8)], [w1r, her])
                P.mm(psg[:, 0:b - a], psgr, [(w1[:, k, 1024 + c * 128:1024 + (c + 1) * 128], he[:, k, a:b]) for k in range(8)], [w1r, her])
                i = c % 2
                P.S(lambda e, c=c, a=a, b=b, i=i, psg=psg: e.activation(
                    out=sg[i][:, 0:b - a], in_=psg[:, 0:b - a], func=AF.Sigmoid,
                    bias=vt[:, c_bpw1 + 8 + c:c_bpw1 + 9 + c], scale=1.0), [psgr, vr], [sgr[i]])
                P.V(lambda e, c=c, a=a, b=b, i=i, psa=psa: e.scalar_tensor_tensor(
                    out=ue[:, c, a:b], in0=psa[:, 0:b - a], scalar=vt[:, c_bpw1 + c:c_bpw1 + c + 1], in1=sg[i][:, 0:b - a],
                    op0=ALU.add, op1=ALU.mult), [psar, vr, sgr[i]], [uer])
        for (edge, lo, hi) in ((t.get("ledge"), 0, 15), (t.get("redge"), We - 15, We)):
            if edge is None:
                continue
            if isinstance(edge, float):
                P.V(lambda e, lo=lo, hi=hi: e.memset(ue[:, :, lo:hi], 0.0), [], [uer])
            else:
                P.V(lambda e, lo=lo, hi=hi, edge=edge: e.tensor_scalar(
                    out=ue[:, :, lo:hi], in0=ue[:, :, lo:hi], scalar1=edge, scalar2=None, op0=ALU.mult),
                    [uer, E.edger], [uer])
        for c in range(8):
            di = c % 2
            dg, dgr = dgb[di], dgrs[di]
            for k in range(31):
                P.V(lambda e, c=c, k=k, dg=dg: e.tensor_scalar(out=dg[:, k, :], in0=E.ident,
                                                               scalar1=vt[:, c_wdw + c * 31 + k:c_wdw + c * 31 + k + 1],
                                                               scalar2=None, op0=ALU.mult), [E.cr, vr], [dgr])
            ps, psr = P.bank()
            P.mm(ps[:, 0:W], psr, [(dg[:, k, :], ue[:, c, k:k + W]) for k in range(31)], [dgr, uer])
            P.S(lambda e, c=c, ps=ps: e.activation(out=v[:, c, 0:W], in_=ps[:, 0:W], func=AF.Identity,
                                                   bias=vt[:, c_bdw + c:c_bdw + c + 1], scale=1.0), [psr, vr], [vvr])
            P.V(lambda e, c=c: e.tensor_tensor(out=v2[:, c, 0:W], in0=v[:, c, 0:W], in1=v[:, c, 0:W], op=ALU.mult),
                [vvr], [v2r])
        if getattr(E, "dbgx", None) is not None and t["col"] == 512:
            P.dma("gpsimd", E.dbgx[:, 16:24, 0:We], ue[:, :, 0:We], reads=[uer], is_out=True)
            P.dma("sync", E.dbgx[:, 24:32, 0:W], v[:, :, 0:W], reads=[vvr], is_out=True)
        ps1, ps1r = P.bank()
        ps2, ps2r = P.bank()
        P.mm(ps1[:, 0:W], ps1r, [(E.ones, v[:, c, 0:W]) for c in range(8)], [E.cr, vvr])
        P.mm(ps2[:, 0:W], ps2r, [(E.ones, v2[:, c, 0:W]) for c in range(8)], [E.cr, v2r])
        mean, var, rstd, nmr = st[:, 0, 0:W], st[:, 1, 0:W], st[:, 2, 0:W], st[:, 3, 0:W]
        P.V(lambda e: e.tensor_scalar(out=mean, in0=ps1[:, 0:W], scalar1=1.0 / D, scalar2=None, op0=ALU.mult), [ps1r], [str_])
        P.V(lambda e: e.tensor_tensor(out=var, in0=mean, in1=mean, op=ALU.mult), [str_], [str_])
        P.V(lambda e: e.scalar_tensor_tensor(out=var, in0=ps2[:, 0:W], scalar=1.0 / D, in1=var, op0=ALU.mult, op1=ALU.subtract),
            [ps2r, str_], [str_])
        P.V(lambda e: e.tensor_scalar(out=rstd, in0=var, scalar1=float(EPS), scalar2=None, op0=ALU.add), [str_], [str_])
        P.S(lambda e: e.sqrt(out=rstd, in_=rstd), [str_], [str_])
        P.V(lambda e: e.reciprocal(out=rstd, in_=rstd), [str_], [str_])
        for c in range(8):
            i = c % 2
            P.V(lambda e, c=c, i=i: e.tensor_tensor(out=tmp[i][:, 0:W], in0=v[:, c, 0:W], in1=mean, op=ALU.subtract),
                [vvr, str_], [tmr[i]])
            P.V(lambda e, c=c, i=i: e.tensor_tensor(out=tmp[i][:, 0:W], in0=tmp[i][:, 0:W], in1=rstd, op=ALU.mult),
                [str_, tmr[i]], [tmr[i]])
            P.S(lambda e, c=c, i=i: e.activation(out=act[:, c, 0:W], in_=tmp[i][:, 0:W], func=AF.Silu,
                                                 bias=vt[:, c_lnb + c:c_lnb + c + 1], scale=vt[:, c_lng + c:c_lng + c + 1]),
                [tmr[i], vr], [actr])
        if getattr(E, "dbgx", None) is not None and t["col"] == 512:
            P.dma("gpsimd", E.dbgx[:, 32:40, 0:W], act[:, :, 0:W], reads=[actr], is_out=True)
            P.dma("sync", E.dbgx[:, 40:44, 0:W], st[:, :, 0:W], reads=[str_], is_out=True)
        for c in range(8):
            ps, psr = P.bank()
            P.mm(ps[:, 0:W], psr, [(w2[:, k, c * 128:(c + 1) * 128], act[:, k, 0:W]) for k in range(8)], [w2r, actr])
            P.V(lambda e, c=c, ps=ps: e.scalar_tensor_tensor(
                out=xo[:, c, 0:W], in0=ps[:, 0:W], scalar=sc["g1"][:, c, j:j + 1], in1=xe[:, c, 15:15 + W],
                op0=ALU.mult, op1=ALU.add), [psr, sc["modr"], xer], [xor_])
            P.S(lambda e, c=c: e.activation(out=xo[:, c, 0:W], in_=xo[:, c, 0:W], func=AF.Identity,
                                            bias=g1b[:, c, j:j + 1], scale=1.0), [xor_, g1br], [xor_])
        P.dma("sync", xout[:, :, t["ocol"]:t["ocol"] + W], xo[:, :, 0:W], reads=[xor_], writes=t["out_regions"])
    for t in tiles:
        tile_body(t)
    P.release()


def stage_ffn(E, groups, xin, sc, wg, wu, wd, xout, moe=None):
    P = E.P
    P.mark()
    GW = max(g["W"] for g in groups)
    x = P.alloc([8, GW])
    xr = P.R()
    tok = P.alloc([8, GW], BF16)
    tokr = P.R()
    h = P.alloc([FC, GW], BF16)
    hr = P.R()
    rc = dict(sq=h[:, 0:8, :], sqr=hr, rstd=P.alloc([GW]), rr=P.R(), tmp=[P.alloc([GW]) for _ in range(2)], tr=P.R(2))
    wgb = [P.alloc([8, 512], BF16) for _ in range(2)]
    wub = [P.alloc([8, 512], BF16) for _ in range(2)]
    wgr, wur = P.R(2), P.R(2)
    wdb = [P.alloc([FC, 128], BF16) for _ in range(2)]
    wdr = P.R(2)
    sgt = [P.alloc([512]) for _ in range(2)]
    sgr = P.R(2)
    nexp = 1
    if moe is not None:
        nexp = NE
        tokf = h[:, 8:24, :].rearrange("p a b -> p (a b)").bitcast(F32).rearrange("p (a b) -> p a b", a=8)
        wrt = P.alloc([8, 8])
        wrr = P.R()
        P.dma("sync", wrt, moe["wr"].rearrange("(k p) n -> p k n", p=128), writes=[wrr])
        nbm = GW // 128
        lg = P.alloc([nbm, 8])
        l2 = P.alloc([nbm, 8])
        sp = P.alloc([nbm, 8])
        sm = P.alloc([4, nbm])
        lgr = P.R()
        gT = P.alloc([GW], parts=8)
        gTr = P.R()
        gb = [P.alloc([GW])] * 2
        gbr = [P.R()] * 2
        sg2 = [P.alloc([512]) for _ in range(2)]
        sg2r = P.R(2)
    wgv = wg.rearrange("e (k p) n -> e p k n", p=128) if moe else wg.rearrange("(k p) n -> p k n", p=128)
    wuv = wu.rearrange("e (k p) n -> e p k n", p=128) if moe else wu.rearrange("(k p) n -> p k n", p=128)
    wdv = wd.rearrange("e (f p) n -> e p f n", p=128) if moe else wd.rearrange("(f p) n -> p f n", p=128)
    ld = 0
    def group_body(g):
        nonlocal ld
        W, j, col = g["W"], g["j"], g["col"]
        subs = [(a, min(a + 512, W)) for a in range(0, W, 512)]
        P.dma("sync", x[:, :, 0:W], xin[:, :, col:col + W], reads=g["in_regions"], writes=[xr])
        if moe is None:
            rms_mod(E, rc, x, xr, W, sc["gsc2"], sc["sh2"], j, sc["r"], tok, tokr)
        else:
            rms_mod(E, rc, x, xr, W, sc["gsc2"], sc["sh2"], j, sc["r"], tok, tokr, out_f32=tokf, out_f32_r=hr)
            nb = W // 128
            psl, pslr = P.bank()
            for tb in range(nb):
                P.mm(psl[:, tb * 8:(tb + 1) * 8], pslr,
                     [(tokf[:, k, tb * 128:(tb + 1) * 128], wrt[:, k, :]) for k in range(8)], [hr, wrr])
            m1, m2, nm1, den = sm[:, 0, 0:nb], sm[:, 1, 0:nb], sm[:, 2, 0:nb], sm[:, 3, 0:nb]
            P.V(lambda e: e.tensor_copy(out=lg[:, 0:nb, :], in_=psl[:, 0:nb * 8].rearrange("p (a b) -> p a b", b=8)),
                [pslr], [lgr])
            P.V(lambda e: e.tensor_reduce(out=m1, in_=lg[:, 0:nb, :], axis=AX.X, op=ALU.max), [lgr], [lgr])
            P.V(lambda e: e.tensor_scalar(out=nm1, in0=m1, scalar1=-1.0, scalar2=None, op0=ALU.mult), [lgr], [lgr])
            for tb in range(nb):
                P.V(lambda e, tb=tb: e.tensor_scalar(out=l2[:, tb, :], in0=lg[:, tb, :], scalar1=sm[:, 0, tb:tb + 1],
                                                     scalar2=-1e30, op0=ALU.is_equal, op1=ALU.mult), [lgr], [lgr])
            P.V(lambda e: e.tensor_tensor(out=l2[:, 0:nb, :], in0=l2[:, 0:nb, :], in1=lg[:, 0:nb, :], op=ALU.add), [lgr], [lgr])
            P.V(lambda e: e.tensor_reduce(out=m2, in_=l2[:, 0:nb, :], axis=AX.X, op=ALU.max), [lgr], [lgr])
            for tb in range(nb):
                P.S(lambda e, tb=tb: e.activation(out=sp[:, tb, :], in_=lg[:, tb, :], func=AF.Exp,
                                                  bias=sm[:, 2, tb:tb + 1], scale=1.0), [lgr], [lgr])
                P.V(lambda e, tb=tb: e.scalar_tensor_tensor(out=sp[:, tb, :], in0=lg[:, tb, :], scalar=sm[:, 1, tb:tb + 1],
                                                            in1=sp[:, tb, :], op0=ALU.is_ge, op1=ALU.mult), [lgr], [lgr])
            P.V(lambda e: e.tensor_reduce(out=den, in_=sp[:, 0:nb, :], axis=AX.X, op=ALU.add), [lgr], [lgr])
            P.V(lambda e: e.reciprocal(out=den, in_=den), [lgr], [lgr])
            for tb in range(nb):
                P.V(lambda e, tb=tb: e.tensor_scalar(out=sp[:, tb, :], in0=sp[:, tb, :], scalar1=sm[:, 3, tb:tb + 1],
                                                     scalar2=None, op0=ALU.mult), [lgr], [lgr])
            pst, pstr = P.bank()
            pst2, pst2r = P.bank()
            for tb in range(nb):
                pp, ppr = (pst, pstr) if tb < 4 else (pst2, pst2r)
                o = (tb % 4) * 128
                P.mm(pp[0:8, o:o + 128], ppr, [(sp[:, tb, :], E.ident)], [lgr, E.cr])
            P.S(lambda e: e.copy(out=gT[:, 0:min(W, 512)], in_=pst[0:8, 0:min(W, 512)]), [pstr], [gTr])
            if W > 512:
                P.S(lambda e: e.copy(out=gT[:, 512:W], in_=pst2[0:8, 0:W - 512]), [pst2r], [gTr])
        for ex in range(nexp):
            if moe is not None:
                gi = ex % 2
                for (a, b) in subs:
                    ps, psr = P.bank()
                    P.mm(ps[:, 0:b - a], psr, [(moe["selc"][:, ex * 128:(ex + 1) * 128], gT[:, a:b])], [moe["selr"], gTr])
                    P.S(lambda e, a=a, b=b, ps=ps, gi=gi: e.copy(out=gb[gi][:, a:b], in_=ps[:, 0:b - a]), [psr], [gbr[gi]])
            for fb in range(7):
                i = ld % 2
                ld += 1
                srcg = wgv[ex][:, :, fb * 512:(fb + 1) * 512] if moe else wgv[:, :, fb * 512:(fb + 1) * 512]
                srcu = wuv[ex][:, :, fb * 512:(fb + 1) * 512] if moe else wuv[:, :, fb * 512:(fb + 1) * 512]
                P.dma("gpsimd", wgb[i], srcg, writes=[wgr[i]])
                P.dma("gpsimd", wub[i], srcu, writes=[wur[i]])
                for (a, b) in subs:
                    for fc in range(4):
                        f = fb * 4 + fc
                        psg, psgr = P.bank()
                        psu, psur = P.bank()
                        P.mm(psg[:, 0:b - a], psgr, [(wgb[i][:, k, fc * 128:(fc + 1) * 128], tok[:, k, a:b]) for k in range(8)],
                             [wgr[i], tokr])
                        P.mm(psu[:, 0:b - a], psur, [(wub[i][:, k, fc * 128:(fc + 1) * 128], tok[:, k, a:b]) for k in range(8)],
                             [wur[i], tokr])
                        si = f % 2
                        P.S(lambda e, psg=psg, si=si, a=a, b=b: e.activation(out=sgt[si][:, 0:b - a], in_=psg[:, 0:b - a],
                                                                             func=AF.Silu), [psgr], [sgr[si]])
                        if moe is None:
                            P.V(lambda e, psu=psu, si=si, a=a, b=b, f=f: e.tensor_tensor(
                                out=h[:, f, a:b], in0=sgt[si][:, 0:b - a], in1=psu[:, 0:b - a], op=ALU.mult),
                                [psur, sgr[si]], [hr])
                        else:
                            P.G(lambda e, si=si, a=a, b=b, gi=gi: e.tensor_tensor(
                                out=sg2[si][:, 0:b - a], in0=sgt[si][:, 0:b - a], in1=gb[gi][:, a:b], op=ALU.mult),
                                [sgr[si], gbr[gi]], [sg2r[si]])
                            P.V(lambda e, psu=psu, si=si, a=a, b=b, f=f: e.tensor_tensor(
                                out=h[:, f, a:b], in0=sg2[si][:, 0:b - a], in1=psu[:, 0:b - a], op=ALU.mult),
                                [psur, sg2r[si]], [hr])
            for d in range(8):
                i = ld % 2
                ld += 1
                srcd = wdv[ex][:, :, d * 128:(d + 1) * 128] if moe else wdv[:, :, d * 128:(d + 1) * 128]
                P.dma("gpsimd", wdb[i], srcd, writes=[wdr[i]])
                for (a, b) in subs:
                    ps, psr = P.bank()
                    P.mm(ps[:, 0:b - a], psr, [(wdb[i][:, f, :], h[:, f, a:b]) for f in range(FC)], [wdr[i], hr])
                    P.V(lambda e, ps=ps, a=a, b=b, d=d: e.scalar_tensor_tensor(
                        out=x[:, d, a:b], in0=ps[:, 0:b - a], scalar=sc["g2"][:, d, j:j + 1], in1=x[:, d, a:b],
                        op0=ALU.mult, op1=ALU.add), [psr, sc["modr"], xr], [xr])
        P.dma("sync", xout[:, :, col:col + W], x[:, :, 0:W], reads=[xr], writes=g["out_regions"], is_out=g.get("is_out", False))
    for g in groups:
        group_body(g)
    P.release()


def stage_pre(E, tiles, xin, sc, w_in, n_out, bias, uout):
    P = E.P
    P.mark()
    w = P.alloc([8, n_out * 128], BF16)
    wr = P.R()
    wv = w_in.rearrange("(k p) n -> p k n", p=128)
    for k in range(8):
        P.dma("gpsimd", w[:, k, :], wv[:, k, :], writes=[wr])
    rc = rms_ctx(E, 512)
    x = P.alloc([8, 512])
    xr = P.R()
    hb = P.alloc([8, 512], BF16)
    hbr = P.R()
    ob = [P.alloc([8, 512]) for _ in range(2)]
    obr = P.R(2)
    n8 = 0
    def tile_body(t):
        nonlocal n8
        W, j, col = t["W"], t["j"], t["col"]
        P.dma("sync", x[:, :, 0:W], xin[:, :, col:col + W], reads=t["in_regions"], writes=[xr])
        rms_mod(E, rc, x, xr, W, sc["gsc1"], sc["sh1"], j, sc["r"], hb, hbr)
        for o8 in range(n_out // 8):
            i = n8 % 2
            n8 += 1
            for oo in range(8):
                oc = o8 * 8 + oo
                ps, psr = P.bank()
                P.mm(ps[:, 0:W], psr, [(w[:, k, oc * 128:(oc + 1) * 128], hb[:, k, 0:W]) for k in range(8)], [wr, hbr])
                if bias is not None:
                    bvt, bcol, bvr = bias
                    P.S(lambda e, ps=ps, oc=oc, oo=oo, i=i: e.activation(out=ob[i][:, oo, 0:W], in_=ps[:, 0:W], func=AF.Identity,
                                                                         bias=bvt[:, bcol + oc:bcol + oc + 1], scale=1.0),
                        [psr, bvr], [obr[i]])
                elif oo % 2 == 0:
                    P.S(lambda e, ps=ps, oo=oo, i=i: e.copy(out=ob[i][:, oo, 0:W], in_=ps[:, 0:W]), [psr], [obr[i]])
                else:
                    P.V(lambda e, ps=ps, oo=oo, i=i: e.tensor_copy(out=ob[i][:, oo, 0:W], in_=ps[:, 0:W]), [psr], [obr[i]])
            P.dma("sync", uout[:, o8 * 8:(o8 + 1) * 8, col:col + W], ob[i][:, :, 0:W], reads=[obr[i]], writes=[], is_out=True)
    for t in tiles:
        tile_body(t)
    P.release()


def tile_list(ncols_lat=TPC, with_ctx=True, step=512):
    tl = [dict(col=c, W=min(step, ncols_lat - c), j=0) for c in range(0, ncols_lat, step)]
    if with_ctx:
        tl.append(dict(col=ncols_lat, W=CTX, j=1))
    return tl


NCA = TPC + CTX


def col_regions(regs, col, W):
    return regs[col // 512:(col + W + 511) // 512]


VA = dict(c=0, adab0=16, adab1=64, n1_0=112, n2_0=120, n1_1=128, bpw1=136, bdw=152, lng=160, lnb=168, bpw2=176,
          wdw=184, hyb=432, edge=456, NV=458)


def build_A(debug=False):
    E = Env("A")
    P = E.P
    E.consts()
    xe_d = E.inp("xe", [8, 128, TPC + 32]).rearrange("c p w -> p c w")
    pos_d = E.inp("pos", [8, 128, TPC + 32]).rearrange("c p w -> p c w")
    ctx_d = E.inp("ctxT", [8, 128, CTX]).rearrange("c p w -> p c w")
    adaw = E.inp("ada_w", [2, 1024, 6144])
    wpw1 = E.inp("w_pw1", [1024, 2048])
    wpw2 = E.inp("w_pw2", [1024, 1024])
    wg = E.inp("w_gate", [1024, DFF])
    wu = E.inp("w_up", [1024, DFF])
    wd = E.inp("w_down", [DFF, 1024])
    win = E.inp("w_in", [1024, 3072])
    vt, vr = load_small(E, "vecs", VA["NV"])
    E.edger = vr
    xm = (E.out("xm", [8, 128, NCA]) if debug else E.scratch("xm", [8, 128, NCA])).rearrange("c p w -> p c w")
    x0 = (E.out("x0", [8, 128, NCA]) if True else E.scratch("x0", [8, 128, NCA])).rearrange("c p w -> p c w")
    uo = E.out("u_pre", [24, 128, NCA]).rearrange("c p w -> p c w")
    nreg = (NCA + 511) // 512
    xmr, x0r = P.R(nreg), P.R(nreg)
    modT = P.alloc([48, 2])
    modr = P.R()
    compute_mod(E, adaw[0], vt, vr, VA["c"], VA["adab0"], modT, modr)
    sc0 = layer_scalars(E, modT, modr, vt, vr, VA["n1_0"], VA["n2_0"])
    if debug:
        dbg = E.out("dbg", [128, 128])
        E.dbgx = E.out("dbgx", [128, 44, 542])
        P.dma("sync", dbg[:, 0:96], modT.rearrange("p a b -> p (a b)"), reads=[modr], is_out=True)
        P.dma("sync", dbg[:, 96:112], sc0["gsc1"].rearrange("p a b -> p (a b)"), reads=[sc0["r"]], is_out=True)
        P.dma("sync", dbg[:, 112:128], sc0["gsc2"].rearrange("p a b -> p (a b)"), reads=[sc0["r"]], is_out=True)
    tiles = []
    for t in tile_list():
        t = dict(t)
        if t["j"] == 0:
            c0 = t["col"] + 1
            t.update(src=xe_d[:, :, c0:c0 + 542], pos=pos_d[:, :, c0:c0 + 542], in_regions=[], ocol=t["col"])
            if t["col"] == 0:
                t["ledge"] = vt[:, VA["edge"]:VA["edge"] + 1]
            if t["col"] == TPC - 512:
                t["redge"] = vt[:, VA["edge"] + 1:VA["edge"] + 2]
        else:
            t.update(src=ctx_d, pos=None, in_regions=[], ocol=t["col"], zero_halo=True, ledge=0.0, redge=0.0)
        t["out_regions"] = col_regions(xmr, t["col"], t["W"])
        tiles.append(t)
    stage_conformer(E, tiles, wpw1, wpw2, vt, vr,
                    (VA["bpw1"], VA["bdw"], VA["lng"], VA["lnb"], VA["bpw2"], VA["wdw"]), sc0, xm, None)
    groups = []
    for g in tile_list(step=1024):
        g = dict(g)
        g["in_regions"] = col_regions(xmr, g["col"], g["W"])
        g["out_regions"] = col_regions(x0r, g["col"], g["W"])
        g["is_out"] = True
        groups.append(g)
    stage_ffn(E, groups, xm, sc0, wg, wu, wd, x0)
    modT1 = P.alloc([48, 2])
    modr1 = P.R()
    compute_mod(E, adaw[1], vt, vr, VA["c"], VA["adab1"], modT1, modr1)
    sc1 = layer_scalars(E, modT1, modr1, vt, vr, VA["n1_1"], VA["n1_1"])
    ptiles = []
    for t in tile_list():
        t = dict(t)
        t["in_regions"] = col_regions(x0r, t["col"], t["W"])
        ptiles.append(t)
    stage_pre(E, ptiles, x0, sc1, win, 24, (vt, VA["hyb"], vr), uo)
    P.finish()
    P.emit()
    return E


def pos_table():
    quarter = D // 4
    omega = (1.0 / (10000.0 ** (np.arange(quarter, dtype=np.float32) / np.float32(quarter)))).astype(np.float32)
    t = np.arange(SEQ)
    r = (t // GRID_W).astype(np.float32)[:, None] * omega
    col = (t % GRID_W).astype(np.float32)[:, None] * omega
    return np.concatenate([np.sin(r), np.cos(r), np.sin(col), np.cos(col)], axis=-1).astype(np.float32)


def chunked_T(a):
    T, C = a.shape
    return np.ascontiguousarray(a.T.reshape(C // 128, 128, T))


def unchunk_T(a):
    n, p, T = a.shape
    return np.ascontiguousarray(a.reshape(n * p, T).T)


def window_T(a, t0, lo, hi):
    S, C = a.shape
    out = np.zeros((hi - lo, C), np.float32)
    s0, s1 = max(0, t0 + lo), min(S, t0 + hi)
    out[s0 - (t0 + lo):s1 - (t0 + lo)] = a[s0:s1]
    return chunked_T(out)


def prep_A(inp):
    pos = pos_table()
    maps = []
    cst = host_consts()
    for k in range(NCORE):
        b, s = k // 4, k % 4
        t0 = s * TPC
        v = np.zeros((128, VA["NV"]), np.float32)
        cc = np.stack([fm(inp["c"][b]), fm(inp["c_ctx"])], axis=-1)
        v[:, VA["c"]:VA["c"] + 16] = cc.reshape(128, 16)
        v[:, VA["adab0"]:VA["adab0"] + 48] = fm(inp["ada_b"][0])
        v[:, VA["adab1"]:VA["adab1"] + 48] = fm(inp["ada_b"][1])
        v[:, VA["n1_0"]:VA["n1_0"] + 8] = fm(inp["norm1_g"][0])
        v[:, VA["n2_0"]:VA["n2_0"] + 8] = fm(inp["norm2_g"][0])
        v[:, VA["n1_1"]:VA["n1_1"] + 8] = fm(inp["norm1_g"][1])
        v[:, VA["bpw1"]:VA["bpw1"] + 16] = fm(inp["cf_b_pw1"][0])
        v[:, VA["bdw"]:VA["bdw"] + 8] = fm(inp["cf_b_dw"][0])
        v[:, VA["lng"]:VA["lng"] + 8] = fm(inp["cf_ln_g"][0])
        v[:, VA["lnb"]:VA["lnb"] + 8] = fm(inp["cf_ln_b"][0])
        v[:, VA["bpw2"]:VA["bpw2"] + 8] = fm(inp["cf_b_pw2"][0])
        wdw = np.asarray(inp["cf_w_dw"][0], np.float32)
        v[:, VA["wdw"]:VA["wdw"] + 248] = wdw.T.reshape(8, 128, 31).transpose(1, 0, 2).reshape(128, 248)
        v[:, VA["hyb"]:VA["hyb"] + 24] = fm(inp["hy_b_in"][0])
        v[:, VA["edge"]] = 0.0 if s == 0 else 1.0
        v[:, VA["edge"] + 1] = 0.0 if s == 3 else 1.0
        maps.append({
            "consts": cst,
            "xe": window_T(np.asarray(inp["x"][b]), t0, -16, TPC + 16),
            "pos": window_T(pos, t0, -16, TPC + 16),
            "ctxT": chunked_T(np.asarray(inp["ctx"][b])),
            "ada_w": np.ascontiguousarray(inp["ada_w"][0:2]),
            "w_pw1": np.asarray(inp["cf_w_pw1"][0]), "w_pw2": np.asarray(inp["cf_w_pw2"][0]),
            "w_gate": np.asarray(inp["ffn_w_gate"][0]), "w_up": np.asarray(inp["ffn_w_up"][0]),
            "w_down": np.asarray(inp["ffn_w_down"][0]), "w_in": np.asarray(inp["hy_w_in"][0]),
            "vecs": v,
        })
    return maps


HY_L = SEQ
TWO_PI = 2.0 * math.pi
VB = dict(b1=0, fr=1, b2=2, b3=3, nd=4, ndc=5, negpi=6, NV=8)


def hyena_filter_stage(E, zT, atau, L2, wf, vt, vr, G, Gr, asum, asr, ndcol):
    P = E.P
    wf1, wf2, wf3, wfr = wf
    nt = L2 // 512
    zt = [P.alloc([512]) for _ in range(2)]
    ztr = P.R(2)
    at = [P.alloc([512]) for _ in range(2)]
    atr = P.R(2)
    hh = [P.alloc([512]) for _ in range(3)]
    hr = P.R(3)
    dec = P.alloc([512])
    decr = P.R()
    gr_ = [P.alloc([512]) for _ in range(2)]
    grr = P.R(2)
    ab = P.alloc([512])
    abr = P.R()
    gb = [P.alloc([512], BF16) for _ in range(2)]
    gbr = P.R(2)
    wrp = P.alloc([512])
    wrp2 = P.alloc([512])
    wrr = P.R()

    def tile(ti):
        i = ti % 2
        c0 = ti * 512
        P.dma("sync", zt[i][0:33, :], zT[:, c0:c0 + 512], writes=[ztr[i]])
        P.dma("sync", at[i], atau[0:1, c0:c0 + 512].partition_broadcast(128), writes=[atr[i]])
        src, srcr = zt[i][0:33, :], ztr[i]
        for l in range(3):
            ps, psr = P.bank()
            lhs = wf1 if l == 0 else wf2[l - 1]
            P.mm(ps[0:64, :], psr, [(lhs, src)], [wfr, srcr])
            h = hh[l]
            P.V(lambda e, ps=ps, h=h, l=l: e.tensor_scalar(out=h[0:64, :], in0=ps[0:64, :],
                                                           scalar1=vt[0:64, VB["b1"] + l:VB["b1"] + l + 1] if l == 0 else vt[0:64, VB["b2"] + l - 1:VB["b2"] + l],
                                                           scalar2=vt[0:64, VB["fr"]:VB["fr"] + 1], op0=ALU.add, op1=ALU.mult),
                [psr, vr], [hr[l]])
            P.V(lambda e, h=h: e.tensor_scalar(out=wrp[0:64, :], in0=h[0:64, :], scalar1=-math.pi, scalar2=TWO_PI,
                                               op0=ALU.is_lt, op1=ALU.mult), [hr[l]], [wrr])
            P.V(lambda e, h=h: e.tensor_scalar(out=wrp2[0:64, :], in0=h[0:64, :], scalar1=math.pi, scalar2=-TWO_PI,
                                               op0=ALU.is_gt, op1=ALU.mult), [hr[l]], [wrr])
            P.V(lambda e, h=h: e.tensor_tensor(out=h[0:64, :], in0=h[0:64, :], in1=wrp[0:64, :], op=ALU.add), [hr[l], wrr], [hr[l]])
            P.V(lambda e, h=h: e.tensor_tensor(out=h[0:64, :], in0=h[0:64, :], in1=wrp2[0:64, :], op=ALU.add), [hr[l], wrr], [hr[l]])
            P.S(lambda e, h=h: e.activation(out=h[0:64, :], in_=h[0:64, :], func=AF.Sin), [hr[l]], [hr[l]])
            src, srcr = h[0:64, :], hr[l]
        P.S(lambda e, i=i: e.activation(out=dec, in_=at[i], func=AF.Exp, scale=vt[:, ndcol:ndcol + 1]), [atr[i], vr], [decr])
        halves = [(0, 512, 1 if c0 < L2 // 2 else 0)] if L2 > 512 else [(0, 256, 1), (256, 512, 0)]
        for o in range(2):
            ps, psr = P.bank()
            for (a, b, dr) in halves:
                blk = (o * 2 + dr) * 128
                P.mm(ps[:, a:b], psr, [(wf3[:, blk:blk + 128], src[:, a:b])], [wfr, srcr])
            g = gr_[o]
            P.V(lambda e, ps=ps, g=g: e.tensor_tensor(out=g, in0=ps[:, :], in1=dec, op=ALU.mult), [psr, decr], [grr[o]])
            if ti == 0:
                P.V(lambda e, g=g: e.memset(g[:, 0:1], 0.0), [], [grr[o]])
            P.S(lambda e, g=g: e.activation(out=ab, in_=g, func=AF.Abs), [grr[o]], [abr])
            P.V(lambda e, o=o: e.reduce_sum(out=asum[:, o, ti:ti + 1], in_=ab, axis=AX.X), [abr], [asr])
            P.S(lambda e, g=g, o=o: e.copy(out=gb[o], in_=g), [grr[o]], [gbr[o]])
            P.dma("sync", G[o][:, c0:c0 + 512], gb[o], reads=[gbr[o]], writes=[Gr])

    for ti in range(nt):
        tile(ti)


def build_B():
    E = Env("B")
    P = E.P
    E.consts()
    L = HY_L
    hu = E.inp("hu", [128, 6, L + 2])
    hc = E.inp("hc", [128, 6, CTX + 2])
    zT = E.inp("zT", [33, 2 * L])
    zcT = E.inp("zcT", [33, 512])
    atau = E.inp("atau", [1, 2 * L])
    atauc = E.inp("atauc", [1, 512])
    wf1_d = E.inp("wf1", [33, 64])
    wf2_d = E.inp("wf2", [64, 128])
    wf3_d = E.inp("wf3", [64, 512])
    hz = E.out("hz", [128, 2, L])
    hzc = E.out("hzc", [128, 2, CTX])
    vt, vr = load_small(E, "vecs", VB["NV"])
    hws, hwsr = load_small(E, "hws", 128 * 12)
    hbs, hbsr = load_small(E, "hbias", 128 * 2)
    G = E.scratch("G", [2, 128, 2 * L], BF16)
    Gc = E.scratch("Gc", [2, 128, 512], BF16)
    invd = E.scratch("invd", [128, 4])
    Gr, Gcr, invr = P.R(), P.R(), P.R()
    wfall = P.alloc([64 + 128 + 512])
    wfr = P.R()
    P.dma("sync", wfall[0:33, 0:64], wf1_d, writes=[wfr])
    P.dma("sync", wfall[0:64, 64:192], wf2_d, writes=[wfr])
    P.dma("sync", wfall[0:64, 192:704], wf3_d, writes=[wfr])
    wf = (wfall[0:33, 0:64], [wfall[0:64, 64:128], wfall[0:64, 128:192]], wfall[0:64, 192:704], wfr)
    asum = P.alloc([2, 64])
    asumc = P.alloc([2, 1])
    asr = P.R()
    inv = P.alloc([4])
    invb = P.alloc([128 * 4])
    invbr = P.R()
    P.mark()
    hyena_filter_stage(E, zT, atau, 2 * L, wf, vt, vr, G, Gr, asum, asr, VB["nd"])
    hyena_filter_stage(E, zcT, atauc, 512, wf, vt, vr, Gc, Gcr, asumc, asr, VB["ndc"])
    P.V(lambda e: e.reduce_sum(out=inv[:, 0:2], in_=asum, axis=AX.X), [asr], [asr])
    P.V(lambda e: e.tensor_copy(out=inv[:, 2:4], in_=asumc[:, :, 0]), [asr], [asr])
    P.V(lambda e: e.reciprocal(out=inv, in_=inv), [asr], [asr])
    P.dma("sync", invd, inv, reads=[asr], writes=[invr])
    P.dma("sync", invb, invd.rearrange("c f -> (c f)").partition_broadcast(128), reads=[invr], writes=[invbr])
    P.release()

    A = [P.alloc([6, 130]) for _ in range(2)]
    Ar = P.R(2)
    Ac = [P.alloc([6, 130]) for _ in range(2)]
    Acr = P.R(2)
    y = P.alloc([6, 128])
    yr = P.R()
    yc = P.alloc([6, 128])
    ycr = P.R()
    SK = [P.alloc([16384], BF16) for _ in range(4)]
    SKr = P.R(4)
    SKc = [P.alloc([384], BF16) for _ in range(2)]
    SKcr = P.R(2)
    st = dict(sk=0)

    def new_set(NB):
        n = 2 * NB
        return dict(Zb=P.alloc([2, NB], BF16), vf=P.alloc([2, NB]), x1=P.alloc([2, NB]), x2=P.alloc([2, NB]),
                    z1=P.alloc([2, NB]), z1b=P.alloc([2, NB], BF16), t1=P.alloc([2, NB]), z2=P.alloc([2, NB]),
                    zo=P.alloc([2, 128]), r=P.R(), NB=NB)
    S_lat = new_set(128)
    S_ctx = new_set(2)

    def conv(o, Z, Zr, NB, ch, lat):
        ps, psr = P.bank()
        pv = ps[:, 0:2 * NB].rearrange("p (b a) -> p b a", b=2)
        Gs = (G if lat else Gc)[o, ch]
        pieces = [(0, 128), (128, 255)] if lat else [(0, 3)]
        order = []
        for (b0, b1) in pieces:
            if lat:
                hb = st["sk"] % 4
                st["sk"] += 1
                buf, bufr = SK[hb], SKr[hb]
            else:
                hb = st.setdefault("skc", 0) % 2
                st["skc"] = hb + 1
                buf, bufr = SKc[hb], SKcr[hb]
            ncol = (b1 - b0) * 128
            src = bass.AP(Gs.tensor, Gs.offset + 1 + b0 * 128, [[1, 128], [1, ncol]])
            P.dma("sync", buf[:, 0:ncol], src, reads=[Gr if lat else Gcr], writes=[bufr])
            blks = list(range(b0, b1))
            if b0 == 0:
                blks = [NB - 1] + [x for x in blks if x != NB - 1]
            pairs = []
            for blk in blks:
                dl = blk - (NB - 1)
                alo, ahi = max(0, dl), min(NB - 1, NB - 1 + dl)
                pairs.append((pv[:, :, alo:ahi + 1], buf[:, (blk - b0) * 128:(blk - b0 + 1) * 128], Z[:, :, alo - dl:ahi - dl + 1]))
            first = (b0 == 0)
            last = (b1 == pieces[-1][1])

            def fn(e, pairs=pairs, first=first, last=last):
                ins = None
                n = len(pairs)
                for i, (o_, l_, r_) in enumerate(pairs):
                    ins = e.matmul(o_, lhsT=l_, rhs=r_, start=(first and i == 0), stop=(last and i == n - 1))
                return ins
            P.op("tensor", fn, [bufr, Zr], [psr])
        return pv, psr

    def channel(ch, lat):
        S = S_lat if lat else S_ctx
        NB = S["NB"]
        sr = S["r"]
        i = ch % 2
        a, ar = (A[i], Ar[i]) if lat else (Ac[i], Acr[i])
        yy, yyr = (y, yr) if lat else (yc, ycr)
        Lx = L if lat else CTX
        src_t = hu if lat else hc
        base = src_t[ch]
        src = bass.AP(base.tensor, base.offset, [[128, NB], [Lx + 2, 6], [1, 130]])
        P.dma("gpsimd", a[0:NB], src, writes=[ar])
        for s in range(3):
            wc = (ch * 3 + s) * 4
            sl = slice(2 * s, 2 * s + 2)
            P.V(lambda e, sl=sl, wc=wc: e.tensor_scalar(out=yy[0:NB, sl, :], in0=a[0:NB, sl, 0:128], scalar1=hws[0:NB, wc:wc + 1],
                                                        scalar2=hws[0:NB, wc + 3:wc + 4], op0=ALU.mult, op1=ALU.add), [ar, hwsr], [yyr])
            for k in (1, 2):
                P.V(lambda e, sl=sl, wc=wc, k=k: e.scalar_tensor_tensor(out=yy[0:NB, sl, :], in0=a[0:NB, sl, k:k + 128],
                                                                        scalar=hws[0:NB, wc + k:wc + k + 1], in1=yy[0:NB, sl, :],
                                                                        op0=ALU.mult, op1=ALU.add), [ar, hwsr, yyr], [yyr])
        pA, pAr = P.bank()
        pB, pBr = P.bank()
        for sb in range(6):
            pp, ppr = (pA, pAr) if sb < 4 else (pB, pBr)
            o_ = (sb % 4) * NB
            P.mm(pp[:, o_:o_ + NB], ppr, [(yy[0:NB, sb, :], E.ident[0:NB, 0:NB])], [yyr, E.cr])
        v3 = lambda t: t.rearrange("p b a -> p (b a)")
        P.V(lambda e: e.tensor_copy(out=v3(S["vf"]), in_=pA[:, 0:2 * NB]), [pAr], [sr])
        pJ, pJr = P.bank()
        P.mm(pJ[:, 0:2 * NB], pJr, [(E.J, v3(S["vf"]))], [sr, E.cr])
        P.S(lambda e: e.copy(out=v3(S["Zb"]), in_=pJ[:, 0:2 * NB]), [pJr], [sr])
        P.S(lambda e: e.copy(out=v3(S["x1"]), in_=pA[:, 2 * NB:4 * NB]), [pAr], [sr])
        P.V(lambda e: e.tensor_copy(out=v3(S["x2"]), in_=pB[:, 0:2 * NB]), [pBr], [sr])
        ic = ch * 4 + (0 if lat else 2)
        zin, zf = S["Zb"], S["vf"]
        for o in range(2):
            pv, psr = conv(o, zin, sr, NB, ch, lat)
            gate = S["x1"] if o == 0 else S["x2"]
            zo_ = S["z1"] if o == 0 else S["z2"]
            P.V(lambda e, pv=pv, o=o: e.tensor_scalar(out=S["t1"], in0=pv, scalar1=invb[:, ic + o:ic + o + 1], scalar2=None,
                                                      op0=ALU.mult), [psr, invbr], [sr])
            P.V(lambda e, o=o, zf=zf: e.scalar_tensor_tensor(out=S["t1"], in0=zf, scalar=hbs[:, ch * 2 + o:ch * 2 + o + 1],
                                                             in1=S["t1"], op0=ALU.mult, op1=ALU.add), [sr, hbsr], [sr])
            P.V(lambda e, gate=gate, zo_=zo_: e.tensor_tensor(out=zo_, in0=S["t1"], in1=gate, op=ALU.mult), [sr], [sr])
            if o == 0:
                pJ2, pJ2r = P.bank()
                P.mm(pJ2[:, 0:2 * NB], pJ2r, [(E.J, v3(S["z1"]))], [sr, E.cr])
                P.S(lambda e, pJ2=pJ2: e.copy(out=v3(S["z1b"]), in_=pJ2[:, 0:2 * NB]), [pJ2r], [sr])
                zin, zf = S["z1b"], S["z1"]
        pT, pTr = P.bank()
        for b in range(2):
            P.mm(pT[0:NB, b * 128:(b + 1) * 128], pTr, [(S["z2"][:, b, :], E.ident)], [sr, E.cr])
        P.S(lambda e: e.copy(out=S["zo"][0:NB].rearrange("p b a -> p (b a)"), in_=pT[0:NB, 0:256]), [pTr], [sr])
        dst_t = hz if lat else hzc
        dbase = dst_t[ch]
        dst = bass.AP(dbase.tensor, dbase.offset, [[128, NB], [Lx, 2], [1, 128]])
        P.dma("gpsimd", dst, S["zo"][0:NB], reads=[sr], writes=[], is_out=True)

    for ch in range(128):
        channel(ch, True)
        channel(ch, False)
    P.finish()
    P.emit()
    return E


def hyena_z_table(L):
    f32 = np.float32
    ip = np.arange(2 * L)
    tau = np.minimum(np.abs(ip - L), L - 1)
    t = (np.linspace(0.0, 1.0, L, dtype=f32))[tau][:, None]
    bands = 16
    w = (f32(2.0 * math.pi) * np.arange(L, dtype=f32) / f32(L))[tau][:, None]
    fb = np.linspace(1e-4, bands - 1, bands, dtype=f32)[None, :]
    z = np.concatenate([t, np.cos(fb * w), -np.sin(fb * w)], axis=-1).astype(f32)
    return np.ascontiguousarray(z.T), tau.astype(f32)[None, :]


def prep_B(inp, upre):
    L = HY_L
    zT, atau = hyena_z_table(L)
    zcT, atauc = hyena_z_table(CTX)
    dmin = math.log(1e-2) / 1.5
    dmax = math.log(1e-2) / 0.3
    deltas = np.abs(np.linspace(dmin, dmax, D, dtype=np.float32))
    cst = host_consts()
    wsh = np.asarray(inp["hy_w_short"][0], np.float32)
    bsh = np.asarray(inp["hy_b_short"][0], np.float32)
    hb = np.asarray(inp["hy_bias"][0], np.float32)
    wf3 = np.asarray(inp["hy_w_f3"][0], np.float32)
    maps = []
    for k in range(NCORE):
        hu = np.zeros((128, 6, L + 2), np.float32)
        hc = np.zeros((128, 6, CTX + 2), np.float32)
        for s in range(3):
            for b in range(BATCH):
                for seg in range(4):
                    hu[:, s * 2 + b, 1 + seg * TPC:1 + (seg + 1) * TPC] = upre[4 * b + seg][s * 8 + k][:, 0:TPC]
                hc[:, s * 2 + b, 1:1 + CTX] = upre[4 * b][s * 8 + k][:, TPC:TPC + CTX]
        v = np.zeros((128, VB["NV"]), np.float32)
        v[0:64, VB["b1"]] = inp["hy_b_f1"][0]
        v[0:64, VB["fr"]] = inp["hy_freq"][0]
        v[0:64, VB["b2"]] = inp["hy_b_f2"][0][0]
        v[0:64, VB["b3"]] = inp["hy_b_f2"][0][1]
        v[:, VB["nd"]] = -deltas[k * 128:(k + 1) * 128] / np.float32(L - 1)
        v[:, VB["ndc"]] = -deltas[k * 128:(k + 1) * 128] / np.float32(CTX - 1)
        v[:, VB["negpi"]] = -math.pi
        hws = np.zeros((128, 3, 4), np.float32)
        for s in range(3):
            cs = s * D + k * 128
            hws[:, s, 0:3] = wsh[:, cs:cs + 128].T
            hws[:, s, 3] = bsh[cs:cs + 128]
        hbias = np.ascontiguousarray(hb[:, k * 128:(k + 1) * 128].T)
        w3 = np.concatenate([wf3[:, o * 2 * D + dr * D + k * 128:o * 2 * D + dr * D + (k + 1) * 128]
                             for o in range(2) for dr in range(2)], axis=1)
        maps.append({
            "consts": cst, "hu": hu, "hc": hc, "zT": zT, "zcT": zcT, "atau": atau, "atauc": atauc,
            "wf1": np.asarray(inp["hy_w_f1"][0], np.float32),
            "wf2": np.ascontiguousarray(np.concatenate([inp["hy_w_f2"][0][0], inp["hy_w_f2"][0][1]], axis=1), dtype=np.float32),
            "wf3": np.ascontiguousarray(w3), "vecs": v,
            "hws": np.ascontiguousarray(np.broadcast_to(hws.reshape(1, -1), (128, 128 * 12))),
            "hbias": np.ascontiguousarray(np.broadcast_to(hbias.reshape(1, -1), (128, 256))),
        })
    return maps


def stage_post(E, tiles, xin, rin, sc, w_out, bias, xout):
    P = E.P
    P.mark()
    w = P.alloc([8, 1024], BF16)
    wr = P.R()
    P.dma("gpsimd", w, w_out.rearrange("(k p) n -> p k n", p=128), writes=[wr])
    x = [P.alloc([8, 512]) for _ in range(2)]
    xr = P.R(2)
    rb = [P.alloc([8, 512], BF16) for _ in range(2)]
    rbr = P.R(2)
    g1b = None
    if bias is not None:
        bvt, bcol, bvr = bias
        g1b = P.alloc([8, 2])
        g1br = P.R()
        for j in range(2):
            P.V(lambda e, j=j: e.tensor_tensor(out=g1b[:, :, j], in0=sc["g1"][:, :, j], in1=bvt[:, bcol:bcol + 8], op=ALU.mult),
                [sc["modr"], bvr], [g1br])
    cnt = [0]

    def tile_body(t):
        W, j, col = t["W"], t["j"], t["col"]
        i = cnt[0] % 2
        cnt[0] += 1
        xx, xxr, rr, rrr = x[i], xr[i], rb[i], rbr[i]
        P.dma("sync", xx[:, :, 0:W], xin[:, :, col:col + W], reads=t["in_regions"], writes=[xxr])
        P.dma("gpsimd", rr[:, :, 0:W], rin[:, :, col:col + W], reads=t.get("r_regions", []), writes=[rrr])
        for c in range(8):
            ps, psr = P.bank()
            P.mm(ps[:, 0:W], psr, [(w[:, k, c * 128:(c + 1) * 128], rr[:, k, 0:W]) for k in range(8)], [wr, rrr])
            P.V(lambda e, c=c, ps=ps: e.scalar_tensor_tensor(out=xx[:, c, 0:W], in0=ps[:, 0:W], scalar=sc["g1"][:, c, j:j + 1],
                                                             in1=xx[:, c, 0:W], op0=ALU.mult, op1=ALU.add), [psr, sc["modr"], xxr], [xxr])
            if g1b is not None:
                P.S(lambda e, c=c: e.activation(out=xx[:, c, 0:W], in_=xx[:, c, 0:W], func=AF.Identity,
                                                bias=g1b[:, c, j:j + 1], scale=1.0), [xxr, g1br], [xxr])
        P.dma("sync", xout[:, :, col:col + W], xx[:, :, 0:W], reads=[xxr], writes=t["out_regions"])
    for t in tiles:
        tile_body(t)
    P.release()


def host_sel():
    s = np.zeros((8, 8 * 128), np.float32)
    for e in range(8):
        s[e, e * 128:(e + 1) * 128] = 1.0
    return s


def load_sel(E):
    P = E.P
    d = E.inp("selc", [8, 1024])
    t = P.alloc([1024], parts=8)
    r = P.R()
    P.dma("sync", t, d, writes=[r])
    return t, r


VC = dict(c=0, adab1=16, adab2=64, n2_1=112, n1_2=120, bout=128, NV=136)


def build_C():
    E = Env("C")
    P = E.P
    E.consts()
    x0 = E.inp("x0", [8, 128, NCA]).rearrange("c p w -> p c w")
    zT = E.inp("zT", [8, 128, NCA]).rearrange("c p w -> p c w")
    adaw = E.inp("ada_w", [2, 1024, 6144])
    wout = E.inp("w_out", [1024, 1024])
    wrt = E.inp("w_router", [1024, 8])
    wg = E.inp("w_gate", [NE, 1024, DFF])
    wu = E.inp("w_up", [NE, 1024, DFF])
    wd = E.inp("w_down", [NE, DFF, 1024])
    win = E.inp("w_in", [1024, 5120])
    vt, vr = load_small(E, "vecs", VC["NV"])
    selc, selr = load_sel(E)
    xm = E.scratch("xm", [8, 128, NCA]).rearrange("c p w -> p c w")
    x1 = E.out("x1", [8, 128, NCA]).rearrange("c p w -> p c w")
    uo = E.out("u2", [40, 128, NCA]).rearrange("c p w -> p c w")
    nreg = (NCA + 511) // 512
    xmr, x1r = P.R(nreg), P.R(nreg)
    modT = P.alloc([48, 2])
    modr = P.R()
    compute_mod(E, adaw[0], vt, vr, VC["c"], VC["adab1"], modT, modr)
    sc1 = layer_scalars(E, modT, modr, vt, vr, VC["n2_1"], VC["n2_1"])
    tiles = []
    for t in tile_list():
        t = dict(t)
        t["in_regions"] = []
        t["out_regions"] = col_regions(xmr, t["col"], t["W"])
        tiles.append(t)
    stage_post(E, tiles, x0, zT, sc1, wout, (vt, VC["bout"], vr), xm)
    groups = []
    for g in tile_list(step=1024):
        g = dict(g)
        g["in_regions"] = col_regions(xmr, g["col"], g["W"])
        g["out_regions"] = col_regions(x1r, g["col"], g["W"])
        g["is_out"] = True
        groups.append(g)
    stage_ffn(E, groups, xm, sc1, wg, wu, wd, x1, moe=dict(wr=wrt, selc=selc, selr=selr))
    modT2 = P.alloc([48, 2])
    modr2 = P.R()
    compute_mod(E, adaw[1], vt, vr, VC["c"], VC["adab2"], modT2, modr2)
    sc2 = layer_scalars(E, modT2, modr2, vt, vr, VC["n1_2"], VC["n1_2"])
    ptiles = []
    for t in tile_list():
        t = dict(t)
        t["in_regions"] = col_regions(x1r, t["col"], t["W"])
        ptiles.append(t)
    stage_pre(E, ptiles, x1, sc2, win, 40, None, uo)
    P.finish()
    P.emit()
    return E


def cvec(inp, b):
    cc = np.stack([fm(inp["c"][b]), fm(inp["c_ctx"])], axis=-1)
    return cc.reshape(128, 16)


def gather_tok(chan_lat, chan_ctx, k):
    b, seg = k // 4, k % 4
    out = np.empty((8, 128, NCA), np.float32)
    for kk in range(8):
        out[kk, :, 0:TPC] = chan_lat[kk][:, b, seg * TPC:(seg + 1) * TPC]
        out[kk, :, TPC:] = chan_ctx[kk][:, b, :]
    return out


def prep_C(inp, x0s, hz, hzc):
    cst = host_consts()
    sel = host_sel()
    maps = []
    for k in range(NCORE):
        b = k // 4
        v = np.zeros((128, VC["NV"]), np.float32)
        v[:, VC["c"]:VC["c"] + 16] = cvec(inp, b)
        v[:, VC["adab1"]:VC["adab1"] + 48] = fm(inp["ada_b"][1])
        v[:, VC["adab2"]:VC["adab2"] + 48] = fm(inp["ada_b"][2])
        v[:, VC["n2_1"]:VC["n2_1"] + 8] = fm(inp["norm2_g"][1])
        v[:, VC["n1_2"]:VC["n1_2"] + 8] = fm(inp["norm1_g"][2])
        v[:, VC["bout"]:VC["bout"] + 8] = fm(inp["hy_b_out"][0])
        maps.append({
            "consts": cst, "selc": sel, "x0": x0s[k], "zT": gather_tok(hz, hzc, k),
            "ada_w": np.ascontiguousarray(inp["ada_w"][1:3]),
            "w_out": np.asarray(inp["hy_w_out"][0]), "w_router": np.asarray(inp["moe_w_router"][0]),
            "w_gate": np.asarray(inp["moe_w_gate"][0]), "w_up": np.asarray(inp["moe_w_up"][0]),
            "w_down": np.asarray(inp["moe_w_down"][0]), "w_in": np.asarray(inp["hg_w_in"][0]),
            "vecs": v,
        })
    return maps


HG_T = CTX + SEQ
HG_NCH = HG_T // 64
VD = dict(lbl=0, gn=4, NV=8)


def build_D(nblk=32):
    E = Env("D")
    P = E.P
    E.consts()
    qT = E.inp("qT", [128, 2, HG_T])
    ogT = E.inp("ogT", [128, 2, HG_T])
    zfT = E.inp("zfT", [128, 2, HG_T])
    zbT = E.inp("zbT", [128, 2, HG_T])
    vTM = E.inp("vTM", [64, 2, HG_NCH, 128])
    tri_d = E.inp("tri", [64, 128])
    rT = E.out("rT", [128, 2, SEQ])
    osc = E.scratch("osc", [128, 2, SEQ])
    oscr = [P.R(32) for _ in range(2)]
    vt, vr = load_small(E, "vecs", VD["NV"])
    tri = P.alloc([128], parts=64)
    trir = P.R()
    P.dma("sync", tri, tri_d, writes=[trir])
    triF, triB = tri[:, 0:64], tri[:, 64:128]
    lbt = P.alloc([8])
    lbr = P.R()
    P.S(lambda e: e.activation(out=lbt[:, 0:4], in_=vt[:, VD["lbl"]:VD["lbl"] + 4], func=AF.Exp), [vr], [lbr])
    P.V(lambda e: e.reduce_sum(out=lbt[:, 4:5], in_=lbt[:, 0:4], axis=AX.X), [lbr], [lbr])
    P.V(lambda e: e.reciprocal(out=lbt[:, 4:5], in_=lbt[:, 4:5]), [lbr], [lbr])
    P.V(lambda e: e.tensor_tensor(out=lbt[:, 5:6], in0=lbt[:, 1:2], in1=lbt[:, 2:3], op=ALU.add), [lbr], [lbr])
    P.V(lambda e: e.tensor_tensor(out=lbt[:, 5:6], in0=lbt[:, 5:6], in1=lbt[:, 4:5], op=ALU.mult), [lbr], [lbr])
    P.V(lambda e: e.tensor_scalar(out=lbt[:, 6:7], in0=lbt[:, 5:6], scalar1=-1.0, scalar2=1.0, op0=ALU.mult, op1=ALU.add),
        [lbr], [lbr])
    lb, oml = lbt[:, 5:6], lbt[:, 6:7]

    def mkbuf():
        d = {}
        for nm in ("qt", "zt", "f", "g", "kk", "qs", "bsb", "eb", "enb", "og", "of", "os", "sq"):
            d[nm] = P.alloc([512])
            d[nm + "_r"] = P.R()
        for nm in ("qd", "ki", "ke"):
            d[nm] = P.alloc([512], BF16)
            d[nm + "_r"] = P.R()
        d["gTM"] = P.alloc([8, 128], parts=64)
        d["gTM_r"] = P.R()
        for nm in ("v", "keTM"):
            d[nm] = P.alloc([8, 128], BF16, parts=64)
            d[nm + "_r"] = P.R()
        d["att"] = P.alloc([64], BF16, parts=64)
        d["att_r"] = P.R()
        d["S"] = P.alloc([128])
        d["S_r"] = P.R()
        d["Sb"] = P.alloc([128], BF16)
        d["Sb_r"] = P.R()
        return d
    bufs = [mkbuf() for _ in range(2)]

    def block(b, fwd, blk):
        B = bufs[b]
        ctxb = blk < 0
        nch = 4 if ctxb else 8
        W = nch * 64
        col0 = 0 if ctxb else CTX + blk * 512
        ch0 = col0 // 64
        zsrc = zfT if fwd else zbT
        tr = triF if fwd else triB
        R = lambda nm: B[nm + "_r"]
        P.dma("sync", B["qt"][:, 0:W], qT[:, b, col0:col0 + W], writes=[R("qt")])
        P.dma("sync", B["zt"][:, 0:W], zsrc[:, b, col0:col0 + W], writes=[R("zt")])
        P.dma("gpsimd", B["v"][:, 0:nch, :], vTM[:, b, ch0:ch0 + nch, :], writes=[R("v")])
        P.S(lambda e: e.activation(out=B["f"][:, 0:W], in_=B["zt"][:, 0:W], func=AF.Sigmoid), [R("zt")], [R("f")])
        P.V(lambda e: e.tensor_scalar(out=B["f"][:, 0:W], in0=B["f"][:, 0:W], scalar1=oml, scalar2=lb, op0=ALU.mult, op1=ALU.add),
            [R("f"), lbr], [R("f")])
        P.S(lambda e: e.activation(out=B["g"][:, 0:W], in_=B["f"][:, 0:W], func=AF.Ln), [R("f")], [R("g")])
        P.V(lambda e: e.tensor_scalar(out=B["kk"][:, 0:W], in0=B["f"][:, 0:W], scalar1=-1.0, scalar2=1.0, op0=ALU.mult, op1=ALU.add),
            [R("f")], [R("kk")])
        P.S(lambda e: e.activation(out=B["qs"][:, 0:W], in_=B["qt"][:, 0:W], func=AF.Silu), [R("qt")], [R("qs")])
        pts = [(P.psum[0], P.psr[0]), (P.psum[1], P.psr[1])]
        for c in range(nch):
            pp, ppr = pts[c // 4]
            P.mm(pp[0:64, (c % 4) * 128:(c % 4 + 1) * 128], ppr, [(B["g"][:, c * 64:(c + 1) * 64], E.ident)], [R("g"), E.cr])
        for hf in range((nch + 3) // 4):
            pp, ppr = pts[hf]
            n4 = min(4, nch - hf * 4)
            P.V(lambda e, pp=pp, hf=hf, n4=n4: e.tensor_copy(
                out=B["gTM"][:, hf * 4:hf * 4 + n4, :].rearrange("p a b -> p (a b)"), in_=pp[0:64, 0:n4 * 128]), [ppr], [R("gTM")])
        pb, pbr = P.psum[2], P.psr[2]
        for c in range(nch):
            P.mm(pb[:, c * 64:(c + 1) * 64], pbr, [(B["gTM"][:, c, :], tr)], [R("gTM"), trir])
        P.V(lambda e: e.tensor_copy(out=B["bsb"][:, 0:W], in_=pb[:, 0:W]), [pbr], [R("bsb")])
        P.S(lambda e: e.activation(out=B["eb"][:, 0:W], in_=B["bsb"][:, 0:W], func=AF.Exp), [R("bsb")], [R("eb")])
        P.S(lambda e: e.activation(out=B["enb"][:, 0:W], in_=B["bsb"][:, 0:W], func=AF.Exp, scale=-1.0), [R("bsb")], [R("enb")])
        P.V(lambda e: e.tensor_tensor(out=B["qd"][:, 0:W], in0=B["qs"][:, 0:W], in1=B["eb"][:, 0:W], op=ALU.mult),
            [R("qs"), R("eb")], [R("qd")])
        P.V(lambda e: e.tensor_tensor(out=B["ki"][:, 0:W], in0=B["kk"][:, 0:W], in1=B["enb"][:, 0:W], op=ALU.mult),
            [R("kk"), R("enb")], [R("ki")])
        lastcol = lambda c: (c * 64 + 63) if fwd else (c * 64)
        for c in range(nch):
            P.V(lambda e, c=c: e.tensor_scalar(out=B["ke"][:, c * 64:(c + 1) * 64], in0=B["ki"][:, c * 64:(c + 1) * 64],
                                               scalar1=B["eb"][:, lastcol(c):lastcol(c) + 1], scalar2=None, op0=ALU.mult),
                [R("ki"), R("eb")], [R("ke")])
        pts2 = [(P.psum[0], P.psr[0]), (P.psum[1], P.psr[1])]
        for c in range(nch):
            pp, ppr = pts2[c // 4]
            P.mm(pp[0:64, (c % 4) * 128:(c % 4 + 1) * 128], ppr, [(B["ke"][:, c * 64:(c + 1) * 64], E.ident_b)], [R("ke"), E.cr])
        for hf in range((nch + 3) // 4):
            pp, ppr = pts2[hf]
            n4 = min(4, nch - hf * 4)
            P.S(lambda e, pp=pp, hf=hf, n4=n4: e.copy(
                out=B["keTM"][:, hf * 4:hf * 4 + n4, :].rearrange("p a b -> p (a b)"), in_=pp[0:64, 0:n4 * 128]), [ppr], [R("keTM")])
        po, por = (None, None) if ctxb else (P.psum[3 + b], P.psr[3 + b])
        order = range(nch) if fwd else range(nch - 1, -1, -1)
        yield
        for c in order:
            cs = slice(c * 64, (c + 1) * 64)
            if not ctxb:
                pa, par = P.psum[5], P.psr[5]
                P.mm(pa[0:64, 0:64], par, [(B["ki"][:, cs], B["qd"][:, cs])], [R("ki"), R("qd")])
                P.V(lambda e, pa=pa: e.tensor_tensor(out=B["att"], in0=pa[0:64, 0:64], in1=tr, op=ALU.mult), [par, trir], [R("att")])
                P.mm(po[:, cs], por, [(B["v"][:, c, :], B["att"]), (B["Sb"], B["qd"][:, cs])], [R("v"), R("att"), R("Sb"), R("qd")])
            pd, pdr = P.psum[6], P.psr[6]
            P.mm(pd[:, 0:128], pdr, [(B["keTM"][:, c, :], B["v"][:, c, :])], [R("keTM"), R("v")])
            P.V(lambda e, pd=pd, c=c: e.scalar_tensor_tensor(out=B["S"], in0=B["S"], scalar=B["eb"][:, lastcol(c):lastcol(c) + 1],
                                                             in1=pd[:, 0:128], op0=ALU.mult, op1=ALU.add), [R("S"), R("eb"), pdr], [R("S")])
            P.S(lambda e: e.copy(out=B["Sb"], in_=B["S"]), [R("S")], [R("Sb")])
            yield
        if ctxb:
            return
        t0 = blk * 512
        if fwd:
            P.S(lambda e: e.copy(out=B["os"], in_=po[:, :]), [por], [R("os")])
            P.dma("sync", osc[:, b, t0:t0 + 512], B["os"], reads=[R("os")], writes=[oscr[b][blk]])
            return
        P.dma("sync", B["of"], osc[:, b, t0:t0 + 512], reads=[oscr[b][blk]], writes=[R("of")])
        P.dma("sync", B["og"], ogT[:, b, CTX + t0:CTX + t0 + 512], writes=[R("og")])
        P.V(lambda e: e.tensor_tensor(out=B["os"], in0=po[:, :], in1=B["of"], op=ALU.add), [por, R("of")], [R("os")])
        P.V(lambda e: e.tensor_tensor(out=B["sq"], in0=B["os"], in1=B["os"], op=ALU.mult), [R("os")], [R("sq")])
        pn, pnr = P.psum[7], P.psr[7]
        P.mm(pn[:, :], pnr, [(E.ones, B["sq"])], [E.cr, R("sq")])
        P.V(lambda e: e.tensor_scalar(out=B["sq"], in0=pn[:, :], scalar1=1.0 / 128.0, scalar2=float(EPS), op0=ALU.mult, op1=ALU.add),
            [pnr], [R("sq")])
        P.S(lambda e: e.sqrt(out=B["sq"], in_=B["sq"]), [R("sq")], [R("sq")])
        P.V(lambda e: e.reciprocal(out=B["sq"], in_=B["sq"]), [R("sq")], [R("sq")])
        P.V(lambda e: e.scalar_tensor_tensor(out=B["os"], in0=B["os"], scalar=vt[:, VD["gn"]:VD["gn"] + 1], in1=B["sq"],
                                             op0=ALU.mult, op1=ALU.mult), [R("os"), vr, R("sq")], [R("os")])
        P.S(lambda e: e.activation(out=B["og"], in_=B["og"], func=AF.Silu), [R("og")], [R("og")])
        P.V(lambda e: e.tensor_tensor(out=B["os"], in0=B["os"], in1=B["og"], op=ALU.mult), [R("os"), R("og")], [R("os")])
        P.dma("sync", rT[:, b, t0:t0 + 512], B["os"], reads=[R("os")], writes=[], is_out=True)

    for fwd in (True, False):
        for b in range(2):
            P.V(lambda e, b=b: e.memset(bufs[b]["S"], 0.0), [], [bufs[b]["S_r"]])
            P.S(lambda e, b=b: e.copy(out=bufs[b]["Sb"], in_=bufs[b]["S"]), [bufs[b]["S_r"]], [bufs[b]["Sb_r"]])
        blks = [-1] + (list(range(nblk)) if fwd else list(range(nblk - 1, -1, -1)))
        for blk in blks:
            alive = [block(b, fwd, blk) for b in range(2)]
            while alive:
                for g_ in list(alive):
                    try:
                        next(g_)
                    except StopIteration:
                        alive.remove(g_)
    P.finish()
    P.emit()
    return E


def host_tri():
    s = np.arange(64)
    f = (s[:, None] <= s[None, :]).astype(np.float32)
    bk = (s[:, None] >= s[None, :]).astype(np.float32)
    return np.concatenate([f, bk], axis=1)


def prep_D(inp, u2s):
    cst = host_consts()
    tri = host_tri()
    maps = []
    lbl = np.asarray(inp["hg_lb_logits"], np.float32)
    for k in range(NCORE):
        def gat(s):
            out = np.empty((128, 2, HG_T), np.float32)
            for b in range(2):
                out[:, b, 0:CTX] = u2s[4 * b][s * 8 + k][:, TPC:TPC + CTX]
                for seg in range(4):
                    out[:, b, CTX + seg * TPC:CTX + (seg + 1) * TPC] = u2s[4 * b + seg][s * 8 + k][:, 0:TPC]
            return out
        iT = gat(1)
        vTM = np.ascontiguousarray(iT.transpose(1, 2, 0).reshape(2, HG_NCH, 64, 128).transpose(2, 0, 1, 3))
        v = np.zeros((128, VD["NV"]), np.float32)
        v[:, VD["lbl"]:VD["lbl"] + 4] = lbl[:, k * 128:(k + 1) * 128].T
        v[:, VD["gn"]] = inp["hg_gn_g"][0][k * 128:(k + 1) * 128]
        maps.append({"consts": cst, "tri": tri, "qT": gat(0), "ogT": gat(2), "zfT": gat(3), "zbT": gat(4), "vTM": vTM, "vecs": v})
    return maps


VE = dict(c=0, adab2=16, n2_2=64, NV=72)


def build_E():
    E = Env("E")
    P = E.P
    E.consts()
    x1 = E.inp("x1", [8, 128, NCA]).rearrange("c p w -> p c w")
    rT = E.inp("rT", [8, 128, TPC]).rearrange("c p w -> p c w")
    adaw = E.inp("ada_w", [1024, 6144])
    wout = E.inp("w_out", [1024, 1024])
    wg = E.inp("w_gate", [1024, DFF])
    wu = E.inp("w_up", [1024, DFF])
    wd = E.inp("w_down", [DFF, 1024])
    vt, vr = load_small(E, "vecs", VE["NV"])
    xm = E.scratch("xm", [8, 128, TPC]).rearrange("c p w -> p c w")
    x2 = E.out("x2", [8, 128, TPC]).rearrange("c p w -> p c w")
    nreg = TPC // 512
    xmr, x2r = P.R(nreg), P.R(nreg)
    modT = P.alloc([48, 2])
    modr = P.R()
    compute_mod(E, adaw, vt, vr, VE["c"], VE["adab2"], modT, modr)
    sc = layer_scalars(E, modT, modr, vt, vr, VE["n2_2"], VE["n2_2"])
    tiles = []
    for t in tile_list(with_ctx=False):
        t = dict(t)
        t["in_regions"] = []
        t["out_regions"] = col_regions(xmr, t["col"], t["W"])
        tiles.append(t)
    stage_post(E, tiles, x1, rT, sc, wout, None, xm)
    groups = []
    for g in tile_list(with_ctx=False, step=1024):
        g = dict(g)
        g["in_regions"] = col_regions(xmr, g["col"], g["W"])
        g["out_regions"] = col_regions(x2r, g["col"], g["W"])
        g["is_out"] = True
        groups.append(g)
    stage_ffn(E, groups, xm, sc, wg, wu, wd, x2)
    P.finish()
    P.emit()
    return E


def prep_E(inp, x1s, rTs):
    cst = host_consts()
    maps = []
    for k in range(NCORE):
        b, seg = k // 4, k % 4
        v = np.zeros((128, VE["NV"]), np.float32)
        v[:, VE["c"]:VE["c"] + 16] = cvec(inp, b)
        v[:, VE["adab2"]:VE["adab2"] + 48] = fm(inp["ada_b"][2])
        v[:, VE["n2_2"]:VE["n2_2"] + 8] = fm(inp["norm2_g"][2])
        r = np.stack([rTs[kk][:, b, seg * TPC:(seg + 1) * TPC] for kk in range(8)], axis=0)
        maps.append({"consts": cst, "x1": x1s[k], "rT": np.ascontiguousarray(r), "ada_w": np.asarray(inp["ada_w"][2]),
                     "w_out": np.asarray(inp["hg_w_out"][0]), "w_gate": np.asarray(inp["ffn_w_gate"][1]),
                     "w_up": np.asarray(inp["ffn_w_up"][1]), "w_down": np.asarray(inp["ffn_w_down"][1]), "vecs": v})
    return maps


VF = dict(c=0, adab3=16, n1_3=64, n2_3=72, bpw1=80, bdw=96, lng=104, lnb=112, bpw2=120, wdw=128, edge=376, nf=378, NV=386)


def stage_final(E, tiles, xin, vt, vr, col_nf, yout):
    P = E.P
    P.mark()
    rc = rms_ctx(E, 512)
    gs = P.alloc([8, 2])
    sh = P.alloc([8, 2])
    gr = P.R()
    P.V(lambda e: e.memset(sh, 0.0), [], [gr])
    P.V(lambda e: e.memset(gs, 0.0), [], [gr])
    P.V(lambda e: e.tensor_scalar(out=gs[:, :, 0], in0=vt[:, col_nf:col_nf + 8], scalar1=float(math.sqrt(D)), scalar2=None,
                                  op0=ALU.mult), [vr, gr], [gr])
    x = P.alloc([8, 512])
    xr = P.R()
    hb = P.alloc([8, 512], BF16)
    hbr = P.R()
    y = P.alloc([8, 512])
    yr = P.R()

    def tile_body(t):
        W, col = t["W"], t["col"]
        P.dma("sync", x[:, :, 0:W], xin[:, :, col:col + W], reads=t["in_regions"], writes=[xr])
        rms_mod(E, rc, x, xr, W, gs, sh, 0, gr, hb, hbr, out_f32=y, out_f32_r=yr)
        P.dma("sync", yout[:, :, col:col + W], y[:, :, 0:W], reads=[yr], writes=[], is_out=True)
    for t in tiles:
        tile_body(t)
    P.release()


def build_F():
    E = Env("F")
    P = E.P
    E.consts()
    xe_d = E.inp("xe", [8, 128, TPC + 32]).rearrange("c p w -> p c w")
    adaw = E.inp("ada_w", [1024, 6144])
    wpw1 = E.inp("w_pw1", [1024, 2048])
    wpw2 = E.inp("w_pw2", [1024, 1024])
    wrt = E.inp("w_router", [1024, 8])
    wg = E.inp("w_gate", [NE, 1024, DFF])
    wu = E.inp("w_up", [NE, 1024, DFF])
    wd = E.inp("w_down", [NE, DFF, 1024])
    vt, vr = load_small(E, "vecs", VF["NV"])
    E.edger = vr
    selc, selr = load_sel(E)
    xm = E.scratch("xm", [8, 128, TPC]).rearrange("c p w -> p c w")
    x3 = E.scratch("x3", [8, 128, TPC]).rearrange("c p w -> p c w")
    yo = E.out("y", [8, 128, TPC]).rearrange("c p w -> p c w")
    nreg = TPC // 512
    xmr, x3r = P.R(nreg), P.R(nreg)
    modT = P.alloc([48, 2])
    modr = P.R()
    compute_mod(E, adaw, vt, vr, VF["c"], VF["adab3"], modT, modr)
    sc = layer_scalars(E, modT, modr, vt, vr, VF["n1_3"], VF["n2_3"])
    tiles = []
    for t in tile_list(with_ctx=False):
        t = dict(t)
        c0 = t["col"] + 1
        t.update(src=xe_d[:, :, c0:c0 + 542], pos=None, in_regions=[], ocol=t["col"])
        if t["col"] == 0:
            t["ledge"] = vt[:, VF["edge"]:VF["edge"] + 1]
        if t["col"] == TPC - 512:
            t["redge"] = vt[:, VF["edge"] + 1:VF["edge"] + 2]
        t["out_regions"] = col_regions(xmr, t["col"], t["W"])
        tiles.append(t)
    stage_conformer(E, tiles, wpw1, wpw2, vt, vr,
                    (VF["bpw1"], VF["bdw"], VF["lng"], VF["lnb"], VF["bpw2"], VF["wdw"]), sc, xm, None)
    groups = []
    for g in tile_list(with_ctx=False, step=1024):
        g = dict(g)
        g["in_regions"] = col_regions(xmr, g["col"], g["W"])
        g["out_regions"] = col_regions(x3r, g["col"], g["W"])
        groups.append(g)
    stage_ffn(E, groups, xm, sc, wg, wu, wd, x3, moe=dict(wr=wrt, selc=selc, selr=selr))
    ftiles = []
    for t in tile_list(with_ctx=False):
        t = dict(t)
        t["in_regions"] = col_regions(x3r, t["col"], t["W"])
        ftiles.append(t)
    stage_final(E, ftiles, x3, vt, vr, VF["nf"], yo)
    P.finish()
    P.emit()
    return E


def prep_F(inp, x2s):
    cst = host_consts()
    sel = host_sel()
    maps = []
    full = None if x2s is None else [np.concatenate([unchunk_T(x2s[4 * b + seg]) for seg in range(4)], axis=0) for b in range(BATCH)]
    for k in range(NCORE):
        b, seg = k // 4, k % 4
        v = np.zeros((128, VF["NV"]), np.float32)
        v[:, VF["c"]:VF["c"] + 16] = cvec(inp, b)
        v[:, VF["adab3"]:VF["adab3"] + 48] = fm(inp["ada_b"][3])
        v[:, VF["n1_3"]:VF["n1_3"] + 8] = fm(inp["norm1_g"][3])
        v[:, VF["n2_3"]:VF["n2_3"] + 8] = fm(inp["norm2_g"][3])
        v[:, VF["bpw1"]:VF["bpw1"] + 16] = fm(inp["cf_b_pw1"][1])
        v[:, VF["bdw"]:VF["bdw"] + 8] = fm(inp["cf_b_dw"][1])
        v[:, VF["lng"]:VF["lng"] + 8] = fm(inp["cf_ln_g"][1])
        v[:, VF["lnb"]:VF["lnb"] + 8] = fm(inp["cf_ln_b"][1])
        v[:, VF["bpw2"]:VF["bpw2"] + 8] = fm(inp["cf_b_pw2"][1])
        wdw = np.asarray(inp["cf_w_dw"][1], np.float32)
        v[:, VF["wdw"]:VF["wdw"] + 248] = wdw.T.reshape(8, 128, 31).transpose(1, 0, 2).reshape(128, 248)
        v[:, VF["edge"]] = 0.0 if seg == 0 else 1.0
        v[:, VF["edge"] + 1] = 0.0 if seg == 3 else 1.0
        v[:, VF["nf"]:VF["nf"] + 8] = fm(inp["normf_g"])
        maps.append({"consts": cst, "selc": sel, "xe": None if full is None else window_T(full[b], seg * TPC, -16, TPC + 16),
                     "ada_w": np.asarray(inp["ada_w"][3]), "w_pw1": np.asarray(inp["cf_w_pw1"][1]),
                     "w_pw2": np.asarray(inp["cf_w_pw2"][1]), "w_router": np.asarray(inp["moe_w_router"][1]),
                     "w_gate": np.asarray(inp["moe_w_gate"][1]), "w_up": np.asarray(inp["moe_w_up"][1]),
                     "w_down": np.asarray(inp["moe_w_down"][1]), "vecs": v})
    return maps


NCE = TPC + 32
VG = dict(VF)
VG.update(c=0, adab2=402, n2_2=450, NV=458)


def build_EF():
    E = Env("EF")
    P = E.P
    E.consts()
    x1 = E.inp("x1", [8, 128, NCE]).rearrange("c p w -> p c w")
    rT = E.inp("rT", [8, 128, NCE]).rearrange("c p w -> p c w")
    adaw2 = E.inp("ada_w2", [1024, 6144])
    wout = E.inp("w_out", [1024, 1024])
    wg2 = E.inp("w_gate2", [1024, DFF])
    wu2 = E.inp("w_up2", [1024, DFF])
    wd2 = E.inp("w_down2", [DFF, 1024])
    adaw = E.inp("ada_w", [1024, 6144])
    wpw1 = E.inp("w_pw1", [1024, 2048])
    wpw2 = E.inp("w_pw2", [1024, 1024])
    wrt = E.inp("w_router", [1024, 8])
    wg = E.inp("w_gate", [NE, 1024, DFF])
    wu = E.inp("w_up", [NE, 1024, DFF])
    wd = E.inp("w_down", [NE, DFF, 1024])
    vt, vr = load_small(E, "vecs", VG["NV"])
    E.edger = vr
    selc, selr = load_sel(E)
    xm2 = E.scratch("xm2", [8, 128, NCE]).rearrange("c p w -> p c w")
    x2 = E.scratch("x2", [8, 128, NCE]).rearrange("c p w -> p c w")
    xm = E.scratch("xm", [8, 128, TPC]).rearrange("c p w -> p c w")
    x3 = E.scratch("x3", [8, 128, TPC]).rearrange("c p w -> p c w")
    yo = E.out("y", [8, 128, TPC]).rearrange("c p w -> p c w")
    nreg = (NCE + 511) // 512
    xm2r, x2r, xmr, x3r = P.R(nreg), P.R(nreg), P.R(nreg), P.R(nreg)
    modT2 = P.alloc([48, 2])
    modr2 = P.R()
    compute_mod(E, adaw2, vt, vr, VG["c"], VG["adab2"], modT2, modr2)
    sc2 = layer_scalars(E, modT2, modr2, vt, vr, VG["n2_2"], VG["n2_2"])
    tl2 = tile_list(with_ctx=False) + [dict(col=TPC, W=32, j=0)]
    tiles = []
    for t in tl2:
        t = dict(t)
        t["in_regions"] = []
        t["out_regions"] = col_regions(xm2r, t["col"], t["W"])
        tiles.append(t)
    stage_post(E, tiles, x1, rT, sc2, wout, None, xm2)
    groups = []
    for g in tile_list(with_ctx=False, step=1024) + [dict(col=TPC, W=32, j=0)]:
        g = dict(g)
        g["in_regions"] = col_regions(xm2r, g["col"], g["W"])
        g["out_regions"] = col_regions(x2r, g["col"], g["W"])
        groups.append(g)
    stage_ffn(E, groups, xm2, sc2, wg2, wu2, wd2, x2)
    modT = P.alloc([48, 2])
    modr = P.R()
    compute_mod(E, adaw, vt, vr, VG["c"], VG["adab3"], modT, modr)
    sc = layer_scalars(E, modT, modr, vt, vr, VG["n1_3"], VG["n2_3"])
    tiles = []
    for t in tile_list(with_ctx=False):
        t = dict(t)
        c = t["col"]
        if c == 0:
            src = [(0, 15, x2[:, :, TPC + 1:TPC + 16]), (15, 527, x2[:, :, 0:527])]
            t["ledge"] = vt[:, VG["edge"]:VG["edge"] + 1]
        elif c == TPC - 512:
            src = [(0, 527, x2[:, :, c - 15:TPC]), (527, 15, x2[:, :, TPC + 16:TPC + 31])]
            t["redge"] = vt[:, VG["edge"] + 1:VG["edge"] + 2]
        else:
            src = x2[:, :, c - 15:c + 527]
        t.update(src=src, pos=None, in_regions=list(x2r), ocol=c)
        t["out_regions"] = col_regions(xmr, c, t["W"])
        tiles.append(t)
    stage_conformer(E, tiles, wpw1, wpw2, vt, vr,
                    (VG["bpw1"], VG["bdw"], VG["lng"], VG["lnb"], VG["bpw2"], VG["wdw"]), sc, xm, None)
    groups = []
    for g in tile_list(with_ctx=False, step=1024):
        g = dict(g)
        g["in_regions"] = col_regions(xmr, g["col"], g["W"])
        g["out_regions"] = col_regions(x3r, g["col"], g["W"])
        groups.append(g)
    stage_ffn(E, groups, xm, sc, wg, wu, wd, x3, moe=dict(wr=wrt, selc=selc, selr=selr))
    ftiles = []
    for t in tile_list(with_ctx=False):
        t = dict(t)
        t["in_regions"] = col_regions(x3r, t["col"], t["W"])
        ftiles.append(t)
    stage_final(E, ftiles, x3, vt, vr, VG["nf"], yo)
    P.finish()
    P.emit()
    return E


def prep_EF(inp, x1s, rTs):
    mF = prep_F(inp, None)
    maps = []
    x1full = [np.concatenate([unchunk_T(x1s[4 * b + seg])[0:TPC] for seg in range(4)], axis=0) for b in range(BATCH)]
    for k in range(NCORE):
        b, seg = k // 4, k % 4
        t0 = seg * TPC
        v = np.zeros((128, VG["NV"]), np.float32)
        v[:, 0:VF["NV"]] = mF[k]["vecs"]
        v[:, VG["adab2"]:VG["adab2"] + 48] = fm(inp["ada_b"][2])
        v[:, VG["n2_2"]:VG["n2_2"] + 8] = fm(inp["norm2_g"][2])
        x1w = window_T(x1full[b], t0, -16, TPC + 16)
        x1e = np.concatenate([x1w[:, :, 16:16 + TPC], x1w[:, :, 0:16], x1w[:, :, 16 + TPC:]], axis=2)
        rw = np.zeros((8, 128, TPC + 32), np.float32)
        lo, hi = max(0, t0 - 16), min(SEQ, t0 + TPC + 16)
        for kk in range(8):
            rw[kk][:, lo - (t0 - 16):hi - (t0 - 16)] = rTs[kk][:, b, lo:hi]
        re_ = np.concatenate([rw[:, :, 16:16 + TPC], rw[:, :, 0:16], rw[:, :, 16 + TPC:]], axis=2)
        m = dict(mF[k])
        m.pop("xe")
        m.update({"x1": np.ascontiguousarray(x1e), "rT": np.ascontiguousarray(re_), "ada_w2": np.asarray(inp["ada_w"][2]),
                  "w_out": np.asarray(inp["hg_w_out"][0]), "w_gate2": np.asarray(inp["ffn_w_gate"][1]),
                  "w_up2": np.asarray(inp["ffn_w_up"][1]), "w_down2": np.asarray(inp["ffn_w_down"][1]), "vecs": v})
        maps.append(m)
    return maps


def _run(E, maps):
    res = run_bass_kernel_spmd(E.nc, maps, core_ids=list(range(NCORE)))
    return res.results


def kernel(**inp):
    inp = {k: np.asarray(v) for k, v in inp.items()}
    ra = _run(build_A(), prep_A(inp))
    x0s = [r["x0"] for r in ra]
    rb = _run(build_B(), prep_B(inp, [r["u_pre"] for r in ra]))
    del ra
    rc_ = _run(build_C(), prep_C(inp, x0s, [r["hz"] for r in rb], [r["hzc"] for r in rb]))
    del rb, x0s
    x1s = [r["x1"] for r in rc_]
    rd = _run(build_D(), prep_D(inp, [r["u2"] for r in rc_]))
    del rc_
    rf = _run(build_EF(), prep_EF(inp, x1s, [r["rT"] for r in rd]))
    del rd, x1s
    out = np.empty((BATCH, SEQ, D), np.float32)
    for k in range(NCORE):
        b, seg = k // 4, k % 4
        out[b, seg * TPC:(seg + 1) * TPC] = unchunk_T(rf[k]["y"])
    return out
```

```python
import math
import contextlib
import numpy as np
import concourse.bass as bass
import concourse.mybir as mybir
from concourse.bass_utils import run_bass_kernel_spmd

F32 = mybir.dt.float32
BF16 = mybir.dt.bfloat16
ALU = mybir.AluOpType
AF = mybir.ActivationFunctionType
AX = mybir.AxisListType

D = 1024
KC = 8
DFF = 3584
FC = 28
SEQ = 16384
BATCH = 2
CTX = 256
NCORE = 8
TPC = 4096
EPS = 1e-6
GRID_W = 64
NE = 8

ENGS = ("sync", "scalar", "vector", "gpsimd", "tensor")
DMA_SLOTS = {"sync": 24, "scalar": 8, "gpsimd": 24}


class Op:
    __slots__ = ("eng", "fn", "waits", "signal", "semkey", "semval", "dma")

    def __init__(self, eng, fn, dma):
        self.eng = eng
        self.fn = fn
        self.waits = []
        self.signal = False
        self.semkey = None
        self.semval = 0
        self.dma = dma


class Region:
    __slots__ = ("w", "rs")

    def __init__(self):
        self.w = None
        self.rs = []


class Prog:
    def __init__(self, nc):
        self.nc = nc
        self.ops = {e: [] for e in ENGS}
        self.stack = contextlib.ExitStack()
        self.dma_slot = {e: [None] * n for e, n in DMA_SLOTS.items()}
        self.dma_rr = {e: 0 for e in DMA_SLOTS}
        self.out_dmas = []
        self.big = self.stack.enter_context(nc.sbuf_tensor("big", [128, SB_WORDS], F32))
        self.off = 0
        self.marks = []
        self.psum = [self.stack.enter_context(nc.psum_tensor(f"ps{i}", [128, 512], F32)) for i in range(8)]
        self.psr = [Region() for _ in range(8)]
        self.ps_i = 0

    def alloc(self, free_shape, dt=F32, parts=128):
        n = int(np.prod(free_shape))
        words = n if dt == F32 else (n + 1) // 2
        words = (words + 7) // 8 * 8
        assert self.off + words <= SB_WORDS, f"SBUF overflow {self.off}+{words}"
        ap = self.big[:, self.off:self.off + words]
        self.off += words
        if dt != F32:
            ap = ap.bitcast(dt)
        ap = ap[:, 0:n]
        if len(free_shape) == 2:
            ap = ap.rearrange("p (a b) -> p a b", a=free_shape[0])
        elif len(free_shape) == 3:
            ap = ap.rearrange("p (a b c) -> p a b c", a=free_shape[0], b=free_shape[1])
        if parts != 128:
            ap = ap[0:parts]
        return ap

    def mark(self):
        self.marks.append(self.off)

    def release(self):
        self.barrier()
        self.off = self.marks.pop()

    def bank(self):
        i = self.ps_i
        self.ps_i = (i + 1) % 8
        return self.psum[i], self.psr[i]

    def R(self, n=None):
        if n is None:
            return Region()
        return [Region() for _ in range(n)]

    def op(self, eng, fn, reads=(), writes=(), dma=False, is_out=False):
        o = Op(eng, fn, dma)
        deps = []
        seen = set()

        def add(d):
            if d is None or id(d) in seen:
                return
            seen.add(id(d))
            deps.append(d)

        for r in reads:
            add(r.w)
        for r in writes:
            add(r.w)
            for x in r.rs:
                add(x)
        if dma:
            slots = self.dma_slot[eng]
            i = self.dma_rr[eng]
            self.dma_rr[eng] = (i + 1) % len(slots)
            add(slots[i])
            o.semkey = (eng, i)
            slots[i] = o
            o.signal = True
        else:
            o.semkey = eng
        for d in deps:
            if d.eng == "tensor" and eng == "tensor" and not d.dma and not dma:
                continue
            if SAME_ENGINE_FREE and d.eng == eng and not d.dma and not dma:
                continue
            d.signal = True
            o.waits.append(d)
        for r in reads:
            if not dma:
                r.rs = [x for x in r.rs if x.dma or x.eng != eng]
            r.rs.append(o)
        for r in writes:
            r.w = o
            r.rs = []
        self.ops[eng].append(o)
        if is_out:
            self.out_dmas.append(o)
        return o

    def dma(self, eng, out, in_, reads=(), writes=(), is_out=False, **kw):
        return self.op(eng, lambda e: e.dma_start(out=out, in_=in_, **kw), reads, writes, dma=True, is_out=is_out)

    def V(self, fn, reads=(), writes=()):
        return self.op("vector", fn, reads, writes)

    def S(self, fn, reads=(), writes=()):
        return self.op("scalar", fn, reads, writes)

    def G(self, fn, reads=(), writes=()):
        return self.op("gpsimd", fn, reads, writes)

    def T(self, fn, reads=(), writes=()):
        return self.op("tensor", fn, reads, writes)

    def mm(self, out, outr, pairs, reads):
        def fn(e):
            n = len(pairs)
            ins = None
            for i, (l, r) in enumerate(pairs):
                ins = e.matmul(out, lhsT=l, rhs=r, start=(i == 0), stop=(i == n - 1))
            return ins
        return self.op("tensor", fn, reads, [outr])

    def barrier(self):
        lasts = []
        for e in ENGS:
            for o in reversed(self.ops[e]):
                if not o.dma and o.fn is not None:
                    lasts.append(o)
                    break
        for e in DMA_SLOTS:
            for o in self.dma_slot[e]:
                if o is not None:
                    lasts.append(o)
        for e in ENGS:
            o = Op(e, None, False)
            o.semkey = e
            for d in lasts:
                if d.eng == e and not d.dma:
                    continue
                d.signal = True
                o.waits.append(d)
            self.ops[e].append(o)

    def finish(self):
        o = Op("sync", None, False)
        o.semkey = "sync"
        o.waits = list(self.out_dmas)
        self.ops["sync"].append(o)

    def emit(self):
        nc = self.nc
        sems = {}
        self.maxcnt = {}
        for e in ENGS:
            cnt = 0
            slotcnt = {}
            for o in self.ops[e]:
                if o.dma:
                    slotcnt[o.semkey] = slotcnt.get(o.semkey, 0) + 16
                    o.semval = slotcnt[o.semkey]
                    sems.setdefault(o.semkey, None)
                elif o.signal:
                    cnt += 1
                    o.semval = cnt
            sems[e] = None
            self.maxcnt[e] = (cnt, len(self.ops[e]))
        for k in list(sems):
            nm = k if isinstance(k, str) else f"d_{k[0]}_{k[1]}"
            sems[k] = self.stack.enter_context(nc.semaphore("s_" + nm))

        for k, h in sems.items():
            nc.sync.sem_clear(h)
        nc.all_engine_barrier()

        def run(e, eng):
            waited = {}
            for o in self.ops[e]:
                for d in o.waits:
                    if waited.get(d.semkey, 0) >= d.semval:
                        continue
                    eng.wait_ge(sems[d.semkey], d.semval)
                    waited[d.semkey] = d.semval
                if o.fn is None:
                    continue
                ins = o.fn(eng)
                if o.dma:
                    ins.then_inc(sems[o.semkey], 16)
                elif o.signal:
                    ins.then_inc(sems[o.semkey], 1)

        with nc.Block() as block:
            @block.sync
            def _(eng):
                run("sync", eng)

            @block.scalar
            def _(eng):
                run("scalar", eng)

            @block.vector
            def _(eng):
                run("vector", eng)

            @block.gpsimd
            def _(eng):
                run("gpsimd", eng)

            @block.tensor
            def _(eng):
                run("tensor", eng)
        self.stack.close()


SAME_ENGINE_FREE = False
SB_WORDS = 49152


class Env:
    def __init__(self, name):
        self.nc = bass.Bass("TRN2", target_bir_lowering=False)
        self.P = Prog(self.nc)
        self.ins = {}
        self.outs = {}

    def inp(self, name, shape, dt=F32):
        t = self.nc.dram_tensor(name, list(shape), dt, kind="ExternalInput").ap()
        self.ins[name] = t
        return t

    def out(self, name, shape, dt=F32):
        t = self.nc.dram_tensor(name, list(shape), dt, kind="ExternalOutput").ap()
        self.outs[name] = t
        return t

    def scratch(self, name, shape, dt=F32):
        return self.nc.dram_tensor(name, list(shape), dt).ap()

    def consts(self):
        P = self.P
        cin = self.inp("consts", [128, 384])
        self.cf = P.alloc([384])
        self.cr = P.R()
        P.dma("sync", self.cf, cin, writes=[self.cr])
        self.ident = self.cf[:, 0:128]
        self.ones = self.cf[:, 128:256]
        self.J = self.cf[:, 256:384]
        cb = P.alloc([256], BF16)
        P.V(lambda e: e.tensor_copy(out=cb, in_=self.cf[:, 0:256]), [self.cr], [self.cr])
        self.ident_b = cb[:, 0:128]
        self.ones_b = cb[:, 128:256]


def host_consts():
    return np.concatenate([np.eye(128, dtype=np.float32), np.ones((128, 128), np.float32),
                           np.eye(128, dtype=np.float32)[::-1]], axis=1)


def fm(v):
    v = np.asarray(v, np.float32)
    return np.ascontiguousarray(v.reshape(-1, 128).T)


def load_small(E, name, ncols):
    P = E.P
    d = E.inp(name, [128, ncols])
    t = P.alloc([ncols])
    r = P.R()
    P.dma("sync", t, d, writes=[r])
    return t, r


def load_w_bf16(E, dst, dst_r, src_ap, reads=()):
    return E.P.dma("gpsimd", dst, src_ap, reads=reads, writes=[dst_r])


def compute_mod(E, adaw, vt, vr, col_c, col_adab, modT, modr):
    P = E.P
    P.mark()
    sc = P.alloc([16])
    scr = P.R()
    P.S(lambda e: e.activation(out=sc, in_=vt[:, col_c:col_c + 16], func=AF.Silu), [vr], [scr])
    sc3 = sc.rearrange("p (k j) -> p k j", j=2)
    wb = [P.alloc([8, 384]) for _ in range(2)]
    wr = P.R(2)
    ps, psr = P.bank()
    adv = adaw.rearrange("(k p) n -> p k n", p=128)
    for mb in range(16):
        i = mb % 2
        P.dma("sync", wb[i], adv[:, :, mb * 384:(mb + 1) * 384], writes=[wr[i]])
        for j in range(3):
            m = mb * 3 + j
            P.mm(ps[:, m * 2:m * 2 + 2], psr,
                 [(wb[i][:, k, j * 128:(j + 1) * 128], sc3[:, k, :]) for k in range(8)], [wr[i], scr])
    ps3 = ps[:, 0:96].rearrange("p (m j) -> p m j", j=2)
    for j in range(2):
        P.V(lambda e, j=j: e.tensor_tensor(out=modT[:, :, j], in0=ps3[:, :, j], in1=vt[:, col_adab:col_adab + 48],
                                           op=ALU.add), [psr, vr], [modr])
    P.release()


def rms_ctx(E, Wmax):
    P = E.P
    return dict(sq=P.alloc([8, Wmax], BF16), sqr=P.R(), rstd=P.alloc([Wmax]), rr=P.R(),
                tmp=[P.alloc([Wmax]) for _ in range(2)], tr=P.R(2))


def rms_mod(E, rc, x, xr, W, gsc, sh, jcol, scalr, out, outr, out_f32=None, out_f32_r=None):
    P = E.P
    sq, sqr, rr, tr = rc["sq"], rc["sqr"], rc["rr"], rc["tr"]
    rstd = rc["rstd"][:, 0:W]
    tmp = [t[:, 0:W] for t in rc["tmp"]]
    for c in range(8):
        P.S(lambda e, c=c: e.activation(out=sq[:, c, 0:W], in_=x[:, c, 0:W], func=AF.Square), [xr], [sqr])
    for a in range(0, W, 512):
        b = min(a + 512, W)
        ps, psr = P.bank()
        P.mm(ps[:, 0:b - a], psr, [(E.ones_b, sq[:, c, a:b]) for c in range(8)], [sqr, E.cr])
        P.V(lambda e, a=a, b=b, ps=ps: e.tensor_scalar(out=rstd[:, a:b], in0=ps[:, 0:b - a], scalar1=float(D * EPS),
                                                       scalar2=None, op0=ALU.add), [psr], [rr])
        P.S(lambda e, a=a, b=b: e.sqrt(out=rstd[:, a:b], in_=rstd[:, a:b]), [rr], [rr])
        P.V(lambda e, a=a, b=b: e.reciprocal(out=rstd[:, a:b], in_=rstd[:, a:b]), [rr], [rr])
    for c in range(8):
        i = c % 2
        P.V(lambda e, c=c, i=i: e.tensor_tensor(out=tmp[i], in0=x[:, c, 0:W], in1=rstd, op=ALU.mult),
            [xr, rr], [tr[i]])
        P.S(lambda e, c=c, i=i: e.activation(out=out[:, c, 0:W], in_=tmp[i], func=AF.Identity,
                                             bias=sh[:, c, jcol:jcol + 1], scale=gsc[:, c, jcol:jcol + 1]),
            [tr[i], scalr], [outr])
        if out_f32 is not None:
            P.V(lambda e, c=c, i=i: e.tensor_scalar(out=out_f32[:, c, 0:W], in0=tmp[i],
                                                    scalar1=gsc[:, c, jcol:jcol + 1], scalar2=sh[:, c, jcol:jcol + 1],
                                                    op0=ALU.mult, op1=ALU.add),
                [tr[i], scalr], [out_f32_r])


def layer_scalars(E, modT, modr, vt, vr, col_n1, col_n2):
    P = E.P
    sc = {}
    r = P.R()
    m4 = modT.rearrange("p (s c) j -> p s c j", s=6)
    for nm, si, col in (("gsc1", 1, col_n1), ("gsc2", 4, col_n2)):
        t = P.alloc([8, 2])
        for j in range(2):
            P.V(lambda e, t=t, j=j, si=si, col=col: e.scalar_tensor_tensor(
                out=t[:, :, j], in0=m4[:, si, :, j], scalar=1.0, in1=vt[:, col:col + 8], op0=ALU.add, op1=ALU.mult),
                [modr, vr], [r])
        P.V(lambda e, t=t: e.tensor_scalar(out=t, in0=t, scalar1=float(math.sqrt(D)), scalar2=None, op0=ALU.mult),
            [r], [r])
        sc[nm] = t
    sc["sh1"] = m4[:, 0]
    sc["g1"] = m4[:, 2]
    sc["sh2"] = m4[:, 3]
    sc["g2"] = m4[:, 5]
    sc["r"] = r
    sc["modr"] = modr
    return sc


def stage_conformer(E, tiles, w_pw1, w_pw2, vt, vr, cols, sc, xout, xout_r):
    P = E.P
    P.mark()
    c_bpw1, c_bdw, c_lng, c_lnb, c_bpw2, c_wdw = cols
    w1 = P.alloc([8, 2048], BF16)
    w2 = P.alloc([8, 1024], BF16)
    w1r, w2r = P.R(), P.R()
    w1v = w_pw1.rearrange("(k p) n -> p k n", p=128)
    for k in range(8):
        P.dma("gpsimd", w1[:, k, :], w1v[:, k, :], writes=[w1r])
    P.dma("gpsimd", w2, w_pw2.rearrange("(k p) n -> p k n", p=128), writes=[w2r])
    dgb = [P.alloc([31, 128], BF16) for _ in range(2)]
    dgrs = P.R(2)
    g1b = P.alloc([8, 2])
    g1br = P.R()
    for j in range(2):
        P.V(lambda e, j=j: e.tensor_tensor(out=g1b[:, :, j], in0=sc["g1"][:, :, j], in1=vt[:, c_bpw2:c_bpw2 + 8],
                                           op=ALU.mult), [sc["modr"], vr], [g1br])
    rc = rms_ctx(E, 542)
    xe = P.alloc([8, 542])
    xer = P.R()
    vv = P.alloc([2 * 8 * 512])
    vvr = P.R()
    pe = vv[:, 0:8 * 542].rearrange("p (a b) -> p a b", a=8)
    per = vvr
    v = vv[:, 0:4096].rearrange("p (a b) -> p a b", a=8)
    v2 = vv[:, 4096:8192].rearrange("p (a b) -> p a b", a=8)
    v2r = vvr
    xo = v2
    xor_ = vvr
    he = P.alloc([8, 542], BF16)
    her = P.R()
    act = he
    actr = her
    ue = P.alloc([8, 542], BF16)
    uer = P.R()
    sg = [P.alloc([512]) for _ in range(2)]
    sgr = P.R(2)
    st = P.alloc([4, 512])
    str_ = P.R()
    tmp = [P.alloc([512]) for _ in range(2)]
    tmr = P.R(2)
    def tile_body(t):
        W = t["W"]
        We = W + 30
        j = t["j"]
        if t.get("zero_halo"):
            P.V(lambda e: e.memset(xe[:, :, :], 0.0), [], [xer])
            P.dma("sync", xe[:, :, 15:15 + W], t["src"], reads=t["in_regions"], writes=[xer])
        else:
            if isinstance(t["src"], list):
                for (o_, w_, ap_) in t["src"]:
                    P.dma("sync", xe[:, :, o_:o_ + w_], ap_, reads=t["in_regions"], writes=[xer])
            else:
                P.dma("sync", xe[:, :, 0:We], t["src"], reads=t["in_regions"], writes=[xer])
        if t.get("pos") is not None:
            P.dma("scalar", pe[:, :, 0:We], t["pos"], writes=[per])
            P.V(lambda e, We=We: e.tensor_tensor(out=xe[:, :, 0:We], in0=xe[:, :, 0:We], in1=pe[:, :, 0:We], op=ALU.add),
                [xer, per], [xer])
        if getattr(E, "dbgx", None) is not None and t["col"] == 512:
            P.dma("sync", E.dbgx[:, 0:8, 0:We], xe[:, :, 0:We], reads=[xer], is_out=True)
        rms_mod(E, rc, xe, xer, We, sc["gsc1"], sc["sh1"], j, sc["r"], he, her)
        if getattr(E, "dbgx", None) is not None and t["col"] == 512:
            P.dma("gpsimd", E.dbgx[:, 8:16, 0:We], he[:, :, 0:We], reads=[her], is_out=True)
        pieces = [(0, min(We, 512))] + ([(512, We)] if We > 512 else [])
        for c in range(8):
            for (a, b) in pieces:
                psa, psar = P.bank()
                psg, psgr = P.bank()
                P.mm(psa[:, 0:b - a], psar, [(w1[:, k, c * 128:(c + 1) * 128], he[:, k, a:b]) for k in range(8)], [w1r, her])
                P.mm(psg[:, 0:b - a], psgr, [(w1[:, k, 1024 + c * 128:1024 + (c + 1) * 128], he[:, k, a:b]) for k in range(8)], [w1r, her])
                i = c % 2
                P.S(lambda e, c=c, a=a, b=b, i=i, psg=psg: e.activation(
                    out=sg[i][:, 0:b - a], in_=psg[:, 0:b - a], func=AF.Sigmoid,
                    bias=vt[:, c_bpw1 + 8 + c:c_bpw1 + 9 + c], scale=1.0), [psgr, vr], [sgr[i]])
                P.V(lambda e, c=c, a=a, b=b, i=i, psa=psa: e.scalar_tensor_tensor(
                    out=ue[:, c, a:b], in0=psa[:, 0:b - a], scalar=vt[:, c_bpw1 + c:c_bpw1 + c + 1], in1=sg[i][:, 0:b - a],
                    op0=ALU.add, op1=ALU.mult), [psar, vr, sgr[i]], [uer])
        for (edge, lo, hi) in ((t.get("ledge"), 0, 15), (t.get("redge"), We - 15, We)):
            if edge is None:
                continue
            if isinstance(edge, float):
                P.V(lambda e, lo=lo, hi=hi: e.memset(ue[:, :, lo:hi], 0.0), [], [uer])
            else:
                P.V(lambda e, lo=lo, hi=hi, edge=edge: e.tensor_scalar(
                    out=ue[:, :, lo:hi], in0=ue[:, :, lo:hi], scalar1=edge, scalar2=None, op0=ALU.mult),
                    [uer, E.edger], [uer])
        for c in range(8):
            di = c % 2
            dg, dgr = dgb[di], dgrs[di]
            for k in range(31):
                P.V(lambda e, c=c, k=k, dg=dg: e.tensor_scalar(out=dg[:, k, :], in0=E.ident,
                                                               scalar1=vt[:, c_wdw + c * 31 + k:c_wdw + c * 31 + k + 1],
                                                               scalar2=None, op0=ALU.mult), [E.cr, vr], [dgr])
            ps, psr = P.bank()
            P.mm(ps[:, 0:W], psr, [(dg[:, k, :], ue[:, c, k:k + W]) for k in range(31)], [dgr, uer])
            P.S(lambda e, c=c, ps=ps: e.activation(out=v[:, c, 0:W], in_=ps[:, 0:W], func=AF.Identity,
                                                   bias=vt[:, c_bdw + c:c_bdw + c + 1], scale=1.0), [psr, vr], [vvr])
            P.V(lambda e, c=c: e.tensor_tensor(out=v2[:, c, 0:W], in0=v[:, c, 0:W], in1=v[:, c, 0:W], op=ALU.mult),
                [vvr], [v2r])
        if getattr(E, "dbgx", None) is not None and t["col"] == 512:
            P.dma("gpsimd", E.dbgx[:, 16:24, 0:We], ue[:, :, 0:We], reads=[uer], is_out=True)
            P.dma("sync", E.dbgx[:, 24:32, 0:W], v[:, :, 0:W], reads=[vvr], is_out=True)
        ps1, ps1r = P.bank()
        ps2, ps2r = P.bank()
        P.mm(ps1[:, 0:W], ps1r, [(E.ones, v[:, c, 0:W]) for c in range(8)], [E.cr, vvr])
        P.mm(ps2[:, 0:W], ps2r, [(E.ones, v2[:, c, 0:W]) for c in range(8)], [E.cr, v2r])
        mean, var, rstd, nmr = st[:, 0, 0:W], st[:, 1, 0:W], st[:, 2, 0:W], st[:, 3, 0:W]
        P.V(lambda e: e.tensor_scalar(out=mean, in0=ps1[:, 0:W], scalar1=1.0 / D, scalar2=None, op0=ALU.mult), [ps1r], [str_])
        P.V(lambda e: e.tensor_tensor(out=var, in0=mean, in1=mean, op=ALU.mult), [str_], [str_])
        P.V(lambda e: e.scalar_tensor_tensor(out=var, in0=ps2[:, 0:W], scalar=1.0 / D, in1=var, op0=ALU.mult, op1=ALU.subtract),
            [ps2r, str_], [str_])
        P.V(lambda e: e.tensor_scalar(out=rstd, in0=var, scalar1=float(EPS), scalar2=None, op0=ALU.add), [str_], [str_])
        P.S(lambda e: e.sqrt(out=rstd, in_=rstd), [str_], [str_])
        P.V(lambda e: e.reciprocal(out=rstd, in_=rstd), [str_], [str_])
        for c in range(8):
            i = c % 2
            P.V(lambda e, c=c, i=i: e.tensor_tensor(out=tmp[i][:, 0:W], in0=v[:, c, 0:W], in1=mean, op=ALU.subtract),
                [vvr, str_], [tmr[i]])
            P.V(lambda e, c=c, i=i: e.tensor_tensor(out=tmp[i][:, 0:W], in0=tmp[i][:, 0:W], in1=rstd, op=ALU.mult),
                [str_, tmr[i]], [tmr[i]])
            P.S(lambda e, c=c, i=i: e.activation(out=act[:, c, 0:W], in_=tmp[i][:, 0:W], func=AF.Silu,
                                                 bias=vt[:, c_lnb + c:c_lnb + c + 1], scale=vt[:, c_lng + c:c_lng + c + 1]),
                [tmr[i], vr], [actr])
        if getattr(E, "dbgx", None) is not None and t["col"] == 512:
            P.dma("gpsimd", E.dbgx[:, 32:40, 0:W], act[:, :, 0:W], reads=[actr], is_out=True)
            P.dma("sync", E.dbgx[:, 40:44, 0:W], st[:, :, 0:W], reads=[str_], is_out=True)
        for c in range(8):
            ps, psr = P.bank()
            P.mm(ps[:, 0:W], psr, [(w2[:, k, c * 128:(c + 1) * 128], act[:, k, 0:W]) for k in range(8)], [w2r, actr])
            P.V(lambda e, c=c, ps=ps: e.scalar_tensor_tensor(
                out=xo[:, c, 0:W], in0=ps[:, 0:W], scalar=sc["g1"][:, c, j:j + 1], in1=xe[:, c, 15:15 + W],
                op0=ALU.mult, op1=ALU.add), [psr, sc["modr"], xer], [xor_])
            P.S(lambda e, c=c: e.activation(out=xo[:, c, 0:W], in_=xo[:, c, 0:W], func=AF.Identity,
                                            bias=g1b[:, c, j:j + 1], scale=1.0), [xor_, g1br], [xor_])
        P.dma("sync", xout[:, :, t["ocol"]:t["ocol"] + W], xo[:, :, 0:W], reads=[xor_], writes=t["out_regions"])
    for t in tiles:
        tile_body(t)
    P.release()


def stage_ffn(E, groups, xin, sc, wg, wu, wd, xout, moe=None):
    P = E.P
    P.mark()
    GW = max(g["W"] for g in groups)
    x = P.alloc([8, GW])
    xr = P.R()
    tok = P.alloc([8, GW], BF16)
    tokr = P.R()
    h = P.alloc([FC, GW], BF16)
    hr = P.R()
    rc = dict(sq=h[:, 0:8, :], sqr=hr, rstd=P.alloc([GW]), rr=P.R(), tmp=[P.alloc([GW]) for _ in range(2)], tr=P.R(2))
    wgb = [P.alloc([8, 512], BF16) for _ in range(2)]
    wub = [P.alloc([8, 512], BF16) for _ in range(2)]
    wgr, wur = P.R(2), P.R(2)
    wdb = [P.alloc([FC, 128], BF16) for _ in range(2)]
    wdr = P.R(2)
    sgt = [P.alloc([512]) for _ in range(2)]
    sgr = P.R(2)
    nexp = 1
    if moe is not None:
        nexp = NE
        tokf = h[:, 8:24, :].rearrange("p a b -> p (a b)").bitcast(F32).rearrange("p (a b) -> p a b", a=8)
        wrt = P.alloc([8, 8])
        wrr = P.R()
        P.dma("sync", wrt, moe["wr"].rearrange("(k p) n -> p k n", p=128), writes=[wrr])
        nbm = GW // 128
        lg = P.alloc([nbm, 8])
        l2 = P.alloc([nbm, 8])
        sp = P.alloc([nbm, 8])
        sm = P.alloc([4, nbm])
        lgr = P.R()
        gT = P.alloc([GW], parts=8)
        gTr = P.R()
        gb = [P.alloc([GW])] * 2
        gbr = [P.R()] * 2
        sg2 = [P.alloc([512]) for _ in range(2)]
        sg2r = P.R(2)
    wgv = wg.rearrange("e (k p) n -> e p k n", p=128) if moe else wg.rearrange("(k p) n -> p k n", p=128)
    wuv = wu.rearrange("e (k p) n -> e p k n", p=128) if moe else wu.rearrange("(k p) n -> p k n", p=128)
    wdv = wd.rearrange("e (f p) n -> e p f n", p=128) if moe else wd.rearrange("(f p) n -> p f n", p=128)
    ld = 0
    def group_body(g):
        nonlocal ld
        W, j, col = g["W"], g["j"], g["col"]
        subs = [(a, min(a + 512, W)) for a in range(0, W, 512)]
        P.dma("sync", x[:, :, 0:W], xin[:, :, col:col + W], reads=g["in_regions"], writes=[xr])
        if moe is None:
            rms_mod(E, rc, x, xr, W, sc["gsc2"], sc["sh2"], j, sc["r"], tok, tokr)
        else:
            rms_mod(E, rc, x, xr, W, sc["gsc2"], sc["sh2"], j, sc["r"], tok, tokr, out_f32=tokf, out_f32_r=hr)
            nb = W // 128
            psl, pslr = P.bank()
            for tb in range(nb):
                P.mm(psl[:, tb * 8:(tb + 1) * 8], pslr,
                     [(tokf[:, k, tb * 128:(tb + 1) * 128], wrt[:, k, :]) for k in range(8)], [hr, wrr])
            m1, m2, nm1, den = sm[:, 0, 0:nb], sm[:, 1, 0:nb], sm[:, 2, 0:nb], sm[:, 3, 0:nb]
            P.V(lambda e: e.tensor_copy(out=lg[:, 0:nb, :], in_=psl[:, 0:nb * 8].rearrange("p (a b) -> p a b", b=8)),
                [pslr], [lgr])
            P.V(lambda e: e.tensor_reduce(out=m1, in_=lg[:, 0:nb, :], axis=AX.X, op=ALU.max), [lgr], [lgr])
            P.V(lambda e: e.tensor_scalar(out=nm1, in0=m1, scalar1=-1.0, scalar2=None, op0=ALU.mult), [lgr], [lgr])
            for tb in range(nb):
                P.V(lambda e, tb=tb: e.tensor_scalar(out=l2[:, tb, :], in0=lg[:, tb, :], scalar1=sm[:, 0, tb:tb + 1],
                                                     scalar2=-1e30, op0=ALU.is_equal, op1=ALU.mult), [lgr], [lgr])
            P.V(lambda e: e.tensor_tensor(out=l2[:, 0:nb, :], in0=l2[:, 0:nb, :], in1=lg[:, 0:nb, :], op=ALU.add), [lgr], [lgr])
            P.V(lambda e: e.tensor_reduce(out=m2, in_=l2[:, 0:nb, :], axis=AX.X, op=ALU.max), [lgr], [lgr])
            for tb in range(nb):
                P.S(lambda e, tb=tb: e.activation(out=sp[:, tb, :], in_=lg[:, tb, :], func=AF.Exp,
                                                  bias=sm[:, 2, tb:tb + 1], scale=1.0), [lgr], [lgr])
                P.V(lambda e, tb=tb: e.scalar_tensor_tensor(out=sp[:, tb, :], in0=lg[:, tb, :], scalar=sm[:, 1, tb:tb + 1],
                                                            in1=sp[:, tb, :], op0=ALU.is_ge, op1=ALU.mult), [lgr], [lgr])
            P.V(lambda e: e.tensor_reduce(out=den, in_=sp[:, 0:nb, :], axis=AX.X, op=ALU.add), [lgr], [lgr])
            P.V(lambda e: e.reciprocal(out=den, in_=den), [lgr], [lgr])
            for tb in range(nb):
                P.V(lambda e, tb=tb: e.tensor_scalar(out=sp[:, tb, :], in0=sp[:, tb, :], scalar1=sm[:, 3, tb:tb + 1],
                                                     scalar2=None, op0=ALU.mult), [lgr], [lgr])
            pst, pstr = P.bank()
            pst2, pst2r = P.bank()
            for tb in range(nb):
                pp, ppr = (pst, pstr) if tb < 4 else (pst2, pst2r)
                o = (tb % 4) * 128
                P.mm(pp[0:8, o:o + 128], ppr, [(sp[:, tb, :], E.ident)], [lgr, E.cr])
            P.S(lambda e: e.copy(out=gT[:, 0:min(W, 512)], in_=pst[0:8, 0:min(W, 512)]), [pstr], [gTr])
            if W > 512:
                P.S(lambda e: e.copy(out=gT[:, 512:W], in_=pst2[0:8, 0:W - 512]), [pst2r], [gTr])
        for ex in range(nexp):
            if moe is not None:
                gi = ex % 2
                for (a, b) in subs:
                    ps, psr = P.bank()
                    P.mm(ps[:, 0:b - a], psr, [(moe["selc"][:, ex * 128:(ex + 1) * 128], gT[:, a:b])], [moe["selr"], gTr])
                    P.S(lambda e, a=a, b=b, ps=ps, gi=gi: e.copy(out=gb[gi][:, a:b], in_=ps[:, 0:b - a]), [psr], [gbr[gi]])
            for fb in range(7):
                i = ld % 2
                ld += 1
                srcg = wgv[ex][:, :, fb * 512:(fb + 1) * 512] if moe else wgv[:, :, fb * 512:(fb + 1) * 512]
                srcu = wuv[ex][:, :, fb * 512:(fb + 1) * 512] if moe else wuv[:, :, fb * 512:(fb + 1) * 512]
                P.dma("gpsimd", wgb[i], srcg, writes=[wgr[i]])
                P.dma("gpsimd", wub[i], srcu, writes=[wur[i]])
                for (a, b) in subs:
                    for fc in range(4):
                        f = fb * 4 + fc
                        psg, psgr = P.bank()
                        psu, psur = P.bank()
                        P.mm(psg[:, 0:b - a], psgr, [(wgb[i][:, k, fc * 128:(fc + 1) * 128], tok[:, k, a:b]) for k in range(8)],
                             [wgr[i], tokr])
                        P.mm(psu[:, 0:b - a], psur, [(wub[i][:, k, fc * 128:(fc + 1) * 128], tok[:, k, a:b]) for k in range(8)],
                             [wur[i], tokr])
                        si = f % 2
                        P.S(lambda e, psg=psg, si=si, a=a, b=b: e.activation(out=sgt[si][:, 0:b - a], in_=psg[:, 0:b - a],
                                                                             func=AF.Silu), [psgr], [sgr[si]])
                        if moe is None:
                            P.V(lambda e, psu=psu, si=si, a=a, b=b, f=f: e.tensor_tensor(
                                out=h[:, f, a:b], in0=sgt[si][:, 0:b - a], in1=psu[:, 0:b - a], op=ALU.mult),
                                [psur, sgr[si]], [hr])
                        else:
                            P.G(lambda e, si=si, a=a, b=b, gi=gi: e.tensor_tensor(
                                out=sg2[si][:, 0:b - a], in0=sgt[si][:, 0:b - a], in1=gb[gi][:, a:b], op=ALU.mult),
                                [sgr[si], gbr[gi]], [sg2r[si]])
                            P.V(lambda e, psu=psu, si=si, a=a, b=b, f=f: e.tensor_tensor(
                                out=h[:, f, a:b], in0=sg2[si][:, 0:b - a], in1=psu[:, 0:b - a], op=ALU.mult),
                                [psur, sg2r[si]], [hr])
            for d in range(8):
                i = ld % 2
                ld += 1
                srcd = wdv[ex][:, :, d * 128:(d + 1) * 128] if moe else wdv[:, :, d * 128:(d + 1) * 128]
                P.dma("gpsimd", wdb[i], srcd, writes=[wdr[i]])
                for (a, b) in subs:
                    ps, psr = P.bank()
                    P.mm(ps[:, 0:b - a], psr, [(wdb[i][:, f, :], h[:, f, a:b]) for f in range(FC)], [wdr[i], hr])
                    P.V(lambda e, ps=ps, a=a, b=b, d=d: e.scalar_tensor_tensor(
                        out=x[:, d, a:b], in0=ps[:, 0:b - a], scalar=sc["g2"][:, d, j:j + 1], in1=x[:, d, a:b],
                        op0=ALU.mult, op1=ALU.add), [psr, sc["modr"], xr], [xr])
        P.dma("sync", xout[:, :, col:col + W], x[:, :, 0:W], reads=[xr], writes=g["out_regions"], is_out=g.get("is_out", False))
    for g in groups:
        group_body(g)
    P.release()


def stage_pre(E, tiles, xin, sc, w_in, n_out, bias, uout):
    P = E.P
    P.mark()
    w = P.alloc([8, n_out * 128], BF16)
    wr = P.R()
    wv = w_in.rearrange("(k p) n -> p k n", p=128)
    for k in range(8):
        P.dma("gpsimd", w[:, k, :], wv[:, k, :], writes=[wr])
    rc = rms_ctx(E, 512)
    x = P.alloc([8, 512])
    xr = P.R()
    hb = P.alloc([8, 512], BF16)
    hbr = P.R()
    ob = [P.alloc([8, 512]) for _ in range(2)]
    obr = P.R(2)
    n8 = 0
    def tile_body(t):
        nonlocal n8
        W, j, col = t["W"], t["j"], t["col"]
        P.dma("sync", x[:, :, 0:W], xin[:, :, col:col + W], reads=t["in_regions"], writes=[xr])
        rms_mod(E, rc, x, xr, W, sc["gsc1"], sc["sh1"], j, sc["r"], hb, hbr)
        for o8 in range(n_out // 8):
            i = n8 % 2
            n8 += 1
            for oo in range(8):
                oc = o8 * 8 + oo
                ps, psr = P.bank()
                P.mm(ps[:, 0:W], psr, [(w[:, k, oc * 128:(oc + 1) * 128], hb[:, k, 0:W]) for k in range(8)], [wr, hbr])
                if bias is not None:
                    bvt, bcol, bvr = bias
                    P.S(lambda e, ps=ps, oc=oc, oo=oo, i=i: e.activation(out=ob[i][:, oo, 0:W], in_=ps[:, 0:W], func=AF.Identity,
                                                                         bias=bvt[:, bcol + oc:bcol + oc + 1], scale=1.0),
                        [psr, bvr], [obr[i]])
                elif oo % 2 == 0:
                    P.S(lambda e, ps=ps, oo=oo, i=i: e.copy(out=ob[i][:, oo, 0:W], in_=ps[:, 0:W]), [psr], [obr[i]])
                else:
                    P.V(lambda e, ps=ps, oo=oo, i=i: e.tensor_copy(out=ob[i][:, oo, 0:W], in_=ps[:, 0:W]), [psr], [obr[i]])
            P.dma("sync", uout[:, o8 * 8:(o8 + 1) * 8, col:col + W], ob[i][:, :, 0:W], reads=[obr[i]], writes=[], is_out=True)
    for t in tiles:
        tile_body(t)
    P.release()


def tile_list(ncols_lat=TPC, with_ctx=True, step=512):
    tl = [dict(col=c, W=min(step, ncols_lat - c), j=0) for c in range(0, ncols_lat, step)]
    if with_ctx:
        tl.append(dict(col=ncols_lat, W=CTX, j=1))
    return tl


NCA = TPC + CTX


def col_regions(regs, col, W):
    return regs[col // 512:(col + W + 511) // 512]


VA = dict(c=0, adab0=16, adab1=64, n1_0=112, n2_0=120, n1_1=128, bpw1=136, bdw=152, lng=160, lnb=168, bpw2=176,
          wdw=184, hyb=432, edge=456, NV=458)


def build_A(debug=False):
    E = Env("A")
    P = E.P
    E.consts()
    xe_d = E.inp("xe", [8, 128, TPC + 32]).rearrange("c p w -> p c w")
    pos_d = E.inp("pos", [8, 128, TPC + 32]).rearrange("c p w -> p c w")
    ctx_d = E.inp("ctxT", [8, 128, CTX]).rearrange("c p w -> p c w")
    adaw = E.inp("ada_w", [2, 1024, 6144])
    wpw1 = E.inp("w_pw1", [1024, 2048])
    wpw2 = E.inp("w_pw2", [1024, 1024])
    wg = E.inp("w_gate", [1024, DFF])
    wu = E.inp("w_up", [1024, DFF])
    wd = E.inp("w_down", [DFF, 1024])
    win = E.inp("w_in", [1024, 3072])
    vt, vr = load_small(E, "vecs", VA["NV"])
    E.edger = vr
    xm = (E.out("xm", [8, 128, NCA]) if debug else E.scratch("xm", [8, 128, NCA])).rearrange("c p w -> p c w")
    x0 = (E.out("x0", [8, 128, NCA]) if True else E.scratch("x0", [8, 128, NCA])).rearrange("c p w -> p c w")
    uo = E.out("u_pre", [24, 128, NCA]).rearrange("c p w -> p c w")
    nreg = (NCA + 511) // 512
    xmr, x0r = P.R(nreg), P.R(nreg)
    modT = P.alloc([48, 2])
    modr = P.R()
    compute_mod(E, adaw[0], vt, vr, VA["c"], VA["adab0"], modT, modr)
    sc0 = layer_scalars(E, modT, modr, vt, vr, VA["n1_0"], VA["n2_0"])
    if debug:
        dbg = E.out("dbg", [128, 128])
        E.dbgx = E.out("dbgx", [128, 44, 542])
        P.dma("sync", dbg[:, 0:96], modT.rearrange("p a b -> p (a b)"), reads=[modr], is_out=True)
        P.dma("sync", dbg[:, 96:112], sc0["gsc1"].rearrange("p a b -> p (a b)"), reads=[sc0["r"]], is_out=True)
        P.dma("sync", dbg[:, 112:128], sc0["gsc2"].rearrange("p a b -> p (a b)"), reads=[sc0["r"]], is_out=True)
    tiles = []
    for t in tile_list():
        t = dict(t)
        if t["j"] == 0:
            c0 = t["col"] + 1
            t.update(src=xe_d[:, :, c0:c0 + 542], pos=pos_d[:, :, c0:c0 + 542], in_regions=[], ocol=t["col"])
            if t["col"] == 0:
                t["ledge"] = vt[:, VA["edge"]:VA["edge"] + 1]
            if t["col"] == TPC - 512:
                t["redge"] = vt[:, VA["edge"] + 1:VA["edge"] + 2]
        else:
            t.update(src=ctx_d, pos=None, in_regions=[], ocol=t["col"], zero_halo=True, ledge=0.0, redge=0.0)
        t["out_regions"] = col_regions(xmr, t["col"], t["W"])
        tiles.append(t)
    stage_conformer(E, tiles, wpw1, wpw2, vt, vr,
                    (VA["bpw1"], VA["bdw"], VA["lng"], VA["lnb"], VA["bpw2"], VA["wdw"]), sc0, xm, None)
    groups = []
    for g in tile_list(step=1024):
        g = dict(g)
        g["in_regions"] = col_regions(xmr, g["col"], g["W"])
        g["out_regions"] = col_regions(x0r, g["col"], g["W"])
        g["is_out"] = True
        groups.append(g)
    stage_ffn(E, groups, xm, sc0, wg, wu, wd, x0)
    modT1 = P.alloc([48, 2])
    modr1 = P.R()
    compute_mod(E, adaw[1], vt, vr, VA["c"], VA["adab1"], modT1, modr1)
    sc1 = layer_scalars(E, modT1, modr1, vt, vr, VA["n1_1"], VA["n1_1"])
    ptiles = []
    for t in tile_list():
        t = dict(t)
        t["in_regions"] = col_regions(x0r, t["col"], t["W"])
        ptiles.append(t)
    stage_pre(E, ptiles, x0, sc1, win, 24, (vt, VA["hyb"], vr), uo)
    P.finish()
    P.emit()
    return E


def pos_table():
    quarter = D // 4
    omega = (1.0 / (10000.0 ** (np.arange(quarter, dtype=np.float32) / np.float32(quarter)))).astype(np.float32)
    t = np.arange(SEQ)
    r = (t // GRID_W).astype(np.float32)[:, None] * omega
    col = (t % GRID_W).astype(np.float32)[:, None] * omega
    return np.concatenate([np.sin(r), np.cos(r), np.sin(col), np.cos(col)], axis=-1).astype(np.float32)


def chunked_T(a):
    T, C = a.shape
    return np.ascontiguousarray(a.T.reshape(C // 128, 128, T))


def unchunk_T(a):
    n, p, T = a.shape
    return np.ascontiguousarray(a.reshape(n * p, T).T)


def window_T(a, t0, lo, hi):
    S, C = a.shape
    out = np.zeros((hi - lo, C), np.float32)
    s0, s1 = max(0, t0 + lo), min(S, t0 + hi)
    out[s0 - (t0 + lo):s1 - (t0 + lo)] = a[s0:s1]
    return chunked_T(out)


def prep_A(inp):
    pos = pos_table()
    maps = []
    cst = host_consts()
    for k in range(NCORE):
        b, s = k // 4, k % 4
        t0 = s * TPC
        v = np.zeros((128, VA["NV"]), np.float32)
        cc = np.stack([fm(inp["c"][b]), fm(inp["c_ctx"])], axis=-1)
        v[:, VA["c"]:VA["c"] + 16] = cc.reshape(128, 16)
        v[:, VA["adab0"]:VA["adab0"] + 48] = fm(inp["ada_b"][0])
        v[:, VA["adab1"]:VA["adab1"] + 48] = fm(inp["ada_b"][1])
        v[:, VA["n1_0"]:VA["n1_0"] + 8] = fm(inp["norm1_g"][0])
        v[:, VA["n2_0"]:VA["n2_0"] + 8] = fm(inp["norm2_g"][0])
        v[:, VA["n1_1"]:VA["n1_1"] + 8] = fm(inp["norm1_g"][1])
        v[:, VA["bpw1"]:VA["bpw1"] + 16] = fm(inp["cf_b_pw1"][0])
        v[:, VA["bdw"]:VA["bdw"] + 8] = fm(inp["cf_b_dw"][0])
        v[:, VA["lng"]:VA["lng"] + 8] = fm(inp["cf_ln_g"][0])
        v[:, VA["lnb"]:VA["lnb"] + 8] = fm(inp["cf_ln_b"][0])
        v[:, VA["bpw2"]:VA["bpw2"] + 8] = fm(inp["cf_b_pw2"][0])
        wdw = np.asarray(inp["cf_w_dw"][0], np.float32)
        v[:, VA["wdw"]:VA["wdw"] + 248] = wdw.T.reshape(8, 128, 31).transpose(1, 0, 2).reshape(128, 248)
        v[:, VA["hyb"]:VA["hyb"] + 24] = fm(inp["hy_b_in"][0])
        v[:, VA["edge"]] = 0.0 if s == 0 else 1.0
        v[:, VA["edge"] + 1] = 0.0 if s == 3 else 1.0
        maps.append({
            "consts": cst,
            "xe": window_T(np.asarray(inp["x"][b]), t0, -16, TPC + 16),
            "pos": window_T(pos, t0, -16, TPC + 16),
            "ctxT": chunked_T(np.asarray(inp["ctx"][b])),
            "ada_w": np.ascontiguousarray(inp["ada_w"][0:2]),
            "w_pw1": np.asarray(inp["cf_w_pw1"][0]), "w_pw2": np.asarray(inp["cf_w_pw2"][0]),
            "w_gate": np.asarray(inp["ffn_w_gate"][0]), "w_up": np.asarray(inp["ffn_w_up"][0]),
            "w_down": np.asarray(inp["ffn_w_down"][0]), "w_in": np.asarray(inp["hy_w_in"][0]),
            "vecs": v,
        })
    return maps


HY_L = SEQ
TWO_PI = 2.0 * math.pi
VB = dict(b1=0, fr=1, b2=2, b3=3, nd=4, ndc=5, negpi=6, NV=8)


def hyena_filter_stage(E, zT, atau, L2, wf, vt, vr, G, Gr, asum, asr, ndcol):
    P = E.P
    wf1, wf2, wf3, wfr = wf
    nt = L2 // 512
    zt = [P.alloc([512]) for _ in range(2)]
    ztr = P.R(2)
    at = [P.alloc([512]) for _ in range(2)]
    atr = P.R(2)
    hh = [P.alloc([512]) for _ in range(3)]
    hr = P.R(3)
    dec = P.alloc([512])
    decr = P.R()
    gr_ = [P.alloc([512]) for _ in range(2)]
    grr = P.R(2)
    ab = P.alloc([512])
    abr = P.R()
    gb = [P.alloc([512], BF16) for _ in range(2)]
    gbr = P.R(2)
    wrp = P.alloc([512])
    wrp2 = P.alloc([512])
    wrr = P.R()

    def tile(ti):
        i = ti % 2
        c0 = ti * 512
        P.dma("sync", zt[i][0:33, :], zT[:, c0:c0 + 512], writes=[ztr[i]])
        P.dma("sync", at[i], atau[0:1, c0:c0 + 512].partition_broadcast(128), writes=[atr[i]])
        src, srcr = zt[i][0:33, :], ztr[i]
        for l in range(3):
            ps, psr = P.bank()
            lhs = wf1 if l == 0 else wf2[l - 1]
            P.mm(ps[0:64, :], psr, [(lhs, src)], [wfr, srcr])
            h = hh[l]
            P.V(lambda e, ps=ps, h=h, l=l: e.tensor_scalar(out=h[0:64, :], in0=ps[0:64, :],
                                                           scalar1=vt[0:64, VB["b1"] + l:VB["b1"] + l + 1] if l == 0 else vt[0:64, VB["b2"] + l - 1:VB["b2"] + l],
                                                           scalar2=vt[0:64, VB["fr"]:VB["fr"] + 1], op0=ALU.add, op1=ALU.mult),
                [psr, vr], [hr[l]])
            P.V(lambda e, h=h: e.tensor_scalar(out=wrp[0:64, :], in0=h[0:64, :], scalar1=-math.pi, scalar2=TWO_PI,
                                               op0=ALU.is_lt, op1=ALU.mult), [hr[l]], [wrr])
            P.V(lambda e, h=h: e.tensor_scalar(out=wrp2[0:64, :], in0=h[0:64, :], scalar1=math.pi, scalar2=-TWO_PI,
                                               op0=ALU.is_gt, op1=ALU.mult), [hr[l]], [wrr])
            P.V(lambda e, h=h: e.tensor_tensor(out=h[0:64, :], in0=h[0:64, :], in1=wrp[0:64, :], op=ALU.add), [hr[l], wrr], [hr[l]])
            P.V(lambda e, h=h: e.tensor_tensor(out=h[0:64, :], in0=h[0:64, :], in1=wrp2[0:64, :], op=ALU.add), [hr[l], wrr], [hr[l]])
            P.S(lambda e, h=h: e.activation(out=h[0:64, :], in_=h[0:64, :], func=AF.Sin), [hr[l]], [hr[l]])
            src, srcr = h[0:64, :], hr[l]
        P.S(lambda e, i=i: e.activation(out=dec, in_=at[i], func=AF.Exp, scale=vt[:, ndcol:ndcol + 1]), [atr[i], vr], [decr])
        halves = [(0, 512, 1 if c0 < L2 // 2 else 0)] if L2 > 512 else [(0, 256, 1), (256, 512, 0)]
        for o in range(2):
            ps, psr = P.bank()
            for (a, b, dr) in halves:
                blk = (o * 2 + dr) * 128
                P.mm(ps[:, a:b], psr, [(wf3[:, blk:blk + 128], src[:, a:b])], [wfr, srcr])
            g = gr_[o]
            P.V(lambda e, ps=ps, g=g: e.tensor_tensor(out=g, in0=ps[:, :], in1=dec, op=ALU.mult), [psr, decr], [grr[o]])
            if ti == 0:
                P.V(lambda e, g=g: e.memset(g[:, 0:1], 0.0), [], [grr[o]])
            P.S(lambda e, g=g: e.activation(out=ab, in_=g, func=AF.Abs), [grr[o]], [abr])
            P.V(lambda e, o=o: e.reduce_sum(out=asum[:, o, ti:ti + 1], in_=ab, axis=AX.X), [abr], [asr])
            P.S(lambda e, g=g, o=o: e.copy(out=gb[o], in_=g), [grr[o]], [gbr[o]])
            P.dma("sync", G[o][:, c0:c0 + 512], gb[o], reads=[gbr[o]], writes=[Gr])

    for ti in range(nt):
        tile(ti)


def build_B():
    E = Env("B")
    P = E.P
    E.consts()
    L = HY_L
    hu = E.inp("hu", [128, 6, L + 2])
    hc = E.inp("hc", [128, 6, CTX + 2])
    zT = E.inp("zT", [33, 2 * L])
    zcT = E.inp("zcT", [33, 512])
    atau = E.inp("atau", [1, 2 * L])
    atauc = E.inp("atauc", [1, 512])
    wf1_d = E.inp("wf1", [33, 64])
    wf2_d = E.inp("wf2", [64, 128])
    wf3_d = E.inp("wf3", [64, 512])
    hz = E.out("hz", [128, 2, L])
    hzc = E.out("hzc", [128, 2, CTX])
    vt, vr = load_small(E, "vecs", VB["NV"])
    hws, hwsr = load_small(E, "hws", 128 * 12)
    hbs, hbsr = load_small(E, "hbias", 128 * 2)
    G = E.scratch("G", [2, 128, 2 * L], BF16)
    Gc = E.scratch("Gc", [2, 128, 512], BF16)
    invd = E.scratch("invd", [128, 4])
    Gr, Gcr, invr = P.R(), P.R(), P.R()
    wfall = P.alloc([64 + 128 + 512])
    wfr = P.R()
    P.dma("sync", wfall[0:33, 0:64], wf1_d, writes=[wfr])
    P.dma("sync", wfall[0:64, 64:192], wf2_d, writes=[wfr])
    P.dma("sync", wfall[0:64, 192:704], wf3_d, writes=[wfr])
    wf = (wfall[0:33, 0:64], [wfall[0:64, 64:128], wfall[0:64, 128:192]], wfall[0:64, 192:704], wfr)
    asum = P.alloc([2, 64])
    asumc = P.alloc([2, 1])
    asr = P.R()
    inv = P.alloc([4])
    invb = P.alloc([128 * 4])
    invbr = P.R()
    P.mark()
    hyena_filter_stage(E, zT, atau, 2 * L, wf, vt, vr, G, Gr, asum, asr, VB["nd"])
    hyena_filter_stage(E, zcT, atauc, 512, wf, vt, vr, Gc, Gcr, asumc, asr, VB["ndc"])
    P.V(lambda e: e.reduce_sum(out=inv[:, 0:2], in_=asum, axis=AX.X), [asr], [asr])
    P.V(lambda e: e.tensor_copy(out=inv[:, 2:4], in_=asumc[:, :, 0]), [asr], [asr])
    P.V(lambda e: e.reciprocal(out=inv, in_=inv), [asr], [asr])
    P.dma("sync", invd, inv, reads=[asr], writes=[invr])
    P.dma("sync", invb, invd.rearrange("c f -> (c f)").partition_broadcast(128), reads=[invr], writes=[invbr])
    P.release()

    A = [P.alloc([6, 130]) for _ in range(2)]
    Ar = P.R(2)
    Ac = [P.alloc([6, 130]) for _ in range(2)]
    Acr = P.R(2)
    SK = [P.alloc([16384], BF16) for _ in range(4)]
    SKr = P.R(4)
    SKc = [P.alloc([384], BF16) for _ in range(2)]
    SKcr = P.R(2)
    st = dict(sk=0)

    def new_set(NB):
        return dict(Zb=P.alloc([2, NB], BF16), vf=P.alloc([2, NB]), x1=P.alloc([2, NB]), x2=P.alloc([2, NB]),
                    z1=P.alloc([2, NB]), z1b=P.alloc([2, NB], BF16), t1=P.alloc([2, NB]), z2=P.alloc([2, NB]),
                    zo=P.alloc([2, 128]), r=P.R(), NB=NB, y=P.alloc([6, 128]), yr=P.R())
    S_lat = [new_set(128), new_set(128)]
    S_ctx = [new_set(2), new_set(2)]
    BK = lambda i: (P.psum[i], P.psr[i])

    def conv(o, Z, Zr, NB, ch, lat, bank):
        ps, psr = BK(bank)
        pv = ps[:, 0:2 * NB].rearrange("p (b a) -> p b a", b=2)
        Gs = (G if lat else Gc)[o, ch]
        pieces = [(0, 128), (128, 255)] if lat else [(0, 3)]
        for (b0, b1) in pieces:
            if lat:
                hb = st["sk"] % 4
                st["sk"] += 1
                buf, bufr = SK[hb], SKr[hb]
            else:
                hb = st.setdefault("skc", 0) % 2
                st["skc"] = hb + 1
                buf, bufr = SKc[hb], SKcr[hb]
            ncol = (b1 - b0) * 128
            src = bass.AP(Gs.tensor, Gs.offset + 1 + b0 * 128, [[1, 128], [1, ncol]])
            P.dma("sync", buf[:, 0:ncol], src, reads=[Gr if lat else Gcr], writes=[bufr])
            blks = list(range(b0, b1))
            if b0 == 0:
                blks = [NB - 1] + [x for x in blks if x != NB - 1]
            pairs = []
            for blk in blks:
                dl = blk - (NB - 1)
                alo, ahi = max(0, dl), min(NB - 1, NB - 1 + dl)
                pairs.append((pv[:, :, alo:ahi + 1], buf[:, (blk - b0) * 128:(blk - b0 + 1) * 128], Z[:, :, alo - dl:ahi - dl + 1]))
            first = (b0 == 0)
            last = (b1 == pieces[-1][1])

            def fn(e, pairs=pairs, first=first, last=last):
                ins = None
                n = len(pairs)
                for i, (o_, l_, r_) in enumerate(pairs):
                    ins = e.matmul(o_, lhsT=l_, rhs=r_, start=(first and i == 0), stop=(last and i == n - 1))
                return ins
            P.op("tensor", fn, [bufr, Zr], [psr])
        return pv, psr

    def channel(ch, lat):
        S = (S_lat if lat else S_ctx)[ch % 2]
        NB = S["NB"]
        sr = S["r"]
        i = ch % 2
        a, ar = (A[i], Ar[i]) if lat else (Ac[i], Acr[i])
        yy, yyr = S["y"], S["yr"]
        Lx = L if lat else CTX
        src_t = hu if lat else hc
        base = src_t[ch]
        bkA, bkB, bkJ, bkC0, bkC1, bkT = (2, 3, 4, 0, 1, 5) if lat else (6, 7, 6, 7, 7, 6)
        src = bass.AP(base.tensor, base.offset, [[128, NB], [Lx + 2, 6], [1, 130]])
        P.dma("gpsimd", a[0:NB], src, writes=[ar])
        for s in range(3):
            wc = (ch * 3 + s) * 4
            sl = slice(2 * s, 2 * s + 2)
            P.V(lambda e, sl=sl, wc=wc: e.tensor_scalar(out=yy[0:NB, sl, :], in0=a[0:NB, sl, 0:128], scalar1=hws[0:NB, wc:wc + 1],
                                                        scalar2=hws[0:NB, wc + 3:wc + 4], op0=ALU.mult, op1=ALU.add), [ar, hwsr], [yyr])
            for k in (1, 2):
                P.V(lambda e, sl=sl, wc=wc, k=k: e.scalar_tensor_tensor(out=yy[0:NB, sl, :], in0=a[0:NB, sl, k:k + 128],
                                                                        scalar=hws[0:NB, wc + k:wc + k + 1], in1=yy[0:NB, sl, :],
                                                                        op0=ALU.mult, op1=ALU.add), [ar, hwsr, yyr], [yyr])
        yield
        pA, pAr = BK(bkA)
        pB, pBr = BK(bkB)
        for sb in range(6):
            pp, ppr = (pA, pAr) if sb < 4 else (pB, pBr)
            o_ = (sb % 4) * NB
            P.mm(pp[:, o_:o_ + NB], ppr, [(yy[0:NB, sb, :], E.ident[0:NB, 0:NB])], [yyr, E.cr])
        v3 = lambda t: t.rearrange("p b a -> p (b a)")
        P.V(lambda e: e.tensor_copy(out=v3(S["vf"]), in_=pA[:, 0:2 * NB]), [pAr], [sr])
        P.S(lambda e: e.copy(out=v3(S["x1"]), in_=pA[:, 2 * NB:4 * NB]), [pAr], [sr])
        P.V(lambda e: e.tensor_copy(out=v3(S["x2"]), in_=pB[:, 0:2 * NB]), [pBr], [sr])
        pJ, pJr = BK(bkJ)
        P.mm(pJ[:, 0:2 * NB], pJr, [(E.J, v3(S["vf"]))], [sr, E.cr])
        P.S(lambda e: e.copy(out=v3(S["Zb"]), in_=pJ[:, 0:2 * NB]), [pJr], [sr])
        yield
        ic = ch * 4 + (0 if lat else 2)
        zin, zf = S["Zb"], S["vf"]
        for o in range(2):
            pv, psr = conv(o, zin, sr, NB, ch, lat, bkC0 if o == 0 else bkC1)
            yield
            gate = S["x1"] if o == 0 else S["x2"]
            zo_ = S["z1"] if o == 0 else S["z2"]
            P.V(lambda e, pv=pv, o=o: e.tensor_scalar(out=S["t1"], in0=pv, scalar1=invb[:, ic + o:ic + o + 1], scalar2=None,
                                                      op0=ALU.mult), [psr, invbr], [sr])
            P.V(lambda e, o=o, zf=zf: e.scalar_tensor_tensor(out=S["t1"], in0=zf, scalar=hbs[:, ch * 2 + o:ch * 2 + o + 1],
                                                             in1=S["t1"], op0=ALU.mult, op1=ALU.add), [sr, hbsr], [sr])
            P.V(lambda e, gate=gate, zo_=zo_: e.tensor_tensor(out=zo_, in0=S["t1"], in1=gate, op=ALU.mult), [sr], [sr])
            if o == 0:
                pJ2, pJ2r = BK(bkJ)
                P.mm(pJ2[:, 0:2 * NB], pJ2r, [(E.J, v3(S["z1"]))], [sr, E.cr])
                P.S(lambda e, pJ2=pJ2: e.copy(out=v3(S["z1b"]), in_=pJ2[:, 0:2 * NB]), [pJ2r], [sr])
                zin, zf = S["z1b"], S["z1"]
            yield
        pT, pTr = BK(bkT)
        for b in range(2):
            P.mm(pT[0:NB, b * 128:(b + 1) * 128], pTr, [(S["z2"][:, b, :], E.ident)], [sr, E.cr])
        P.S(lambda e: e.copy(out=S["zo"][0:NB].rearrange("p b a -> p (b a)"), in_=pT[0:NB, 0:256]), [pTr], [sr])
        dst_t = hz if lat else hzc
        dbase = dst_t[ch]
        dst = bass.AP(dbase.tensor, dbase.offset, [[128, NB], [Lx, 2], [1, 128]])
        P.dma("gpsimd", dst, S["zo"][0:NB], reads=[sr], writes=[], is_out=True)

    def adv(g, n=1):
        for _ in range(n):
            next(g, None)

    glat = [channel(ch, True) for ch in range(128)]
    gctx = [channel(ch, False) for ch in range(128)]
    adv(glat[0], 2)
    for ch in range(128):
        adv(glat[ch])
        if ch + 1 < 128:
            adv(glat[ch + 1], 2)
        adv(gctx[ch], 2)
        adv(glat[ch])
        adv(gctx[ch])
        adv(glat[ch])
        adv(gctx[ch], 2)
        adv(glat[ch])
        for _ in glat[ch]:
            pass
        for _ in gctx[ch]:
            pass
    P.finish()
    P.emit()
    return E


def hyena_z_table(L):
    f32 = np.float32
    ip = np.arange(2 * L)
    tau = np.minimum(np.abs(ip - L), L - 1)
    t = (np.linspace(0.0, 1.0, L, dtype=f32))[tau][:, None]
    bands = 16
    w = (f32(2.0 * math.pi) * np.arange(L, dtype=f32) / f32(L))[tau][:, None]
    fb = np.linspace(1e-4, bands - 1, bands, dtype=f32)[None, :]
    z = np.concatenate([t, np.cos(fb * w), -np.sin(fb * w)], axis=-1).astype(f32)
    return np.ascontiguousarray(z.T), tau.astype(f32)[None, :]


def prep_B(inp, upre):
    L = HY_L
    zT, atau = hyena_z_table(L)
    zcT, atauc = hyena_z_table(CTX)
    dmin = math.log(1e-2) / 1.5
    dmax = math.log(1e-2) / 0.3
    deltas = np.abs(np.linspace(dmin, dmax, D, dtype=np.float32))
    cst = host_consts()
    wsh = np.asarray(inp["hy_w_short"][0], np.float32)
    bsh = np.asarray(inp["hy_b_short"][0], np.float32)
    hb = np.asarray(inp["hy_bias"][0], np.float32)
    wf3 = np.asarray(inp["hy_w_f3"][0], np.float32)
    maps = []
    for k in range(NCORE):
        hu = np.zeros((128, 6, L + 2), np.float32)
        hc = np.zeros((128, 6, CTX + 2), np.float32)
        for s in range(3):
            for b in range(BATCH):
                for seg in range(4):
                    hu[:, s * 2 + b, 1 + seg * TPC:1 + (seg + 1) * TPC] = upre[4 * b + seg][s * 8 + k][:, 0:TPC]
                hc[:, s * 2 + b, 1:1 + CTX] = upre[4 * b][s * 8 + k][:, TPC:TPC + CTX]
        v = np.zeros((128, VB["NV"]), np.float32)
        v[0:64, VB["b1"]] = inp["hy_b_f1"][0]
        v[0:64, VB["fr"]] = inp["hy_freq"][0]
        v[0:64, VB["b2"]] = inp["hy_b_f2"][0][0]
        v[0:64, VB["b3"]] = inp["hy_b_f2"][0][1]
        v[:, VB["nd"]] = -deltas[k * 128:(k + 1) * 128] / np.float32(L - 1)
        v[:, VB["ndc"]] = -deltas[k * 128:(k + 1) * 128] / np.float32(CTX - 1)
        v[:, VB["negpi"]] = -math.pi
        hws = np.zeros((128, 3, 4), np.float32)
        for s in range(3):
            cs = s * D + k * 128
            hws[:, s, 0:3] = wsh[:, cs:cs + 128].T
            hws[:, s, 3] = bsh[cs:cs + 128]
        hbias = np.ascontiguousarray(hb[:, k * 128:(k + 1) * 128].T)
        w3 = np.concatenate([wf3[:, o * 2 * D + dr * D + k * 128:o * 2 * D + dr * D + (k + 1) * 128]
                             for o in range(2) for dr in range(2)], axis=1)
        maps.append({
            "consts": cst, "hu": hu, "hc": hc, "zT": zT, "zcT": zcT, "atau": atau, "atauc": atauc,
            "wf1": np.asarray(inp["hy_w_f1"][0], np.float32),
            "wf2": np.ascontiguousarray(np.concatenate([inp["hy_w_f2"][0][0], inp["hy_w_f2"][0][1]], axis=1), dtype=np.float32),
            "wf3": np.ascontiguousarray(w3), "vecs": v,
            "hws": np.ascontiguousarray(np.broadcast_to(hws.reshape(1, -1), (128, 128 * 12))),
            "hbias": np.ascontiguousarray(np.broadcast_to(hbias.reshape(1, -1), (128, 256))),
        })
    return maps


def stage_post(E, tiles, xin, rin, sc, w_out, bias, xout):
    P = E.P
    P.mark()
    w = P.alloc([8, 1024], BF16)
    wr = P.R()
    P.dma("gpsimd", w, w_out.rearrange("(k p) n -> p k n", p=128), writes=[wr])
    x = [P.alloc([8, 512]) for _ in range(2)]
    xr = P.R(2)
    rb = [P.alloc([8, 512], BF16) for _ in range(2)]
    rbr = P.R(2)
    g1b = None
    if bias is not None:
        bvt, bcol, bvr = bias
        g1b = P.alloc([8, 2])
        g1br = P.R()
        for j in range(2):
            P.V(lambda e, j=j: e.tensor_tensor(out=g1b[:, :, j], in0=sc["g1"][:, :, j], in1=bvt[:, bcol:bcol + 8], op=ALU.mult),
                [sc["modr"], bvr], [g1br])
    cnt = [0]

    def tile_body(t):
        W, j, col = t["W"], t["j"], t["col"]
        i = cnt[0] % 2
        cnt[0] += 1
        xx, xxr, rr, rrr = x[i], xr[i], rb[i], rbr[i]
        P.dma("sync", xx[:, :, 0:W], xin[:, :, col:col + W], reads=t["in_regions"], writes=[xxr])
        P.dma("gpsimd", rr[:, :, 0:W], rin[:, :, col:col + W], reads=t.get("r_regions", []), writes=[rrr])
        for c in range(8):
            ps, psr = P.bank()
            P.mm(ps[:, 0:W], psr, [(w[:, k, c * 128:(c + 1) * 128], rr[:, k, 0:W]) for k in range(8)], [wr, rrr])
            P.V(lambda e, c=c, ps=ps: e.scalar_tensor_tensor(out=xx[:, c, 0:W], in0=ps[:, 0:W], scalar=sc["g1"][:, c, j:j + 1],
                                                             in1=xx[:, c, 0:W], op0=ALU.mult, op1=ALU.add), [psr, sc["modr"], xxr], [xxr])
            if g1b is not None:
                P.S(lambda e, c=c: e.activation(out=xx[:, c, 0:W], in_=xx[:, c, 0:W], func=AF.Identity,
                                                bias=g1b[:, c, j:j + 1], scale=1.0), [xxr, g1br], [xxr])
        P.dma("sync", xout[:, :, col:col + W], xx[:, :, 0:W], reads=[xxr], writes=t["out_regions"])
    for t in tiles:
        tile_body(t)
    P.release()


def host_sel():
    s = np.zeros((8, 8 * 128), np.float32)
    for e in range(8):
        s[e, e * 128:(e + 1) * 128] = 1.0
    return s


def load_sel(E):
    P = E.P
    d = E.inp("selc", [8, 1024])
    t = P.alloc([1024], parts=8)
    r = P.R()
    P.dma("sync", t, d, writes=[r])
    return t, r


VC = dict(c=0, adab1=16, adab2=64, n2_1=112, n1_2=120, bout=128, NV=136)


def build_C():
    E = Env("C")
    P = E.P
    E.consts()
    x0 = E.inp("x0", [8, 128, NCA]).rearrange("c p w -> p c w")
    zT = E.inp("zT", [8, 128, NCA]).rearrange("c p w -> p c w")
    adaw = E.inp("ada_w", [2, 1024, 6144])
    wout = E.inp("w_out", [1024, 1024])
    wrt = E.inp("w_router", [1024, 8])
    wg = E.inp("w_gate", [NE, 1024, DFF])
    wu = E.inp("w_up", [NE, 1024, DFF])
    wd = E.inp("w_down", [NE, DFF, 1024])
    win = E.inp("w_in", [1024, 5120])
    vt, vr = load_small(E, "vecs", VC["NV"])
    selc, selr = load_sel(E)
    xm = E.scratch("xm", [8, 128, NCA]).rearrange("c p w -> p c w")
    x1 = E.out("x1", [8, 128, NCA]).rearrange("c p w -> p c w")
    uo = E.out("u2", [40, 128, NCA]).rearrange("c p w -> p c w")
    nreg = (NCA + 511) // 512
    xmr, x1r = P.R(nreg), P.R(nreg)
    modT = P.alloc([48, 2])
    modr = P.R()
    compute_mod(E, adaw[0], vt, vr, VC["c"], VC["adab1"], modT, modr)
    sc1 = layer_scalars(E, modT, modr, vt, vr, VC["n2_1"], VC["n2_1"])
    tiles = []
    for t in tile_list():
        t = dict(t)
        t["in_regions"] = []
        t["out_regions"] = col_regions(xmr, t["col"], t["W"])
        tiles.append(t)
    stage_post(E, tiles, x0, zT, sc1, wout, (vt, VC["bout"], vr), xm)
    groups = []
    for g in tile_list(step=1024):
        g = dict(g)
        g["in_regions"] = col_regions(xmr, g["col"], g["W"])
        g["out_regions"] = col_regions(x1r, g["col"], g["W"])
        g["is_out"] = True
        groups.append(g)
    stage_ffn(E, groups, xm, sc1, wg, wu, wd, x1, moe=dict(wr=wrt, selc=selc, selr=selr))
    modT2 = P.alloc([48, 2])
    modr2 = P.R()
    compute_mod(E, adaw[1], vt, vr, VC["c"], VC["adab2"], modT2, modr2)
    sc2 = layer_scalars(E, modT2, modr2, vt, vr, VC["n1_2"], VC["n1_2"])
    ptiles = []
    for t in tile_list():
        t = dict(t)
        t["in_regions"] = col_regions(x1r, t["col"], t["W"])
        ptiles.append(t)
    stage_pre(E, ptiles, x1, sc2, win, 40, None, uo)
    P.finish()
    P.emit()
    return E


def cvec(inp, b):
    cc = np.stack([fm(inp["c"][b]), fm(inp["c_ctx"])], axis=-1)
    return cc.reshape(128, 16)


def gather_tok(chan_lat, chan_ctx, k):
    b, seg = k // 4, k % 4
    out = np.empty((8, 128, NCA), np.float32)
    for kk in range(8):
        out[kk, :, 0:TPC] = chan_lat[kk][:, b, seg * TPC:(seg + 1) * TPC]
        out[kk, :, TPC:] = chan_ctx[kk][:, b, :]
    return out


def prep_C(inp, x0s, hz, hzc):
    cst = host_consts()
    sel = host_sel()
    maps = []
    for k in range(NCORE):
        b = k // 4
        v = np.zeros((128, VC["NV"]), np.float32)
        v[:, VC["c"]:VC["c"] + 16] = cvec(inp, b)
        v[:, VC["adab1"]:VC["adab1"] + 48] = fm(inp["ada_b"][1])
        v[:, VC["adab2"]:VC["adab2"] + 48] = fm(inp["ada_b"][2])
        v[:, VC["n2_1"]:VC["n2_1"] + 8] = fm(inp["norm2_g"][1])
        v[:, VC["n1_2"]:VC["n1_2"] + 8] = fm(inp["norm1_g"][2])
        v[:, VC["bout"]:VC["bout"] + 8] = fm(inp["hy_b_out"][0])
        maps.append({
            "consts": cst, "selc": sel, "x0": x0s[k], "zT": gather_tok(hz, hzc, k),
            "ada_w": np.ascontiguousarray(inp["ada_w"][1:3]),
            "w_out": np.asarray(inp["hy_w_out"][0]), "w_router": np.asarray(inp["moe_w_router"][0]),
            "w_gate": np.asarray(inp["moe_w_gate"][0]), "w_up": np.asarray(inp["moe_w_up"][0]),
            "w_down": np.asarray(inp["moe_w_down"][0]), "w_in": np.asarray(inp["hg_w_in"][0]),
            "vecs": v,
        })
    return maps


HG_T = CTX + SEQ
HG_NCH = HG_T // 64
VD = dict(lbl=0, gn=4, NV=8)


def build_D(nblk=32):
    E = Env("D")
    P = E.P
    E.consts()
    qT = E.inp("qT", [128, 2, HG_T])
    ogT = E.inp("ogT", [128, 2, HG_T])
    zfT = E.inp("zfT", [128, 2, HG_T])
    zbT = E.inp("zbT", [128, 2, HG_T])
    vTM = E.inp("vTM", [64, 2, HG_NCH, 128])
    tri_d = E.inp("tri", [64, 128])
    rT = E.out("rT", [128, 2, SEQ])
    osc = E.scratch("osc", [128, 2, SEQ])
    oscr = [P.R(32) for _ in range(2)]
    vt, vr = load_small(E, "vecs", VD["NV"])
    tri = P.alloc([128], parts=64)
    trir = P.R()
    P.dma("sync", tri, tri_d, writes=[trir])
    triF, triB = tri[:, 0:64], tri[:, 64:128]
    lbt = P.alloc([8])
    lbr = P.R()
    P.S(lambda e: e.activation(out=lbt[:, 0:4], in_=vt[:, VD["lbl"]:VD["lbl"] + 4], func=AF.Exp), [vr], [lbr])
    P.V(lambda e: e.reduce_sum(out=lbt[:, 4:5], in_=lbt[:, 0:4], axis=AX.X), [lbr], [lbr])
    P.V(lambda e: e.reciprocal(out=lbt[:, 4:5], in_=lbt[:, 4:5]), [lbr], [lbr])
    P.V(lambda e: e.tensor_tensor(out=lbt[:, 5:6], in0=lbt[:, 1:2], in1=lbt[:, 2:3], op=ALU.add), [lbr], [lbr])
    P.V(lambda e: e.tensor_tensor(out=lbt[:, 5:6], in0=lbt[:, 5:6], in1=lbt[:, 4:5], op=ALU.mult), [lbr], [lbr])
    P.V(lambda e: e.tensor_scalar(out=lbt[:, 6:7], in0=lbt[:, 5:6], scalar1=-1.0, scalar2=1.0, op0=ALU.mult, op1=ALU.add),
        [lbr], [lbr])
    lb, oml = lbt[:, 5:6], lbt[:, 6:7]

    def mkbuf():
        d = {}
        for nm in ("qt", "zt", "f", "g", "kk", "qs", "bsb", "eb", "enb", "og", "of", "os", "sq"):
            d[nm] = P.alloc([512])
            d[nm + "_r"] = P.R()
        for nm in ("qd", "ki", "ke"):
            d[nm] = P.alloc([512], BF16)
            d[nm + "_r"] = P.R()
        d["gTM"] = P.alloc([8, 128], parts=64)
        d["gTM_r"] = P.R()
        for nm in ("v", "keTM"):
            d[nm] = P.alloc([8, 128], BF16, parts=64)
            d[nm + "_r"] = P.R()
        d["att"] = P.alloc([64], BF16, parts=64)
        d["att_r"] = P.R()
        d["S"] = P.alloc([128])
        d["S_r"] = P.R()
        d["Sb"] = P.alloc([128], BF16)
        d["Sb_r"] = P.R()
        return d
    bufs = [mkbuf() for _ in range(2)]

    def block(b, fwd, blk):
        B = bufs[b]
        ctxb = blk < 0
        nch = 4 if ctxb else 8
        W = nch * 64
        col0 = 0 if ctxb else CTX + blk * 512
        ch0 = col0 // 64
        zsrc = zfT if fwd else zbT
        tr = triF if fwd else triB
        R = lambda nm: B[nm + "_r"]
        P.dma("sync", B["qt"][:, 0:W], qT[:, b, col0:col0 + W], writes=[R("qt")])
        P.dma("sync", B["zt"][:, 0:W], zsrc[:, b, col0:col0 + W], writes=[R("zt")])
        P.dma("gpsimd", B["v"][:, 0:nch, :], vTM[:, b, ch0:ch0 + nch, :], writes=[R("v")])
        P.S(lambda e: e.activation(out=B["f"][:, 0:W], in_=B["zt"][:, 0:W], func=AF.Sigmoid), [R("zt")], [R("f")])
        P.V(lambda e: e.tensor_scalar(out=B["f"][:, 0:W], in0=B["f"][:, 0:W], scalar1=oml, scalar2=lb, op0=ALU.mult, op1=ALU.add),
            [R("f"), lbr], [R("f")])
        P.S(lambda e: e.activation(out=B["g"][:, 0:W], in_=B["f"][:, 0:W], func=AF.Ln), [R("f")], [R("g")])
        P.V(lambda e: e.tensor_scalar(out=B["kk"][:, 0:W], in0=B["f"][:, 0:W], scalar1=-1.0, scalar2=1.0, op0=ALU.mult, op1=ALU.add),
            [R("f")], [R("kk")])
        P.S(lambda e: e.activation(out=B["qs"][:, 0:W], in_=B["qt"][:, 0:W], func=AF.Silu), [R("qt")], [R("qs")])
        pts = [(P.psum[0], P.psr[0]), (P.psum[1], P.psr[1])]
        for c in range(nch):
            pp, ppr = pts[c // 4]
            P.mm(pp[0:64, (c % 4) * 128:(c % 4 + 1) * 128], ppr, [(B["g"][:, c * 64:(c + 1) * 64], E.ident)], [R("g"), E.cr])
        for hf in range((nch + 3) // 4):
            pp, ppr = pts[hf]
            n4 = min(4, nch - hf * 4)
            P.V(lambda e, pp=pp, hf=hf, n4=n4: e.tensor_copy(
                out=B["gTM"][:, hf * 4:hf * 4 + n4, :].rearrange("p a b -> p (a b)"), in_=pp[0:64, 0:n4 * 128]), [ppr], [R("gTM")])
        pb, pbr = P.psum[2], P.psr[2]
        for c in range(nch):
            P.mm(pb[:, c * 64:(c + 1) * 64], pbr, [(B["gTM"][:, c, :], tr)], [R("gTM"), trir])
        P.V(lambda e: e.tensor_copy(out=B["bsb"][:, 0:W], in_=pb[:, 0:W]), [pbr], [R("bsb")])
        P.S(lambda e: e.activation(out=B["eb"][:, 0:W], in_=B["bsb"][:, 0:W], func=AF.Exp), [R("bsb")], [R("eb")])
        P.S(lambda e: e.activation(out=B["enb"][:, 0:W], in_=B["bsb"][:, 0:W], func=AF.Exp, scale=-1.0), [R("bsb")], [R("enb")])
        P.V(lambda e: e.tensor_tensor(out=B["qd"][:, 0:W], in0=B["qs"][:, 0:W], in1=B["eb"][:, 0:W], op=ALU.mult),
            [R("qs"), R("eb")], [R("qd")])
        P.V(lambda e: e.tensor_tensor(out=B["ki"][:, 0:W], in0=B["kk"][:, 0:W], in1=B["enb"][:, 0:W], op=ALU.mult),
            [R("kk"), R("enb")], [R("ki")])
        lastcol = lambda c: (c * 64 + 63) if fwd else (c * 64)
        for c in range(nch):
            P.V(lambda e, c=c: e.tensor_scalar(out=B["ke"][:, c * 64:(c + 1) * 64], in0=B["ki"][:, c * 64:(c + 1) * 64],
                                               scalar1=B["eb"][:, lastcol(c):lastcol(c) + 1], scalar2=None, op0=ALU.mult),
                [R("ki"), R("eb")], [R("ke")])
        pts2 = [(P.psum[0], P.psr[0]), (P.psum[1], P.psr[1])]
        for c in range(nch):
            pp, ppr = pts2[c // 4]
            P.mm(pp[0:64, (c % 4) * 128:(c % 4 + 1) * 128], ppr, [(B["ke"][:, c * 64:(c + 1) * 64], E.ident_b)], [R("ke"), E.cr])
        for hf in range((nch + 3) // 4):
            pp, ppr = pts2[hf]
            n4 = min(4, nch - hf * 4)
            P.S(lambda e, pp=pp, hf=hf, n4=n4: e.copy(
                out=B["keTM"][:, hf * 4:hf * 4 + n4, :].rearrange("p a b -> p (a b)"), in_=pp[0:64, 0:n4 * 128]), [ppr], [R("keTM")])
        po, por = (None, None) if ctxb else (P.psum[3 + b], P.psr[3 + b])
        order = range(nch) if fwd else range(nch - 1, -1, -1)
        yield
        for c in order:
            cs = slice(c * 64, (c + 1) * 64)
            if not ctxb:
                pa, par = P.psum[5], P.psr[5]
                P.mm(pa[0:64, 0:64], par, [(B["ki"][:, cs], B["qd"][:, cs])], [R("ki"), R("qd")])
                P.V(lambda e, pa=pa: e.tensor_tensor(out=B["att"], in0=pa[0:64, 0:64], in1=tr, op=ALU.mult), [par, trir], [R("att")])
                P.mm(po[:, cs], por, [(B["v"][:, c, :], B["att"]), (B["Sb"], B["qd"][:, cs])], [R("v"), R("att"), R("Sb"), R("qd")])
            pd, pdr = P.psum[6], P.psr[6]
            P.mm(pd[:, 0:128], pdr, [(B["keTM"][:, c, :], B["v"][:, c, :])], [R("keTM"), R("v")])
            P.V(lambda e, pd=pd, c=c: e.scalar_tensor_tensor(out=B["S"], in0=B["S"], scalar=B["eb"][:, lastcol(c):lastcol(c) + 1],
                                                             in1=pd[:, 0:128], op0=ALU.mult, op1=ALU.add), [R("S"), R("eb"), pdr], [R("S")])
            P.S(lambda e: e.copy(out=B["Sb"], in_=B["S"]), [R("S")], [R("Sb")])
            yield
        if ctxb:
            return
        t0 = blk * 512
        if fwd:
            P.S(lambda e: e.copy(out=B["os"], in_=po[:, :]), [por], [R("os")])
            P.dma("sync", osc[:, b, t0:t0 + 512], B["os"], reads=[R("os")], writes=[oscr[b][blk]])
            return
        P.dma("sync", B["of"], osc[:, b, t0:t0 + 512], reads=[oscr[b][blk]], writes=[R("of")])
        P.dma("sync", B["og"], ogT[:, b, CTX + t0:CTX + t0 + 512], writes=[R("og")])
        P.V(lambda e: e.tensor_tensor(out=B["os"], in0=po[:, :], in1=B["of"], op=ALU.add), [por, R("of")], [R("os")])
        P.V(lambda e: e.tensor_tensor(out=B["sq"], in0=B["os"], in1=B["os"], op=ALU.mult), [R("os")], [R("sq")])
        pn, pnr = P.psum[7], P.psr[7]
        P.mm(pn[:, :], pnr, [(E.ones, B["sq"])], [E.cr, R("sq")])
        P.V(lambda e: e.tensor_scalar(out=B["sq"], in0=pn[:, :], scalar1=1.0 / 128.0, scalar2=float(EPS), op0=ALU.mult, op1=ALU.add),
            [pnr], [R("sq")])
        P.S(lambda e: e.sqrt(out=B["sq"], in_=B["sq"]), [R("sq")], [R("sq")])
        P.V(lambda e: e.reciprocal(out=B["sq"], in_=B["sq"]), [R("sq")], [R("sq")])
        P.V(lambda e: e.scalar_tensor_tensor(out=B["os"], in0=B["os"], scalar=vt[:, VD["gn"]:VD["gn"] + 1], in1=B["sq"],
                                             op0=ALU.mult, op1=ALU.mult), [R("os"), vr, R("sq")], [R("os")])
        P.S(lambda e: e.activation(out=B["og"], in_=B["og"], func=AF.Silu), [R("og")], [R("og")])
        P.V(lambda e: e.tensor_tensor(out=B["os"], in0=B["os"], in1=B["og"], op=ALU.mult), [R("os"), R("og")], [R("os")])
        P.dma("sync", rT[:, b, t0:t0 + 512], B["os"], reads=[R("os")], writes=[], is_out=True)

    for fwd in (True, False):
        for b in range(2):
            P.V(lambda e, b=b: e.memset(bufs[b]["S"], 0.0), [], [bufs[b]["S_r"]])
            P.S(lambda e, b=b: e.copy(out=bufs[b]["Sb"], in_=bufs[b]["S"]), [bufs[b]["S_r"]], [bufs[b]["Sb_r"]])
        blks = [-1] + (list(range(nblk)) if fwd else list(range(nblk - 1, -1, -1)))
        for blk in blks:
            alive = [block(b, fwd, blk) for b in range(2)]
            while alive:
                for g_ in list(alive):
                    try:
                        next(g_)
                    except StopIteration:
                        alive.remove(g_)
    P.finish()
    P.emit()
    return E


def host_tri():
    s = np.arange(64)
    f = (s[:, None] <= s[None, :]).astype(np.float32)
    bk = (s[:, None] >= s[None, :]).astype(np.float32)
    return np.concatenate([f, bk], axis=1)


def prep_D(inp, u2s):
    cst = host_consts()
    tri = host_tri()
    maps = []
    lbl = np.asarray(inp["hg_lb_logits"], np.float32)
    for k in range(NCORE):
        def gat(s):
            out = np.empty((128, 2, HG_T), np.float32)
            for b in range(2):
                out[:, b, 0:CTX] = u2s[4 * b][s * 8 + k][:, TPC:TPC + CTX]
                for seg in range(4):
                    out[:, b, CTX + seg * TPC:CTX + (seg + 1) * TPC] = u2s[4 * b + seg][s * 8 + k][:, 0:TPC]
            return out
        iT = gat(1)
        vTM = np.ascontiguousarray(iT.transpose(1, 2, 0).reshape(2, HG_NCH, 64, 128).transpose(2, 0, 1, 3))
        v = np.zeros((128, VD["NV"]), np.float32)
        v[:, VD["lbl"]:VD["lbl"] + 4] = lbl[:, k * 128:(k + 1) * 128].T
        v[:, VD["gn"]] = inp["hg_gn_g"][0][k * 128:(k + 1) * 128]
        maps.append({"consts": cst, "tri": tri, "qT": gat(0), "ogT": gat(2), "zfT": gat(3), "zbT": gat(4), "vTM": vTM, "vecs": v})
    return maps


VE = dict(c=0, adab2=16, n2_2=64, NV=72)


def build_E():
    E = Env("E")
    P = E.P
    E.consts()
    x1 = E.inp("x1", [8, 128, NCA]).rearrange("c p w -> p c w")
    rT = E.inp("rT", [8, 128, TPC]).rearrange("c p w -> p c w")
    adaw = E.inp("ada_w", [1024, 6144])
    wout = E.inp("w_out", [1024, 1024])
    wg = E.inp("w_gate", [1024, DFF])
    wu = E.inp("w_up", [1024, DFF])
    wd = E.inp("w_down", [DFF, 1024])
    vt, vr = load_small(E, "vecs", VE["NV"])
    xm = E.scratch("xm", [8, 128, TPC]).rearrange("c p w -> p c w")
    x2 = E.out("x2", [8, 128, TPC]).rearrange("c p w -> p c w")
    nreg = TPC // 512
    xmr, x2r = P.R(nreg), P.R(nreg)
    modT = P.alloc([48, 2])
    modr = P.R()
    compute_mod(E, adaw, vt, vr, VE["c"], VE["adab2"], modT, modr)
    sc = layer_scalars(E, modT, modr, vt, vr, VE["n2_2"], VE["n2_2"])
    tiles = []
    for t in tile_list(with_ctx=False):
        t = dict(t)
        t["in_regions"] = []
        t["out_regions"] = col_regions(xmr, t["col"], t["W"])
        tiles.append(t)
    stage_post(E, tiles, x1, rT, sc, wout, None, xm)
    groups = []
    for g in tile_list(with_ctx=False, step=1024):
        g = dict(g)
        g["in_regions"] = col_regions(xmr, g["col"], g["W"])
        g["out_regions"] = col_regions(x2r, g["col"], g["W"])
        g["is_out"] = True
        groups.append(g)
    stage_ffn(E, groups, xm, sc, wg, wu, wd, x2)
    P.finish()
    P.emit()
    return E


def prep_E(inp, x1s, rTs):
    cst = host_consts()
    maps = []
    for k in range(NCORE):
        b, seg = k // 4, k % 4
        v = np.zeros((128, VE["NV"]), np.float32)
        v[:, VE["c"]:VE["c"] + 16] = cvec(inp, b)
        v[:, VE["adab2"]:VE["adab2"] + 48] = fm(inp["ada_b"][2])
        v[:, VE["n2_2"]:VE["n2_2"] + 8] = fm(inp["norm2_g"][2])
        r = np.stack([rTs[kk][:, b, seg * TPC:(seg + 1) * TPC] for kk in range(8)], axis=0)
        maps.append({"consts": cst, "x1": x1s[k], "rT": np.ascontiguousarray(r), "ada_w": np.asarray(inp["ada_w"][2]),
                     "w_out": np.asarray(inp["hg_w_out"][0]), "w_gate": np.asarray(inp["ffn_w_gate"][1]),
                     "w_up": np.asarray(inp["ffn_w_up"][1]), "w_down": np.asarray(inp["ffn_w_down"][1]), "vecs": v})
    return maps


VF = dict(c=0, adab3=16, n1_3=64, n2_3=72, bpw1=80, bdw=96, lng=104, lnb=112, bpw2=120, wdw=128, edge=376, nf=378, NV=386)


def stage_final(E, tiles, xin, vt, vr, col_nf, yout):
    P = E.P
    P.mark()
    rc = rms_ctx(E, 512)
    gs = P.alloc([8, 2])
    sh = P.alloc([8, 2])
    gr = P.R()
    P.V(lambda e: e.memset(sh, 0.0), [], [gr])
    P.V(lambda e: e.memset(gs, 0.0), [], [gr])
    P.V(lambda e: e.tensor_scalar(out=gs[:, :, 0], in0=vt[:, col_nf:col_nf + 8], scalar1=float(math.sqrt(D)), scalar2=None,
                                  op0=ALU.mult), [vr, gr], [gr])
    x = P.alloc([8, 512])
    xr = P.R()
    hb = P.alloc([8, 512], BF16)
    hbr = P.R()
    y = P.alloc([8, 512])
    yr = P.R()

    def tile_body(t):
        W, col = t["W"], t["col"]
        P.dma("sync", x[:, :, 0:W], xin[:, :, col:col + W], reads=t["in_regions"], writes=[xr])
        rms_mod(E, rc, x, xr, W, gs, sh, 0, gr, hb, hbr, out_f32=y, out_f32_r=yr)
        P.dma("sync", yout[:, :, col:col + W], y[:, :, 0:W], reads=[yr], writes=[], is_out=True)
    for t in tiles:
        tile_body(t)
    P.release()


def build_F():
    E = Env("F")
    P = E.P
    E.consts()
    xe_d = E.inp("xe", [8, 128, TPC + 32]).rearrange("c p w -> p c w")
    adaw = E.inp("ada_w", [1024, 6144])
    wpw1 = E.inp("w_pw1", [1024, 2048])
    wpw2 = E.inp("w_pw2", [1024, 1024])
    wrt = E.inp("w_router", [1024, 8])
    wg = E.inp("w_gate", [NE, 1024, DFF])
    wu = E.inp("w_up", [NE, 1024, DFF])
    wd = E.inp("w_down", [NE, DFF, 1024])
    vt, vr = load_small(E, "vecs", VF["NV"])
    E.edger = vr
    selc, selr = load_sel(E)
    xm = E.scratch("xm", [8, 128, TPC]).rearrange("c p w -> p c w")
    x3 = E.scratch("x3", [8, 128, TPC]).rearrange("c p w -> p c w")
    yo = E.out("y", [8, 128, TPC]).rearrange("c p w -> p c w")
    nreg = TPC // 512
    xmr, x3r = P.R(nreg), P.R(nreg)
    modT = P.alloc([48, 2])
    modr = P.R()
    compute_mod(E, adaw, vt, vr, VF["c"], VF["adab3"], modT, modr)
    sc = layer_scalars(E, modT, modr, vt, vr, VF["n1_3"], VF["n2_3"])
    tiles = []
    for t in tile_list(with_ctx=False):
        t = dict(t)
        c0 = t["col"] + 1
        t.update(src=xe_d[:, :, c0:c0 + 542], pos=None, in_regions=[], ocol=t["col"])
        if t["col"] == 0:
            t["ledge"] = vt[:, VF["edge"]:VF["edge"] + 1]
        if t["col"] == TPC - 512:
            t["redge"] = vt[:, VF["edge"] + 1:VF["edge"] + 2]
        t["out_regions"] = col_regions(xmr, t["col"], t["W"])
        tiles.append(t)
    stage_conformer(E, tiles, wpw1, wpw2, vt, vr,
                    (VF["bpw1"], VF["bdw"], VF["lng"], VF["lnb"], VF["bpw2"], VF["wdw"]), sc, xm, None)
    groups = []
    for g in tile_list(with_ctx=False, step=1024):
        g = dict(g)
        g["in_regions"] = col_regions(xmr, g["col"], g["W"])
        g["out_regions"] = col_regions(x3r, g["col"], g["W"])
        groups.append(g)
    stage_ffn(E, groups, xm, sc, wg, wu, wd, x3, moe=dict(wr=wrt, selc=selc, selr=selr))
    ftiles = []
    for t in tile_list(with_ctx=False):
        t = dict(t)
        t["in_regions"] = col_regions(x3r, t["col"], t["W"])
        ftiles.append(t)
    stage_final(E, ftiles, x3, vt, vr, VF["nf"], yo)
    P.finish()
    P.emit()
    return E


def prep_F(inp, x2s):
    cst = host_consts()
    sel = host_sel()
    maps = []
    full = None if x2s is None else [np.concatenate([unchunk_T(x2s[4 * b + seg]) for seg in range(4)], axis=0) for b in range(BATCH)]
    for k in range(NCORE):
        b, seg = k // 4, k % 4
        v = np.zeros((128, VF["NV"]), np.float32)
        v[:, VF["c"]:VF["c"] + 16] = cvec(inp, b)
        v[:, VF["adab3"]:VF["adab3"] + 48] = fm(inp["ada_b"][3])
        v[:, VF["n1_3"]:VF["n1_3"] + 8] = fm(inp["norm1_g"][3])
        v[:, VF["n2_3"]:VF["n2_3"] + 8] = fm(inp["norm2_g"][3])
        v[:, VF["bpw1"]:VF["bpw1"] + 16] = fm(inp["cf_b_pw1"][1])
        v[:, VF["bdw"]:VF["bdw"] + 8] = fm(inp["cf_b_dw"][1])
        v[:, VF["lng"]:VF["lng"] + 8] = fm(inp["cf_ln_g"][1])
        v[:, VF["lnb"]:VF["lnb"] + 8] = fm(inp["cf_ln_b"][1])
        v[:, VF["bpw2"]:VF["bpw2"] + 8] = fm(inp["cf_b_pw2"][1])
        wdw = np.asarray(inp["cf_w_dw"][1], np.float32)
        v[:, VF["wdw"]:VF["wdw"] + 248] = wdw.T.reshape(8, 128, 31).transpose(1, 0, 2).reshape(128, 248)
        v[:, VF["edge"]] = 0.0 if seg == 0 else 1.0
        v[:, VF["edge"] + 1] = 0.0 if seg == 3 else 1.0
        v[:, VF["nf"]:VF["nf"] + 8] = fm(inp["normf_g"])
        maps.append({"consts": cst, "selc": sel, "xe": None if full is None else window_T(full[b], seg * TPC, -16, TPC + 16),
                     "ada_w": np.asarray(inp["ada_w"][3]), "w_pw1": np.asarray(inp["cf_w_pw1"][1]),
                     "w_pw2": np.asarray(inp["cf_w_pw2"][1]), "w_router": np.asarray(inp["moe_w_router"][1]),
                     "w_gate": np.asarray(inp["moe_w_gate"][1]), "w_up": np.asarray(inp["moe_w_up"][1]),
                     "w_down": np.asarray(inp["moe_w_down"][1]), "vecs": v})
    return maps


NCE = TPC + 32
VG = dict(VF)
VG.update(c=0, adab2=402, n2_2=450, NV=458)


def build_EF():
    E = Env("EF")
    P = E.P
    E.consts()
    x1 = E.inp("x1", [8, 128, NCE]).rearrange("c p w -> p c w")
    rT = E.inp("rT", [8, 128, NCE]).rearrange("c p w -> p c w")
    adaw2 = E.inp("ada_w2", [1024, 6144])
    wout = E.inp("w_out", [1024, 1024])
    wg2 = E.inp("w_gate2", [1024, DFF])
    wu2 = E.inp("w_up2", [1024, DFF])
    wd2 = E.inp("w_down2", [DFF, 1024])
    adaw = E.inp("ada_w", [1024, 6144])
    wpw1 = E.inp("w_pw1", [1024, 2048])
    wpw2 = E.inp("w_pw2", [1024, 1024])
    wrt = E.inp("w_router", [1024, 8])
    wg = E.inp("w_gate", [NE, 1024, DFF])
    wu = E.inp("w_up", [NE, 1024, DFF])
    wd = E.inp("w_down", [NE, DFF, 1024])
    vt, vr = load_small(E, "vecs", VG["NV"])
    E.edger = vr
    selc, selr = load_sel(E)
    xm2 = E.scratch("xm2", [8, 128, NCE]).rearrange("c p w -> p c w")
    x2 = E.scratch("x2", [8, 128, NCE]).rearrange("c p w -> p c w")
    xm = E.scratch("xm", [8, 128, TPC]).rearrange("c p w -> p c w")
    x3 = E.scratch("x3", [8, 128, TPC]).rearrange("c p w -> p c w")
    yo = E.out("y", [8, 128, TPC]).rearrange("c p w -> p c w")
    nreg = (NCE + 511) // 512
    xm2r, x2r, xmr, x3r = P.R(nreg), P.R(nreg), P.R(nreg), P.R(nreg)
    modT2 = P.alloc([48, 2])
    modr2 = P.R()
    compute_mod(E, adaw2, vt, vr, VG["c"], VG["adab2"], modT2, modr2)
    sc2 = layer_scalars(E, modT2, modr2, vt, vr, VG["n2_2"], VG["n2_2"])
    tl2 = tile_list(with_ctx=False) + [dict(col=TPC, W=32, j=0)]
    tiles = []
    for t in tl2:
        t = dict(t)
        t["in_regions"] = []
        t["out_regions"] = col_regions(xm2r, t["col"], t["W"])
        tiles.append(t)
    stage_post(E, tiles, x1, rT, sc2, wout, None, xm2)
    groups = []
    for g in [dict(col=0, W=1024, j=0), dict(col=1024, W=1024, j=0), dict(col=2048, W=1024, j=0), dict(col=3072, W=1024 + 32, j=0)]:
        g = dict(g)
        g["in_regions"] = col_regions(xm2r, g["col"], g["W"])
        g["out_regions"] = col_regions(x2r, g["col"], g["W"])
        groups.append(g)
    stage_ffn(E, groups, xm2, sc2, wg2, wu2, wd2, x2)
    modT = P.alloc([48, 2])
    modr = P.R()
    compute_mod(E, adaw, vt, vr, VG["c"], VG["adab3"], modT, modr)
    sc = layer_scalars(E, modT, modr, vt, vr, VG["n1_3"], VG["n2_3"])
    tiles = []
    for t in tile_list(with_ctx=False):
        t = dict(t)
        c = t["col"]
        if c == 0:
            src = [(0, 15, x2[:, :, TPC + 1:TPC + 16]), (15, 527, x2[:, :, 0:527])]
            t["ledge"] = vt[:, VG["edge"]:VG["edge"] + 1]
        elif c == TPC - 512:
            src = [(0, 527, x2[:, :, c - 15:TPC]), (527, 15, x2[:, :, TPC + 16:TPC + 31])]
            t["redge"] = vt[:, VG["edge"] + 1:VG["edge"] + 2]
        else:
            src = x2[:, :, c - 15:c + 527]
        t.update(src=src, pos=None, in_regions=list(x2r), ocol=c)
        t["out_regions"] = col_regions(xmr, c, t["W"])
        tiles.append(t)
    stage_conformer(E, tiles, wpw1, wpw2, vt, vr,
                    (VG["bpw1"], VG["bdw"], VG["lng"], VG["lnb"], VG["bpw2"], VG["wdw"]), sc, xm, None)
    groups = []
    for g in tile_list(with_ctx=False, step=1024):
        g = dict(g)
        g["in_regions"] = col_regions(xmr, g["col"], g["W"])
        g["out_regions"] = col_regions(x3r, g["col"], g["W"])
        groups.append(g)
    stage_ffn(E, groups, xm, sc, wg, wu, wd, x3, moe=dict(wr=wrt, selc=selc, selr=selr))
    ftiles = []
    for t in tile_list(with_ctx=False):
        t = dict(t)
        t["in_regions"] = col_regions(x3r, t["col"], t["W"])
        ftiles.append(t)
    stage_final(E, ftiles, x3, vt, vr, VG["nf"], yo)
    P.finish()
    P.emit()
    return E


def prep_EF(inp, x1s, rTs):
    mF = prep_F(inp, None)
    maps = []
    x1full = [np.concatenate([unchunk_T(x1s[4 * b + seg])[0:TPC] for seg in range(4)], axis=0) for b in range(BATCH)]
    for k in range(NCORE):
        b, seg = k // 4, k % 4
        t0 = seg * TPC
        v = np.zeros((128, VG["NV"]), np.float32)
        v[:, 0:VF["NV"]] = mF[k]["vecs"]
        v[:, VG["adab2"]:VG["adab2"] + 48] = fm(inp["ada_b"][2])
        v[:, VG["n2_2"]:VG["n2_2"] + 8] = fm(inp["norm2_g"][2])
        x1w = window_T(x1full[b], t0, -16, TPC + 16)
        x1e = np.concatenate([x1w[:, :, 16:16 + TPC], x1w[:, :, 0:16], x1w[:, :, 16 + TPC:]], axis=2)
        rw = np.zeros((8, 128, TPC + 32), np.float32)
        lo, hi = max(0, t0 - 16), min(SEQ, t0 + TPC + 16)
        for kk in range(8):
            rw[kk][:, lo - (t0 - 16):hi - (t0 - 16)] = rTs[kk][:, b, lo:hi]
        re_ = np.concatenate([rw[:, :, 16:16 + TPC], rw[:, :, 0:16], rw[:, :, 16 + TPC:]], axis=2)
        m = dict(mF[k])
        m.pop("xe")
        m.update({"x1": np.ascontiguousarray(x1e), "rT": np.ascontiguousarray(re_), "ada_w2": np.asarray(inp["ada_w"][2]),
                  "w_out": np.asarray(inp["hg_w_out"][0]), "w_gate2": np.asarray(inp["ffn_w_gate"][1]),
                  "w_up2": np.asarray(inp["ffn_w_up"][1]), "w_down2": np.asarray(inp["ffn_w_down"][1]), "vecs": v})
        maps.append(m)
    return maps


def _run(E, maps):
    res = run_bass_kernel_spmd(E.nc, maps, core_ids=list(range(NCORE)))
    return res.results


def kernel(**inp):
    inp = {k: np.asarray(v) for k, v in inp.items()}
    ra = _run(build_A(), prep_A(inp))
    x0s = [r["x0"] for r in ra]
    rb = _run(build_B(), prep_B(inp, [r["u_pre"] for r in ra]))
    del ra
    rc_ = _run(build_C(), prep_C(inp, x0s, [r["hz"] for r in rb], [r["hzc"] for r in rb]))
    del rb, x0s
    x1s = [r["x1"] for r in rc_]
    rd = _run(build_D(), prep_D(inp, [r["u2"] for r in rc_]))
    del rc_
    rf = _run(build_EF(), prep_EF(inp, x1s, [r["rT"] for r in rd]))
    del rd, x1s
    out = np.empty((BATCH, SEQ, D), np.float32)
    for k in range(NCORE):
        b, seg = k // 4, k % 4
        out[b, seg * TPC:(seg + 1) * TPC] = unchunk_T(rf[k]["y"])
    return out
```

```python
import math
import contextlib
import numpy as np
import concourse.bass as bass
import concourse.mybir as mybir
from concourse.bass_utils import run_bass_kernel_spmd

F32 = mybir.dt.float32
BF16 = mybir.dt.bfloat16
ALU = mybir.AluOpType
AF = mybir.ActivationFunctionType
AX = mybir.AxisListType

D = 1024
KC = 8
DFF = 3584
FC = 28
SEQ = 16384
BATCH = 2
CTX = 256
NCORE = 8
TPC = 4096
EPS = 1e-6
GRID_W = 64
NE = 8

ENGS = ("sync", "scalar", "vector", "gpsimd", "tensor")
DMA_SLOTS = {"sync": 24, "scalar": 8, "gpsimd": 24}


class Op:
    __slots__ = ("eng", "fn", "waits", "signal", "semkey", "semval", "dma")

    def __init__(self, eng, fn, dma):
        self.eng = eng
        self.fn = fn
        self.waits = []
        self.signal = False
        self.semkey = None
        self.semval = 0
        self.dma = dma


class Region:
    __slots__ = ("w", "rs")

    def __init__(self):
        self.w = None
        self.rs = []


class Prog:
    def __init__(self, nc):
        self.nc = nc
        self.ops = {e: [] for e in ENGS}
        self.stack = contextlib.ExitStack()
        self.dma_slot = {e: [None] * n for e, n in DMA_SLOTS.items()}
        self.dma_rr = {e: 0 for e in DMA_SLOTS}
        self.out_dmas = []
        self.big = self.stack.enter_context(nc.sbuf_tensor("big", [128, SB_WORDS], F32))
        self.off = 0
        self.marks = []
        self.psum = [self.stack.enter_context(nc.psum_tensor(f"ps{i}", [128, 512], F32)) for i in range(8)]
        self.psr = [Region() for _ in range(8)]
        self.ps_i = 0

    def alloc(self, free_shape, dt=F32, parts=128):
        n = int(np.prod(free_shape))
        words = n if dt == F32 else (n + 1) // 2
        words = (words + 7) // 8 * 8
        assert self.off + words <= SB_WORDS, f"SBUF overflow {self.off}+{words}"
        ap = self.big[:, self.off:self.off + words]
        self.off += words
        if dt != F32:
            ap = ap.bitcast(dt)
        ap = ap[:, 0:n]
        if len(free_shape) == 2:
            ap = ap.rearrange("p (a b) -> p a b", a=free_shape[0])
        elif len(free_shape) == 3:
            ap = ap.rearrange("p (a b c) -> p a b c", a=free_shape[0], b=free_shape[1])
        if parts != 128:
            ap = ap[0:parts]
        return ap

    def mark(self):
        self.marks.append(self.off)

    def release(self):
        self.barrier()
        self.off = self.marks.pop()

    def bank(self):
        i = self.ps_i
        self.ps_i = (i + 1) % 8
        return self.psum[i], self.psr[i]

    def R(self, n=None):
        if n is None:
            return Region()
        return [Region() for _ in range(n)]

    def op(self, eng, fn, reads=(), writes=(), dma=False, is_out=False):
        o = Op(eng, fn, dma)
        deps = []
        seen = set()

        def add(d):
            if d is None or id(d) in seen:
                return
            seen.add(id(d))
            deps.append(d)

        for r in reads:
            add(r.w)
        for r in writes:
            add(r.w)
            for x in r.rs:
                add(x)
        if dma:
            slots = self.dma_slot[eng]
            i = self.dma_rr[eng]
            self.dma_rr[eng] = (i + 1) % len(slots)
            add(slots[i])
            o.semkey = (eng, i)
            slots[i] = o
            o.signal = True
        else:
            o.semkey = eng
        for d in deps:
            if d.eng == "tensor" and eng == "tensor" and not d.dma and not dma:
                continue
            if SAME_ENGINE_FREE and d.eng == eng and not d.dma and not dma:
                continue
            d.signal = True
            o.waits.append(d)
        for r in reads:
            if not dma:
                r.rs = [x for x in r.rs if x.dma or x.eng != eng]
            r.rs.append(o)
        for r in writes:
            r.w = o
            r.rs = []
        self.ops[eng].append(o)
        if is_out:
            self.out_dmas.append(o)
        return o

    def dma(self, eng, out, in_, reads=(), writes=(), is_out=False, **kw):
        return self.op(eng, lambda e: e.dma_start(out=out, in_=in_, **kw), reads, writes, dma=True, is_out=is_out)

    def V(self, fn, reads=(), writes=()):
        return self.op("vector", fn, reads, writes)

    def S(self, fn, reads=(), writes=()):
        return self.op("scalar", fn, reads, writes)

    def G(self, fn, reads=(), writes=()):
        return self.op("gpsimd", fn, reads, writes)

    def T(self, fn, reads=(), writes=()):
        return self.op("tensor", fn, reads, writes)

    def mm(self, out, outr, pairs, reads):
        def fn(e):
            n = len(pairs)
            ins = None
            for i, (l, r) in enumerate(pairs):
                ins = e.matmul(out, lhsT=l, rhs=r, start=(i == 0), stop=(i == n - 1))
            return ins
        return self.op("tensor", fn, reads, [outr])

    def barrier(self):
        lasts = []
        for e in ENGS:
            for o in reversed(self.ops[e]):
                if not o.dma and o.fn is not None:
                    lasts.append(o)
                    break
        for e in DMA_SLOTS:
            for o in self.dma_slot[e]:
                if o is not None:
                    lasts.append(o)
        for e in ENGS:
            o = Op(e, None, False)
            o.semkey = e
            for d in lasts:
                if d.eng == e and not d.dma:
                    continue
                d.signal = True
                o.waits.append(d)
            self.ops[e].append(o)

    def finish(self):
        o = Op("sync", None, False)
        o.semkey = "sync"
        o.waits = list(self.out_dmas)
        self.ops["sync"].append(o)

    def emit(self):
        nc = self.nc
        sems = {}
        self.maxcnt = {}
        for e in ENGS:
            cnt = 0
            slotcnt = {}
            for o in self.ops[e]:
                if o.dma:
                    slotcnt[o.semkey] = slotcnt.get(o.semkey, 0) + 16
                    o.semval = slotcnt[o.semkey]
                    sems.setdefault(o.semkey, None)
                elif o.signal:
                    cnt += 1
                    o.semval = cnt
            sems[e] = None
            self.maxcnt[e] = (cnt, len(self.ops[e]))
        for k in list(sems):
            nm = k if isinstance(k, str) else f"d_{k[0]}_{k[1]}"
            sems[k] = self.stack.enter_context(nc.semaphore("s_" + nm))

        for k, h in sems.items():
            nc.sync.sem_clear(h)
        nc.all_engine_barrier()

        def run(e, eng):
            waited = {}
            for o in self.ops[e]:
                for d in o.waits:
                    if waited.get(d.semkey, 0) >= d.semval:
                        continue
                    eng.wait_ge(sems[d.semkey], d.semval)
                    waited[d.semkey] = d.semval
                if o.fn is None:
                    continue
                ins = o.fn(eng)
                if o.dma:
                    ins.then_inc(sems[o.semkey], 16)
                elif o.signal:
                    ins.then_inc(sems[o.semkey], 1)

        with nc.Block() as block:
            @block.sync
            def _(eng):
                run("sync", eng)

            @block.scalar
            def _(eng):
                run("scalar", eng)

            @block.vector
            def _(eng):
                run("vector", eng)

            @block.gpsimd
            def _(eng):
                run("gpsimd", eng)

            @block.tensor
            def _(eng):
                run("tensor", eng)
        self.stack.close()


SAME_ENGINE_FREE = False
SB_WORDS = 49152


class Env:
    def __init__(self, name):
        self.nc = bass.Bass("TRN2", target_bir_lowering=False)
        self.P = Prog(self.nc)
        self.ins = {}
        self.outs = {}

    def inp(self, name, shape, dt=F32):
        t = self.nc.dram_tensor(name, list(shape), dt, kind="ExternalInput").ap()
        self.ins[name] = t
        return t

    def out(self, name, shape, dt=F32):
        t = self.nc.dram_tensor(name, list(shape), dt, kind="ExternalOutput").ap()
        self.outs[name] = t
        return t

    def scratch(self, name, shape, dt=F32):
        return self.nc.dram_tensor(name, list(shape), dt).ap()

    def consts(self):
        P = self.P
        cin = self.inp("consts", [128, 384])
        self.cf = P.alloc([384])
        self.cr = P.R()
        P.dma("sync", self.cf, cin, writes=[self.cr])
        self.ident = self.cf[:, 0:128]
        self.ones = self.cf[:, 128:256]
        self.J = self.cf[:, 256:384]
        cb = P.alloc([256], BF16)
        P.V(lambda e: e.tensor_copy(out=cb, in_=self.cf[:, 0:256]), [self.cr], [self.cr])
        self.ident_b = cb[:, 0:128]
        self.ones_b = cb[:, 128:256]


def host_consts():
    return np.concatenate([np.eye(128, dtype=np.float32), np.ones((128, 128), np.float32),
                           np.eye(128, dtype=np.float32)[::-1]], axis=1)


def fm(v):
    v = np.asarray(v, np.float32)
    return np.ascontiguousarray(v.reshape(-1, 128).T)


def load_small(E, name, ncols):
    P = E.P
    d = E.inp(name, [128, ncols])
    t = P.alloc([ncols])
    r = P.R()
    P.dma("sync", t, d, writes=[r])
    return t, r


def load_w_bf16(E, dst, dst_r, src_ap, reads=()):
    return E.P.dma("gpsimd", dst, src_ap, reads=reads, writes=[dst_r])


def compute_mod(E, adaw, vt, vr, col_c, col_adab, modT, modr):
    P = E.P
    P.mark()
    sc = P.alloc([16])
    scr = P.R()
    P.S(lambda e: e.activation(out=sc, in_=vt[:, col_c:col_c + 16], func=AF.Silu), [vr], [scr])
    sc3 = sc.rearrange("p (k j) -> p k j", j=2)
    wb = [P.alloc([8, 384]) for _ in range(2)]
    wr = P.R(2)
    ps, psr = P.bank()
    adv = adaw.rearrange("(k p) n -> p k n", p=128)
    for mb in range(16):
        i = mb % 2
        P.dma("sync", wb[i], adv[:, :, mb * 384:(mb + 1) * 384], writes=[wr[i]])
        for j in range(3):
            m = mb * 3 + j
            P.mm(ps[:, m * 2:m * 2 + 2], psr,
                 [(wb[i][:, k, j * 128:(j + 1) * 128], sc3[:, k, :]) for k in range(8)], [wr[i], scr])
    ps3 = ps[:, 0:96].rearrange("p (m j) -> p m j", j=2)
    for j in range(2):
        P.V(lambda e, j=j: e.tensor_tensor(out=modT[:, :, j], in0=ps3[:, :, j], in1=vt[:, col_adab:col_adab + 48],
                                           op=ALU.add), [psr, vr], [modr])
    P.release()


def rms_ctx(E, Wmax):
    P = E.P
    return dict(sq=P.alloc([8, Wmax], BF16), sqr=P.R(), rstd=P.alloc([Wmax]), rr=P.R(),
                tmp=[P.alloc([Wmax]) for _ in range(2)], tr=P.R(2))


def rms_mod(E, rc, x, xr, W, gsc, sh, jcol, scalr, out, outr, out_f32=None, out_f32_r=None):
    P = E.P
    sq, sqr, rr, tr = rc["sq"], rc["sqr"], rc["rr"], rc["tr"]
    rstd = rc["rstd"][:, 0:W]
    tmp = [t[:, 0:W] for t in rc["tmp"]]
    for c in range(8):
        P.S(lambda e, c=c: e.activation(out=sq[:, c, 0:W], in_=x[:, c, 0:W], func=AF.Square), [xr], [sqr])
    for a in range(0, W, 512):
        b = min(a + 512, W)
        ps, psr = P.bank()
        P.mm(ps[:, 0:b - a], psr, [(E.ones_b, sq[:, c, a:b]) for c in range(8)], [sqr, E.cr])
        P.V(lambda e, a=a, b=b, ps=ps: e.tensor_scalar(out=rstd[:, a:b], in0=ps[:, 0:b - a], scalar1=float(D * EPS),
                                                       scalar2=None, op0=ALU.add), [psr], [rr])
        P.S(lambda e, a=a, b=b: e.sqrt(out=rstd[:, a:b], in_=rstd[:, a:b]), [rr], [rr])
        P.V(lambda e, a=a, b=b: e.reciprocal(out=rstd[:, a:b], in_=rstd[:, a:b]), [rr], [rr])
    for c in range(8):
        i = c % 2
        P.V(lambda e, c=c, i=i: e.tensor_tensor(out=tmp[i], in0=x[:, c, 0:W], in1=rstd, op=ALU.mult),
            [xr, rr], [tr[i]])
        P.S(lambda e, c=c, i=i: e.activation(out=out[:, c, 0:W], in_=tmp[i], func=AF.Identity,
                                             bias=sh[:, c, jcol:jcol + 1], scale=gsc[:, c, jcol:jcol + 1]),
            [tr[i], scalr], [outr])
        if out_f32 is not None:
            P.V(lambda e, c=c, i=i: e.tensor_scalar(out=out_f32[:, c, 0:W], in0=tmp[i],
                                                    scalar1=gsc[:, c, jcol:jcol + 1], scalar2=sh[:, c, jcol:jcol + 1],
                                                    op0=ALU.mult, op1=ALU.add),
                [tr[i], scalr], [out_f32_r])


def layer_scalars(E, modT, modr, vt, vr, col_n1, col_n2):
    P = E.P
    sc = {}
    r = P.R()
    m4 = modT.rearrange("p (s c) j -> p s c j", s=6)
    for nm, si, col in (("gsc1", 1, col_n1), ("gsc2", 4, col_n2)):
        t = P.alloc([8, 2])
        for j in range(2):
            P.V(lambda e, t=t, j=j, si=si, col=col: e.scalar_tensor_tensor(
                out=t[:, :, j], in0=m4[:, si, :, j], scalar=1.0, in1=vt[:, col:col + 8], op0=ALU.add, op1=ALU.mult),
                [modr, vr], [r])
        P.V(lambda e, t=t: e.tensor_scalar(out=t, in0=t, scalar1=float(math.sqrt(D)), scalar2=None, op0=ALU.mult),
            [r], [r])
        sc[nm] = t
    sc["sh1"] = m4[:, 0]
    sc["g1"] = m4[:, 2]
    sc["sh2"] = m4[:, 3]
    sc["g2"] = m4[:, 5]
    sc["r"] = r
    sc["modr"] = modr
    return sc


def stage_conformer(E, tiles, w_pw1, w_pw2, vt, vr, cols, sc, xout, xout_r):
    P = E.P
    P.mark()
    c_bpw1, c_bdw, c_lng, c_lnb, c_bpw2, c_wdw = cols
    w1 = P.alloc([8, 2048], BF16)
    w2 = P.alloc([8, 1024], BF16)
    w1r, w2r = P.R(), P.R()
    w1v = w_pw1.rearrange("(k p) n -> p k n", p=128)
    for k in range(8):
        P.dma("gpsimd", w1[:, k, :], w1v[:, k, :], writes=[w1r])
    P.dma("gpsimd", w2, w_pw2.rearrange("(k p) n -> p k n", p=128), writes=[w2r])
    dgb = [P.alloc([31, 128], BF16) for _ in range(2)]
    dgrs = P.R(2)
    g1b = P.alloc([8, 2])
    g1br = P.R()
    for j in range(2):
        P.V(lambda e, j=j: e.tensor_tensor(out=g1b[:, :, j], in0=sc["g1"][:, :, j], in1=vt[:, c_bpw2:c_bpw2 + 8],
                                           op=ALU.mult), [sc["modr"], vr], [g1br])
    rc = rms_ctx(E, 542)
    xe = P.alloc([8, 542])
    xer = P.R()
    vv = P.alloc([2 * 8 * 512])
    vvr = P.R()
    pe = vv[:, 0:8 * 542].rearrange("p (a b) -> p a b", a=8)
    per = vvr
    v = vv[:, 0:4096].rearrange("p (a b) -> p a b", a=8)
    v2 = vv[:, 4096:8192].rearrange("p (a b) -> p a b", a=8)
    v2r = vvr
    xo = v2
    xor_ = vvr
    he = P.alloc([8, 542], BF16)
    her = P.R()
    act = he
    actr = her
    ue = P.alloc([8, 542], BF16)
    uer = P.R()
    sg = [P.alloc([512]) for _ in range(2)]
    sgr = P.R(2)
    st = P.alloc([4, 512])
    str_ = P.R()
    tmp = [P.alloc([512]) for _ in range(2)]
    tmr = P.R(2)
    def tile_body(t):
        W = t["W"]
        We = W + 30
        j = t["j"]
        if t.get("zero_halo"):
            P.V(lambda e: e.memset(xe[:, :, :], 0.0), [], [xer])
            P.dma("sync", xe[:, :, 15:15 + W], t["src"], reads=t["in_regions"], writes=[xer])
        else:
            if isinstance(t["src"], list):
                for (o_, w_, ap_) in t["src"]:
                    P.dma("sync", xe[:, :, o_:o_ + w_], ap_, reads=t["in_regions"], writes=[xer])
            else:
                P.dma("sync", xe[:, :, 0:We], t["src"], reads=t["in_regions"], writes=[xer])
        if t.get("pos") is not None:
            P.dma("scalar", pe[:, :, 0:We], t["pos"], writes=[per])
            P.V(lambda e, We=We: e.tensor_tensor(out=xe[:, :, 0:We], in0=xe[:, :, 0:We], in1=pe[:, :, 0:We], op=ALU.add),
                [xer, per], [xer])
        if getattr(E, "dbgx", None) is not None and t["col"] == 512:
            P.dma("sync", E.dbgx[:, 0:8, 0:We], xe[:, :, 0:We], reads=[xer], is_out=True)
        rms_mod(E, rc, xe, xer, We, sc["gsc1"], sc["sh1"], j, sc["r"], he, her)
        if getattr(E, "dbgx", None) is not None and t["col"] == 512:
            P.dma("gpsimd", E.dbgx[:, 8:16, 0:We], he[:, :, 0:We], reads=[her], is_out=True)
        pieces = [(0, min(We, 512))] + ([(512, We)] if We > 512 else [])
        for c in range(8):
            for (a, b) in pieces:
                psa, psar = P.bank()
                psg, psgr = P.bank()
                P.mm(psa[:, 0:b - a], psar, [(w1[:, k, c * 128:(c + 1) * 128], he[:, k, a:b]) for k in range(8)], [w1r, her])
                P.mm(psg[:, 0:b - a], psgr, [(w1[:, k, 1024 + c * 128:1024 + (c + 1) * 128], he[:, k, a:b]) for k in range(8)], [w1r, her])
                i = c % 2
                P.S(lambda e, c=c, a=a, b=b, i=i, psg=psg: e.activation(
                    out=sg[i][:, 0:b - a], in_=psg[:, 0:b - a], func=AF.Sigmoid,
                    bias=vt[:, c_bpw1 + 8 + c:c_bpw1 + 9 + c], scale=1.0), [psgr, vr], [sgr[i]])
                P.V(lambda e, c=c, a=a, b=b, i=i, psa=psa: e.scalar_tensor_tensor(
                    out=ue[:, c, a:b], in0=psa[:, 0:b - a], scalar=vt[:, c_bpw1 + c:c_bpw1 + c + 1], in1=sg[i][:, 0:b - a],
                    op0=ALU.add, op1=ALU.mult), [psar, vr, sgr[i]], [uer])
        for (edge, lo, hi) in ((t.get("ledge"), 0, 15), (t.get("redge"), We - 15, We)):
            if edge is None:
                continue
            if isinstance(edge, float):
                P.V(lambda e, lo=lo, hi=hi: e.memset(ue[:, :, lo:hi], 0.0), [], [uer])
            else:
                P.V(lambda e, lo=lo, hi=hi, edge=edge: e.tensor_scalar(
                    out=ue[:, :, lo:hi], in0=ue[:, :, lo:hi], scalar1=edge, scalar2=None, op0=ALU.mult),
                    [uer, E.edger], [uer])
        for c in range(8):
            di = c % 2
            dg, dgr = dgb[di], dgrs[di]
            for k in range(31):
                P.V(lambda e, c=c, k=k, dg=dg: e.tensor_scalar(out=dg[:, k, :], in0=E.ident,
                                                               scalar1=vt[:, c_wdw + c * 31 + k:c_wdw + c * 31 + k + 1],
                                                               scalar2=None, op0=ALU.mult), [E.cr, vr], [dgr])
            ps, psr = P.bank()
            P.mm(ps[:, 0:W], psr, [(dg[:, k, :], ue[:, c, k:k + W]) for k in range(31)], [dgr, uer])
            P.S(lambda e, c=c, ps=ps: e.activation(out=v[:, c, 0:W], in_=ps[:, 0:W], func=AF.Identity,
                                                   bias=vt[:, c_bdw + c:c_bdw + c + 1], scale=1.0), [psr, vr], [vvr])
            P.V(lambda e, c=c: e.tensor_tensor(out=v2[:, c, 0:W], in0=v[:, c, 0:W], in1=v[:, c, 0:W], op=ALU.mult),
                [vvr], [v2r])
        if getattr(E, "dbgx", None) is not None and t["col"] == 512:
            P.dma("gpsimd", E.dbgx[:, 16:24, 0:We], ue[:, :, 0:We], reads=[uer], is_out=True)
            P.dma("sync", E.dbgx[:, 24:32, 0:W], v[:, :, 0:W], reads=[vvr], is_out=True)
        ps1, ps1r = P.bank()
        ps2, ps2r = P.bank()
        P.mm(ps1[:, 0:W], ps1r, [(E.ones, v[:, c, 0:W]) for c in range(8)], [E.cr, vvr])
        P.mm(ps2[:, 0:W], ps2r, [(E.ones, v2[:, c, 0:W]) for c in range(8)], [E.cr, v2r])
        mean, var, rstd, nmr = st[:, 0, 0:W], st[:, 1, 0:W], st[:, 2, 0:W], st[:, 3, 0:W]
        P.V(lambda e: e.tensor_scalar(out=mean, in0=ps1[:, 0:W], scalar1=1.0 / D, scalar2=None, op0=ALU.mult), [ps1r], [str_])
        P.V(lambda e: e.tensor_tensor(out=var, in0=mean, in1=mean, op=ALU.mult), [str_], [str_])
        P.V(lambda e: e.scalar_tensor_tensor(out=var, in0=ps2[:, 0:W], scalar=1.0 / D, in1=var, op0=ALU.mult, op1=ALU.subtract),
            [ps2r, str_], [str_])
        P.V(lambda e: e.tensor_scalar(out=rstd, in0=var, scalar1=float(EPS), scalar2=None, op0=ALU.add), [str_], [str_])
        P.S(lambda e: e.sqrt(out=rstd, in_=rstd), [str_], [str_])
        P.V(lambda e: e.reciprocal(out=rstd, in_=rstd), [str_], [str_])
        for c in range(8):
            i = c % 2
            P.V(lambda e, c=c, i=i: e.tensor_tensor(out=tmp[i][:, 0:W], in0=v[:, c, 0:W], in1=mean, op=ALU.subtract),
                [vvr, str_], [tmr[i]])
            P.V(lambda e, c=c, i=i: e.tensor_tensor(out=tmp[i][:, 0:W], in0=tmp[i][:, 0:W], in1=rstd, op=ALU.mult),
                [str_, tmr[i]], [tmr[i]])
            P.S(lambda e, c=c, i=i: e.activation(out=act[:, c, 0:W], in_=tmp[i][:, 0:W], func=AF.Silu,
                                                 bias=vt[:, c_lnb + c:c_lnb + c + 1], scale=vt[:, c_lng + c:c_lng + c + 1]),
                [tmr[i], vr], [actr])
        if getattr(E, "dbgx", None) is not None and t["col"] == 512:
            P.dma("gpsimd", E.dbgx[:, 32:40, 0:W], act[:, :, 0:W], reads=[actr], is_out=True)
            P.dma("sync", E.dbgx[:, 40:44, 0:W], st[:, :, 0:W], reads=[str_], is_out=True)
        for c in range(8):
            ps, psr = P.bank()
            P.mm(ps[:, 0:W], psr, [(w2[:, k, c * 128:(c + 1) * 128], act[:, k, 0:W]) for k in range(8)], [w2r, actr])
            P.V(lambda e, c=c, ps=ps: e.scalar_tensor_tensor(
                out=xo[:, c, 0:W], in0=ps[:, 0:W], scalar=sc["g1"][:, c, j:j + 1], in1=xe[:, c, 15:15 + W],
                op0=ALU.mult, op1=ALU.add), [psr, sc["modr"], xer], [xor_])
            P.S(lambda e, c=c: e.activation(out=xo[:, c, 0:W], in_=xo[:, c, 0:W], func=AF.Identity,
                                            bias=g1b[:, c, j:j + 1], scale=1.0), [xor_, g1br], [xor_])
        P.dma("sync", xout[:, :, t["ocol"]:t["ocol"] + W], xo[:, :, 0:W], reads=[xor_], writes=t["out_regions"])
    for t in tiles:
        tile_body(t)
    P.release()


def stage_ffn(E, groups, xin, sc, wg, wu, wd, xout, moe=None):
    P = E.P
    P.mark()
    GW = max(g["W"] for g in groups)
    x = P.alloc([8, GW])
    xr = P.R()
    tok = P.alloc([8, GW], BF16)
    tokr = P.R()
    h = P.alloc([FC, GW], BF16)
    hr = P.R()
    rc = dict(sq=h[:, 0:8, :], sqr=hr, rstd=P.alloc([GW]), rr=P.R(), tmp=[P.alloc([GW]) for _ in range(2)], tr=P.R(2))
    wgb = [P.alloc([8, 512], BF16) for _ in range(2)]
    wub = [P.alloc([8, 512], BF16) for _ in range(2)]
    wgr, wur = P.R(2), P.R(2)
    wdb = [P.alloc([FC, 128], BF16) for _ in range(2)]
    wdr = P.R(2)
    sgt = [P.alloc([512]) for _ in range(2)]
    sgr = P.R(2)
    nexp = 1
    if moe is not None:
        nexp = NE
        tokf = h[:, 8:24, :].rearrange("p a b -> p (a b)").bitcast(F32).rearrange("p (a b) -> p a b", a=8)
        wrt = P.alloc([8, 8])
        wrr = P.R()
        P.dma("sync", wrt, moe["wr"].rearrange("(k p) n -> p k n", p=128), writes=[wrr])
        nbm = GW // 128
        lg = P.alloc([nbm, 8])
        l2 = P.alloc([nbm, 8])
        sp = P.alloc([nbm, 8])
        sm = P.alloc([4, nbm])
        lgr = P.R()
        gT = P.alloc([GW], parts=8)
        gTr = P.R()
        gb = [P.alloc([GW])] * 2
        gbr = [P.R()] * 2
        sg2 = [P.alloc([512]) for _ in range(2)]
        sg2r = P.R(2)
    wgv = wg.rearrange("e (k p) n -> e p k n", p=128) if moe else wg.rearrange("(k p) n -> p k n", p=128)
    wuv = wu.rearrange("e (k p) n -> e p k n", p=128) if moe else wu.rearrange("(k p) n -> p k n", p=128)
    wdv = wd.rearrange("e (f p) n -> e p f n", p=128) if moe else wd.rearrange("(f p) n -> p f n", p=128)
    ld = 0
    def group_body(g):
        nonlocal ld
        W, j, col = g["W"], g["j"], g["col"]
        subs = [(a, min(a + 512, W)) for a in range(0, W, 512)]
        P.dma("sync", x[:, :, 0:W], xin[:, :, col:col + W], reads=g["in_regions"], writes=[xr])
        if moe is None:
            rms_mod(E, rc, x, xr, W, sc["gsc2"], sc["sh2"], j, sc["r"], tok, tokr)
        else:
            rms_mod(E, rc, x, xr, W, sc["gsc2"], sc["sh2"], j, sc["r"], tok, tokr, out_f32=tokf, out_f32_r=hr)
            nb = W // 128
            psl, pslr = P.bank()
            for tb in range(nb):
                P.mm(psl[:, tb * 8:(tb + 1) * 8], pslr,
                     [(tokf[:, k, tb * 128:(tb + 1) * 128], wrt[:, k, :]) for k in range(8)], [hr, wrr])
            m1, m2, nm1, den = sm[:, 0, 0:nb], sm[:, 1, 0:nb], sm[:, 2, 0:nb], sm[:, 3, 0:nb]
            P.V(lambda e: e.tensor_copy(out=lg[:, 0:nb, :], in_=psl[:, 0:nb * 8].rearrange("p (a b) -> p a b", b=8)),
                [pslr], [lgr])
            P.V(lambda e: e.tensor_reduce(out=m1, in_=lg[:, 0:nb, :], axis=AX.X, op=ALU.max), [lgr], [lgr])
            P.V(lambda e: e.tensor_scalar(out=nm1, in0=m1, scalar1=-1.0, scalar2=None, op0=ALU.mult), [lgr], [lgr])
            for tb in range(nb):
                P.V(lambda e, tb=tb: e.tensor_scalar(out=l2[:, tb, :], in0=lg[:, tb, :], scalar1=sm[:, 0, tb:tb + 1],
                                                     scalar2=-1e30, op0=ALU.is_equal, op1=ALU.mult), [lgr], [lgr])
            P.V(lambda e: e.tensor_tensor(out=l2[:, 0:nb, :], in0=l2[:, 0:nb, :], in1=lg[:, 0:nb, :], op=ALU.add), [lgr], [lgr])
            P.V(lambda e: e.tensor_reduce(out=m2, in_=l2[:, 0:nb, :], axis=AX.X, op=ALU.max), [lgr], [lgr])
            for tb in range(nb):
                P.S(lambda e, tb=tb: e.activation(out=sp[:, tb, :], in_=lg[:, tb, :], func=AF.Exp,
                                                  bias=sm[:, 2, tb:tb + 1], scale=1.0), [lgr], [lgr])
                P.V(lambda e, tb=tb: e.scalar_tensor_tensor(out=sp[:, tb, :], in0=lg[:, tb, :], scalar=sm[:, 1, tb:tb + 1],
                                                            in1=sp[:, tb, :], op0=ALU.is_ge, op1=ALU.mult), [lgr], [lgr])
            P.V(lambda e: e.tensor_reduce(out=den, in_=sp[:, 0:nb, :], axis=AX.X, op=ALU.add), [lgr], [lgr])
            P.V(lambda e: e.reciprocal(out=den, in_=den), [lgr], [lgr])
            for tb in range(nb):
                P.V(lambda e, tb=tb: e.tensor_scalar(out=sp[:, tb, :], in0=sp[:, tb, :], scalar1=sm[:, 3, tb:tb + 1],
                                                     scalar2=None, op0=ALU.mult), [lgr], [lgr])
            pst, pstr = P.bank()
            pst2, pst2r = P.bank()
            for tb in range(nb):
                pp, ppr = (pst, pstr) if tb < 4 else (pst2, pst2r)
                o = (tb % 4) * 128
                P.mm(pp[0:8, o:o + 128], ppr, [(sp[:, tb, :], E.ident)], [lgr, E.cr])
            P.S(lambda e: e.copy(out=gT[:, 0:min(W, 512)], in_=pst[0:8, 0:min(W, 512)]), [pstr], [gTr])
            if W > 512:
                P.S(lambda e: e.copy(out=gT[:, 512:W], in_=pst2[0:8, 0:W - 512]), [pst2r], [gTr])
        for ex in range(nexp):
            if moe is not None:
                gi = ex % 2
                for (a, b) in subs:
                    ps, psr = P.bank()
                    P.mm(ps[:, 0:b - a], psr, [(moe["selc"][:, ex * 128:(ex + 1) * 128], gT[:, a:b])], [moe["selr"], gTr])
                    P.S(lambda e, a=a, b=b, ps=ps, gi=gi: e.copy(out=gb[gi][:, a:b], in_=ps[:, 0:b - a]), [psr], [gbr[gi]])
            for fb in range(7):
                i = ld % 2
                ld += 1
                srcg = wgv[ex][:, :, fb * 512:(fb + 1) * 512] if moe else wgv[:, :, fb * 512:(fb + 1) * 512]
                srcu = wuv[ex][:, :, fb * 512:(fb + 1) * 512] if moe else wuv[:, :, fb * 512:(fb + 1) * 512]
                P.dma("gpsimd", wgb[i], srcg, writes=[wgr[i]])
                P.dma("gpsimd", wub[i], srcu, writes=[wur[i]])
                for (a, b) in subs:
                    for fc in range(4):
                        f = fb * 4 + fc
                        psg, psgr = P.bank()
                        psu, psur = P.bank()
                        P.mm(psg[:, 0:b - a], psgr, [(wgb[i][:, k, fc * 128:(fc + 1) * 128], tok[:, k, a:b]) for k in range(8)],
                             [wgr[i], tokr])
                        P.mm(psu[:, 0:b - a], psur, [(wub[i][:, k, fc * 128:(fc + 1) * 128], tok[:, k, a:b]) for k in range(8)],
                             [wur[i], tokr])
                        si = f % 2
                        P.S(lambda e, psg=psg, si=si, a=a, b=b: e.activation(out=sgt[si][:, 0:b - a], in_=psg[:, 0:b - a],
                                                                             func=AF.Silu), [psgr], [sgr[si]])
                        if moe is None:
                            P.V(lambda e, psu=psu, si=si, a=a, b=b, f=f: e.tensor_tensor(
                                out=h[:, f, a:b], in0=sgt[si][:, 0:b - a], in1=psu[:, 0:b - a], op=ALU.mult),
                                [psur, sgr[si]], [hr])
                        else:
                            P.V(lambda e, si=si, a=a, b=b, gi=gi: e.tensor_tensor(
                                out=sg2[si][:, 0:b - a], in0=sgt[si][:, 0:b - a], in1=gb[gi][:, a:b], op=ALU.mult),
                                [sgr[si], gbr[gi]], [sg2r[si]])
                            P.V(lambda e, psu=psu, si=si, a=a, b=b, f=f: e.tensor_tensor(
                                out=h[:, f, a:b], in0=sg2[si][:, 0:b - a], in1=psu[:, 0:b - a], op=ALU.mult),
                                [psur, sg2r[si]], [hr])
            for d in range(8):
                i = ld % 2
                ld += 1
                srcd = wdv[ex][:, :, d * 128:(d + 1) * 128] if moe else wdv[:, :, d * 128:(d + 1) * 128]
                P.dma("gpsimd", wdb[i], srcd, writes=[wdr[i]])
                for (a, b) in subs:
                    ps, psr = P.bank()
                    P.mm(ps[:, 0:b - a], psr, [(wdb[i][:, f, :], h[:, f, a:b]) for f in range(FC)], [wdr[i], hr])
                    P.V(lambda e, ps=ps, a=a, b=b, d=d: e.scalar_tensor_tensor(
                        out=x[:, d, a:b], in0=ps[:, 0:b - a], scalar=sc["g2"][:, d, j:j + 1], in1=x[:, d, a:b],
                        op0=ALU.mult, op1=ALU.add), [psr, sc["modr"], xr], [xr])
        P.dma("sync", xout[:, :, col:col + W], x[:, :, 0:W], reads=[xr], writes=g["out_regions"], is_out=g.get("is_out", False))
    for g in groups:
        group_body(g)
    P.release()


def stage_pre(E, tiles, xin, sc, w_in, n_out, bias, uout):
    P = E.P
    P.mark()
    w = P.alloc([8, n_out * 128], BF16)
    wr = P.R()
    wv = w_in.rearrange("(k p) n -> p k n", p=128)
    for k in range(8):
        P.dma("gpsimd", w[:, k, :], wv[:, k, :], writes=[wr])
    rc = rms_ctx(E, 512)
    x = P.alloc([8, 512])
    xr = P.R()
    hb = P.alloc([8, 512], BF16)
    hbr = P.R()
    ob = [P.alloc([8, 512]) for _ in range(2)]
    obr = P.R(2)
    n8 = 0
    def tile_body(t):
        nonlocal n8
        W, j, col = t["W"], t["j"], t["col"]
        P.dma("sync", x[:, :, 0:W], xin[:, :, col:col + W], reads=t["in_regions"], writes=[xr])
        rms_mod(E, rc, x, xr, W, sc["gsc1"], sc["sh1"], j, sc["r"], hb, hbr)
        for o8 in range(n_out // 8):
            i = n8 % 2
            n8 += 1
            for oo in range(8):
                oc = o8 * 8 + oo
                ps, psr = P.bank()
                P.mm(ps[:, 0:W], psr, [(w[:, k, oc * 128:(oc + 1) * 128], hb[:, k, 0:W]) for k in range(8)], [wr, hbr])
                if bias is not None:
                    bvt, bcol, bvr = bias
                    P.S(lambda e, ps=ps, oc=oc, oo=oo, i=i: e.activation(out=ob[i][:, oo, 0:W], in_=ps[:, 0:W], func=AF.Identity,
                                                                         bias=bvt[:, bcol + oc:bcol + oc + 1], scale=1.0),
                        [psr, bvr], [obr[i]])
                elif oo % 2 == 0:
                    P.S(lambda e, ps=ps, oo=oo, i=i: e.copy(out=ob[i][:, oo, 0:W], in_=ps[:, 0:W]), [psr], [obr[i]])
                else:
                    P.V(lambda e, ps=ps, oo=oo, i=i: e.tensor_copy(out=ob[i][:, oo, 0:W], in_=ps[:, 0:W]), [psr], [obr[i]])
            P.dma("sync", uout[:, o8 * 8:(o8 + 1) * 8, col:col + W], ob[i][:, :, 0:W], reads=[obr[i]], writes=[], is_out=True)
    for t in tiles:
        tile_body(t)
    P.release()


def tile_list(ncols_lat=TPC, with_ctx=True, step=512):
    tl = [dict(col=c, W=min(step, ncols_lat - c), j=0) for c in range(0, ncols_lat, step)]
    if with_ctx:
        tl.append(dict(col=ncols_lat, W=CTX, j=1))
    return tl


NCA = TPC + CTX


def col_regions(regs, col, W):
    return regs[col // 512:(col + W + 511) // 512]


VA = dict(c=0, adab0=16, adab1=64, n1_0=112, n2_0=120, n1_1=128, bpw1=136, bdw=152, lng=160, lnb=168, bpw2=176,
          wdw=184, hyb=432, edge=456, NV=458)


def build_A(debug=False):
    E = Env("A")
    P = E.P
    E.consts()
    xe_d = E.inp("xe", [8, 128, TPC + 32]).rearrange("c p w -> p c w")
    pos_d = E.inp("pos", [8, 128, TPC + 32]).rearrange("c p w -> p c w")
    ctx_d = E.inp("ctxT", [8, 128, CTX]).rearrange("c p w -> p c w")
    adaw = E.inp("ada_w", [2, 1024, 6144])
    wpw1 = E.inp("w_pw1", [1024, 2048])
    wpw2 = E.inp("w_pw2", [1024, 1024])
    wg = E.inp("w_gate", [1024, DFF])
    wu = E.inp("w_up", [1024, DFF])
    wd = E.inp("w_down", [DFF, 1024])
    win = E.inp("w_in", [1024, 3072])
    vt, vr = load_small(E, "vecs", VA["NV"])
    E.edger = vr
    xm = (E.out("xm", [8, 128, NCA]) if debug else E.scratch("xm", [8, 128, NCA])).rearrange("c p w -> p c w")
    x0 = (E.out("x0", [8, 128, NCA]) if True else E.scratch("x0", [8, 128, NCA])).rearrange("c p w -> p c w")
    uo = E.out("u_pre", [24, 128, NCA]).rearrange("c p w -> p c w")
    nreg = (NCA + 511) // 512
    xmr, x0r = P.R(nreg), P.R(nreg)
    modT = P.alloc([48, 2])
    modr = P.R()
    compute_mod(E, adaw[0], vt, vr, VA["c"], VA["adab0"], modT, modr)
    sc0 = layer_scalars(E, modT, modr, vt, vr, VA["n1_0"], VA["n2_0"])
    if debug:
        dbg = E.out("dbg", [128, 128])
        E.dbgx = E.out("dbgx", [128, 44, 542])
        P.dma("sync", dbg[:, 0:96], modT.rearrange("p a b -> p (a b)"), reads=[modr], is_out=True)
        P.dma("sync", dbg[:, 96:112], sc0["gsc1"].rearrange("p a b -> p (a b)"), reads=[sc0["r"]], is_out=True)
        P.dma("sync", dbg[:, 112:128], sc0["gsc2"].rearrange("p a b -> p (a b)"), reads=[sc0["r"]], is_out=True)
    tiles = []
    for t in tile_list():
        t = dict(t)
        if t["j"] == 0:
            c0 = t["col"] + 1
            t.update(src=xe_d[:, :, c0:c0 + 542], pos=pos_d[:, :, c0:c0 + 542], in_regions=[], ocol=t["col"])
            if t["col"] == 0:
                t["ledge"] = vt[:, VA["edge"]:VA["edge"] + 1]
            if t["col"] == TPC - 512:
                t["redge"] = vt[:, VA["edge"] + 1:VA["edge"] + 2]
        else:
            t.update(src=ctx_d, pos=None, in_regions=[], ocol=t["col"], zero_halo=True, ledge=0.0, redge=0.0)
        t["out_regions"] = col_regions(xmr, t["col"], t["W"])
        tiles.append(t)
    stage_conformer(E, tiles, wpw1, wpw2, vt, vr,
                    (VA["bpw1"], VA["bdw"], VA["lng"], VA["lnb"], VA["bpw2"], VA["wdw"]), sc0, xm, None)
    groups = []
    for g in tile_list(step=1024):
        g = dict(g)
        g["in_regions"] = col_regions(xmr, g["col"], g["W"])
        g["out_regions"] = col_regions(x0r, g["col"], g["W"])
        g["is_out"] = True
        groups.append(g)
    stage_ffn(E, groups, xm, sc0, wg, wu, wd, x0)
    modT1 = P.alloc([48, 2])
    modr1 = P.R()
    compute_mod(E, adaw[1], vt, vr, VA["c"], VA["adab1"], modT1, modr1)
    sc1 = layer_scalars(E, modT1, modr1, vt, vr, VA["n1_1"], VA["n1_1"])
    ptiles = []
    for t in tile_list():
        t = dict(t)
        t["in_regions"] = col_regions(x0r, t["col"], t["W"])
        ptiles.append(t)
    stage_pre(E, ptiles, x0, sc1, win, 24, (vt, VA["hyb"], vr), uo)
    P.finish()
    P.emit()
    return E


def pos_table():
    quarter = D // 4
    omega = (1.0 / (10000.0 ** (np.arange(quarter, dtype=np.float32) / np.float32(quarter)))).astype(np.float32)
    t = np.arange(SEQ)
    r = (t // GRID_W).astype(np.float32)[:, None] * omega
    col = (t % GRID_W).astype(np.float32)[:, None] * omega
    return np.concatenate([np.sin(r), np.cos(r), np.sin(col), np.cos(col)], axis=-1).astype(np.float32)


def chunked_T(a):
    T, C = a.shape
    return np.ascontiguousarray(a.T.reshape(C // 128, 128, T))


def unchunk_T(a):
    n, p, T = a.shape
    return np.ascontiguousarray(a.reshape(n * p, T).T)


def window_T(a, t0, lo, hi):
    S, C = a.shape
    out = np.zeros((hi - lo, C), np.float32)
    s0, s1 = max(0, t0 + lo), min(S, t0 + hi)
    out[s0 - (t0 + lo):s1 - (t0 + lo)] = a[s0:s1]
    return chunked_T(out)


def prep_A(inp):
    pos = pos_table()
    maps = []
    cst = host_consts()
    for k in range(NCORE):
        b, s = k // 4, k % 4
        t0 = s * TPC
        v = np.zeros((128, VA["NV"]), np.float32)
        cc = np.stack([fm(inp["c"][b]), fm(inp["c_ctx"])], axis=-1)
        v[:, VA["c"]:VA["c"] + 16] = cc.reshape(128, 16)
        v[:, VA["adab0"]:VA["adab0"] + 48] = fm(inp["ada_b"][0])
        v[:, VA["adab1"]:VA["adab1"] + 48] = fm(inp["ada_b"][1])
        v[:, VA["n1_0"]:VA["n1_0"] + 8] = fm(inp["norm1_g"][0])
        v[:, VA["n2_0"]:VA["n2_0"] + 8] = fm(inp["norm2_g"][0])
        v[:, VA["n1_1"]:VA["n1_1"] + 8] = fm(inp["norm1_g"][1])
        v[:, VA["bpw1"]:VA["bpw1"] + 16] = fm(inp["cf_b_pw1"][0])
        v[:, VA["bdw"]:VA["bdw"] + 8] = fm(inp["cf_b_dw"][0])
        v[:, VA["lng"]:VA["lng"] + 8] = fm(inp["cf_ln_g"][0])
        v[:, VA["lnb"]:VA["lnb"] + 8] = fm(inp["cf_ln_b"][0])
        v[:, VA["bpw2"]:VA["bpw2"] + 8] = fm(inp["cf_b_pw2"][0])
        wdw = np.asarray(inp["cf_w_dw"][0], np.float32)
        v[:, VA["wdw"]:VA["wdw"] + 248] = wdw.T.reshape(8, 128, 31).transpose(1, 0, 2).reshape(128, 248)
        v[:, VA["hyb"]:VA["hyb"] + 24] = fm(inp["hy_b_in"][0])
        v[:, VA["edge"]] = 0.0 if s == 0 else 1.0
        v[:, VA["edge"] + 1] = 0.0 if s == 3 else 1.0
        maps.append({
            "consts": cst,
            "xe": window_T(np.asarray(inp["x"][b]), t0, -16, TPC + 16),
            "pos": window_T(pos, t0, -16, TPC + 16),
            "ctxT": chunked_T(np.asarray(inp["ctx"][b])),
            "ada_w": np.ascontiguousarray(inp["ada_w"][0:2]),
            "w_pw1": np.asarray(inp["cf_w_pw1"][0]), "w_pw2": np.asarray(inp["cf_w_pw2"][0]),
            "w_gate": np.asarray(inp["ffn_w_gate"][0]), "w_up": np.asarray(inp["ffn_w_up"][0]),
            "w_down": np.asarray(inp["ffn_w_down"][0]), "w_in": np.asarray(inp["hy_w_in"][0]),
            "vecs": v,
        })
    return maps


HY_L = SEQ
TWO_PI = 2.0 * math.pi
VB = dict(b1=0, fr=1, b2=2, b3=3, nd=4, ndc=5, negpi=6, NV=8)


def hyena_filter_stage(E, zT, atau, L2, wf, vt, vr, G, Gr, asum, asr, ndcol):
    P = E.P
    wf1, wf2, wf3, wfr = wf
    nt = L2 // 512
    zt = [P.alloc([512]) for _ in range(2)]
    ztr = P.R(2)
    at = [P.alloc([512]) for _ in range(2)]
    atr = P.R(2)
    hh = [P.alloc([512]) for _ in range(3)]
    hr = P.R(3)
    dec = P.alloc([512])
    decr = P.R()
    gr_ = [P.alloc([512]) for _ in range(2)]
    grr = P.R(2)
    ab = P.alloc([512])
    abr = P.R()
    gb = [P.alloc([512], BF16) for _ in range(2)]
    gbr = P.R(2)
    wrp = P.alloc([512])
    wrp2 = P.alloc([512])
    wrr = P.R()

    def tile(ti):
        i = ti % 2
        c0 = ti * 512
        P.dma("sync", zt[i][0:33, :], zT[:, c0:c0 + 512], writes=[ztr[i]])
        P.dma("sync", at[i], atau[0:1, c0:c0 + 512].partition_broadcast(128), writes=[atr[i]])
        src, srcr = zt[i][0:33, :], ztr[i]
        for l in range(3):
            ps, psr = P.bank()
            lhs = wf1 if l == 0 else wf2[l - 1]
            P.mm(ps[0:64, :], psr, [(lhs, src)], [wfr, srcr])
            h = hh[l]
            P.V(lambda e, ps=ps, h=h, l=l: e.tensor_scalar(out=h[0:64, :], in0=ps[0:64, :],
                                                           scalar1=vt[0:64, VB["b1"] + l:VB["b1"] + l + 1] if l == 0 else vt[0:64, VB["b2"] + l - 1:VB["b2"] + l],
                                                           scalar2=vt[0:64, VB["fr"]:VB["fr"] + 1], op0=ALU.add, op1=ALU.mult),
                [psr, vr], [hr[l]])
            P.V(lambda e, h=h: e.tensor_scalar(out=wrp[0:64, :], in0=h[0:64, :], scalar1=-math.pi, scalar2=TWO_PI,
                                               op0=ALU.is_lt, op1=ALU.mult), [hr[l]], [wrr])
            P.V(lambda e, h=h: e.tensor_scalar(out=wrp2[0:64, :], in0=h[0:64, :], scalar1=math.pi, scalar2=-TWO_PI,
                                               op0=ALU.is_gt, op1=ALU.mult), [hr[l]], [wrr])
            P.V(lambda e, h=h: e.tensor_tensor(out=h[0:64, :], in0=h[0:64, :], in1=wrp[0:64, :], op=ALU.add), [hr[l], wrr], [hr[l]])
            P.V(lambda e, h=h: e.tensor_tensor(out=h[0:64, :], in0=h[0:64, :], in1=wrp2[0:64, :], op=ALU.add), [hr[l], wrr], [hr[l]])
            P.S(lambda e, h=h: e.activation(out=h[0:64, :], in_=h[0:64, :], func=AF.Sin), [hr[l]], [hr[l]])
            src, srcr = h[0:64, :], hr[l]
        P.S(lambda e, i=i: e.activation(out=dec, in_=at[i], func=AF.Exp, scale=vt[:, ndcol:ndcol + 1]), [atr[i], vr], [decr])
        halves = [(0, 512, 1 if c0 < L2 // 2 else 0)] if L2 > 512 else [(0, 256, 1), (256, 512, 0)]
        for o in range(2):
            ps, psr = P.bank()
            for (a, b, dr) in halves:
                blk = (o * 2 + dr) * 128
                P.mm(ps[:, a:b], psr, [(wf3[:, blk:blk + 128], src[:, a:b])], [wfr, srcr])
            g = gr_[o]
            P.V(lambda e, ps=ps, g=g: e.tensor_tensor(out=g, in0=ps[:, :], in1=dec, op=ALU.mult), [psr, decr], [grr[o]])
            if ti == 0:
                P.V(lambda e, g=g: e.memset(g[:, 0:1], 0.0), [], [grr[o]])
            P.S(lambda e, g=g: e.activation(out=ab, in_=g, func=AF.Abs), [grr[o]], [abr])
            P.V(lambda e, o=o: e.reduce_sum(out=asum[:, o, ti:ti + 1], in_=ab, axis=AX.X), [abr], [asr])
            P.S(lambda e, g=g, o=o: e.copy(out=gb[o], in_=g), [grr[o]], [gbr[o]])
            P.dma("sync", G[o][:, c0:c0 + 512], gb[o], reads=[gbr[o]], writes=[Gr])

    for ti in range(nt):
        tile(ti)


def build_B():
    E = Env("B")
    P = E.P
    E.consts()
    L = HY_L
    hu = E.inp("hu", [128, 6, L + 2])
    hc = E.inp("hc", [128, 6, CTX + 2])
    zT = E.inp("zT", [33, 2 * L])
    zcT = E.inp("zcT", [33, 512])
    atau = E.inp("atau", [1, 2 * L])
    atauc = E.inp("atauc", [1, 512])
    wf1_d = E.inp("wf1", [33, 64])
    wf2_d = E.inp("wf2", [64, 128])
    wf3_d = E.inp("wf3", [64, 512])
    hz = E.out("hz", [128, 2, L])
    hzc = E.out("hzc", [128, 2, CTX])
    vt, vr = load_small(E, "vecs", VB["NV"])
    hws, hwsr = load_small(E, "hws", 128 * 12)
    hbs, hbsr = load_small(E, "hbias", 128 * 2)
    G = E.scratch("G", [2, 128, 2 * L], BF16)
    Gc = E.scratch("Gc", [2, 128, 512], BF16)
    invd = E.scratch("invd", [128, 4])
    Gr, Gcr, invr = P.R(), P.R(), P.R()
    wfall = P.alloc([64 + 128 + 512])
    wfr = P.R()
    P.dma("sync", wfall[0:33, 0:64], wf1_d, writes=[wfr])
    P.dma("sync", wfall[0:64, 64:192], wf2_d, writes=[wfr])
    P.dma("sync", wfall[0:64, 192:704], wf3_d, writes=[wfr])
    wf = (wfall[0:33, 0:64], [wfall[0:64, 64:128], wfall[0:64, 128:192]], wfall[0:64, 192:704], wfr)
    asum = P.alloc([2, 64])
    asumc = P.alloc([2, 1])
    asr = P.R()
    inv = P.alloc([4])
    invb = P.alloc([128 * 4])
    invbr = P.R()
    P.mark()
    hyena_filter_stage(E, zT, atau, 2 * L, wf, vt, vr, G, Gr, asum, asr, VB["nd"])
    hyena_filter_stage(E, zcT, atauc, 512, wf, vt, vr, Gc, Gcr, asumc, asr, VB["ndc"])
    P.V(lambda e: e.reduce_sum(out=inv[:, 0:2], in_=asum, axis=AX.X), [asr], [asr])
    P.V(lambda e: e.tensor_copy(out=inv[:, 2:4], in_=asumc[:, :, 0]), [asr], [asr])
    P.V(lambda e: e.reciprocal(out=inv, in_=inv), [asr], [asr])
    P.dma("sync", invd, inv, reads=[asr], writes=[invr])
    P.dma("sync", invb, invd.rearrange("c f -> (c f)").partition_broadcast(128), reads=[invr], writes=[invbr])
    P.release()

    A = [P.alloc([6, 130]) for _ in range(2)]
    Ar = P.R(2)
    Ac = [P.alloc([6, 130]) for _ in range(2)]
    Acr = P.R(2)
    SK = [P.alloc([16384], BF16) for _ in range(4)]
    SKr = P.R(4)
    SKc = [P.alloc([384], BF16) for _ in range(2)]
    SKcr = P.R(2)
    st = dict(sk=0)

    def new_set(NB):
        return dict(Zb=P.alloc([2, NB], BF16), vf=P.alloc([2, NB]), x1=P.alloc([2, NB]), x2=P.alloc([2, NB]),
                    z1=P.alloc([2, NB]), z1b=P.alloc([2, NB], BF16), t1=P.alloc([2, NB]), z2=P.alloc([2, NB]),
                    zo=P.alloc([2, 128]), r=P.R(), NB=NB, y=P.alloc([6, 128]), yr=P.R())
    S_lat = [new_set(128), new_set(128)]
    S_ctx = [new_set(2), new_set(2)]
    BK = lambda i: (P.psum[i], P.psr[i])

    def conv(o, Z, Zr, NB, ch, lat, bank):
        ps, psr = BK(bank)
        pv = ps[:, 0:2 * NB].rearrange("p (b a) -> p b a", b=2)
        Gs = (G if lat else Gc)[o, ch]
        pieces = [(0, 128), (128, 255)] if lat else [(0, 3)]
        for (b0, b1) in pieces:
            if lat:
                hb = st["sk"] % 4
                st["sk"] += 1
                buf, bufr = SK[hb], SKr[hb]
            else:
                hb = st.setdefault("skc", 0) % 2
                st["skc"] = hb + 1
                buf, bufr = SKc[hb], SKcr[hb]
            ncol = (b1 - b0) * 128
            src = bass.AP(Gs.tensor, Gs.offset + 1 + b0 * 128, [[1, 128], [1, ncol]])
            P.dma("sync", buf[:, 0:ncol], src, reads=[Gr if lat else Gcr], writes=[bufr])
            blks = list(range(b0, b1))
            if b0 == 0:
                blks = [NB - 1] + [x for x in blks if x != NB - 1]
            pairs = []
            for blk in blks:
                dl = blk - (NB - 1)
                alo, ahi = max(0, dl), min(NB - 1, NB - 1 + dl)
                pairs.append((pv[:, :, alo:ahi + 1], buf[:, (blk - b0) * 128:(blk - b0 + 1) * 128], Z[:, :, alo - dl:ahi - dl + 1]))
            first = (b0 == 0)
            last = (b1 == pieces[-1][1])

            def fn(e, pairs=pairs, first=first, last=last):
                ins = None
                n = len(pairs)
                for i, (o_, l_, r_) in enumerate(pairs):
                    ins = e.matmul(o_, lhsT=l_, rhs=r_, start=(first and i == 0), stop=(last and i == n - 1))
                return ins
            P.op("tensor", fn, [bufr, Zr], [psr])
        return pv, psr

    def channel(ch, lat):
        S = (S_lat if lat else S_ctx)[ch % 2]
        NB = S["NB"]
        sr = S["r"]
        i = ch % 2
        a, ar = (A[i], Ar[i]) if lat else (Ac[i], Acr[i])
        yy, yyr = S["y"], S["yr"]
        Lx = L if lat else CTX
        src_t = hu if lat else hc
        base = src_t[ch]
        bkA, bkB, bkJ, bkC0, bkC1, bkT = (2, 3, 4, 0, 1, 5) if lat else (6, 7, 6, 7, 7, 6)
        src = bass.AP(base.tensor, base.offset, [[128, NB], [Lx + 2, 6], [1, 130]])
        P.dma("gpsimd", a[0:NB], src, writes=[ar])
        for s in range(3):
            wc = (ch * 3 + s) * 4
            sl = slice(2 * s, 2 * s + 2)
            P.V(lambda e, sl=sl, wc=wc: e.tensor_scalar(out=yy[0:NB, sl, :], in0=a[0:NB, sl, 0:128], scalar1=hws[0:NB, wc:wc + 1],
                                                        scalar2=hws[0:NB, wc + 3:wc + 4], op0=ALU.mult, op1=ALU.add), [ar, hwsr], [yyr])
            for k in (1, 2):
                P.V(lambda e, sl=sl, wc=wc, k=k: e.scalar_tensor_tensor(out=yy[0:NB, sl, :], in0=a[0:NB, sl, k:k + 128],
                                                                        scalar=hws[0:NB, wc + k:wc + k + 1], in1=yy[0:NB, sl, :],
                                                                        op0=ALU.mult, op1=ALU.add), [ar, hwsr, yyr], [yyr])
        yield
        pA, pAr = BK(bkA)
        pB, pBr = BK(bkB)
        for sb in range(6):
            pp, ppr = (pA, pAr) if sb < 4 else (pB, pBr)
            o_ = (sb % 4) * NB
            P.mm(pp[:, o_:o_ + NB], ppr, [(yy[0:NB, sb, :], E.ident[0:NB, 0:NB])], [yyr, E.cr])
        v3 = lambda t: t.rearrange("p b a -> p (b a)")
        P.V(lambda e: e.tensor_copy(out=v3(S["vf"]), in_=pA[:, 0:2 * NB]), [pAr], [sr])
        P.S(lambda e: e.copy(out=v3(S["x1"]), in_=pA[:, 2 * NB:4 * NB]), [pAr], [sr])
        P.V(lambda e: e.tensor_copy(out=v3(S["x2"]), in_=pB[:, 0:2 * NB]), [pBr], [sr])
        pJ, pJr = BK(bkJ)
        P.mm(pJ[:, 0:2 * NB], pJr, [(E.J, v3(S["vf"]))], [sr, E.cr])
        P.S(lambda e: e.copy(out=v3(S["Zb"]), in_=pJ[:, 0:2 * NB]), [pJr], [sr])
        yield
        ic = ch * 4 + (0 if lat else 2)
        zin, zf = S["Zb"], S["vf"]
        for o in range(2):
            pv, psr = conv(o, zin, sr, NB, ch, lat, bkC0 if o == 0 else bkC1)
            yield
            gate = S["x1"] if o == 0 else S["x2"]
            zo_ = S["z1"] if o == 0 else S["z2"]
            P.V(lambda e, pv=pv, o=o: e.tensor_scalar(out=S["t1"], in0=pv, scalar1=invb[:, ic + o:ic + o + 1], scalar2=None,
                                                      op0=ALU.mult), [psr, invbr], [sr])
            P.V(lambda e, o=o, zf=zf: e.scalar_tensor_tensor(out=S["t1"], in0=zf, scalar=hbs[:, ch * 2 + o:ch * 2 + o + 1],
                                                             in1=S["t1"], op0=ALU.mult, op1=ALU.add), [sr, hbsr], [sr])
            P.V(lambda e, gate=gate, zo_=zo_: e.tensor_tensor(out=zo_, in0=S["t1"], in1=gate, op=ALU.mult), [sr], [sr])
            if o == 0:
                pJ2, pJ2r = BK(bkJ)
                P.mm(pJ2[:, 0:2 * NB], pJ2r, [(E.J, v3(S["z1"]))], [sr, E.cr])
                P.S(lambda e, pJ2=pJ2: e.copy(out=v3(S["z1b"]), in_=pJ2[:, 0:2 * NB]), [pJ2r], [sr])
                zin, zf = S["z1b"], S["z1"]
            yield
        pT, pTr = BK(bkT)
        for b in range(2):
            P.mm(pT[0:NB, b * 128:(b + 1) * 128], pTr, [(S["z2"][:, b, :], E.ident)], [sr, E.cr])
        P.S(lambda e: e.copy(out=S["zo"][0:NB].rearrange("p b a -> p (b a)"), in_=pT[0:NB, 0:256]), [pTr], [sr])
        dst_t = hz if lat else hzc
        dbase = dst_t[ch]
        dst = bass.AP(dbase.tensor, dbase.offset, [[128, NB], [Lx, 2], [1, 128]])
        P.dma("gpsimd", dst, S["zo"][0:NB], reads=[sr], writes=[], is_out=True)

    def adv(g, n=1):
        for _ in range(n):
            next(g, None)

    glat = [channel(ch, True) for ch in range(128)]
    gctx = [channel(ch, False) for ch in range(128)]
    adv(glat[0], 2)
    for ch in range(128):
        adv(glat[ch])
        if ch + 1 < 128:
            adv(glat[ch + 1], 2)
        adv(gctx[ch], 2)
        adv(glat[ch])
        adv(gctx[ch])
        adv(glat[ch])
        adv(gctx[ch], 2)
        adv(glat[ch])
        for _ in glat[ch]:
            pass
        for _ in gctx[ch]:
            pass
    P.finish()
    P.emit()
    return E


def hyena_z_table(L):
    f32 = np.float32
    ip = np.arange(2 * L)
    tau = np.minimum(np.abs(ip - L), L - 1)
    t = (np.linspace(0.0, 1.0, L, dtype=f32))[tau][:, None]
    bands = 16
    w = (f32(2.0 * math.pi) * np.arange(L, dtype=f32) / f32(L))[tau][:, None]
    fb = np.linspace(1e-4, bands - 1, bands, dtype=f32)[None, :]
    z = np.concatenate([t, np.cos(fb * w), -np.sin(fb * w)], axis=-1).astype(f32)
    return np.ascontiguousarray(z.T), tau.astype(f32)[None, :]


def prep_B(inp, upre):
    L = HY_L
    zT, atau = hyena_z_table(L)
    zcT, atauc = hyena_z_table(CTX)
    dmin = math.log(1e-2) / 1.5
    dmax = math.log(1e-2) / 0.3
    deltas = np.abs(np.linspace(dmin, dmax, D, dtype=np.float32))
    cst = host_consts()
    wsh = np.asarray(inp["hy_w_short"][0], np.float32)
    bsh = np.asarray(inp["hy_b_short"][0], np.float32)
    hb = np.asarray(inp["hy_bias"][0], np.float32)
    wf3 = np.asarray(inp["hy_w_f3"][0], np.float32)
    maps = []
    for k in range(NCORE):
        hu = np.zeros((128, 6, L + 2), np.float32)
        hc = np.zeros((128, 6, CTX + 2), np.float32)
        for s in range(3):
            for b in range(BATCH):
                for seg in range(4):
                    hu[:, s * 2 + b, 1 + seg * TPC:1 + (seg + 1) * TPC] = upre[4 * b + seg][s * 8 + k][:, 0:TPC]
                hc[:, s * 2 + b, 1:1 + CTX] = upre[4 * b][s * 8 + k][:, TPC:TPC + CTX]
        v = np.zeros((128, VB["NV"]), np.float32)
        v[0:64, VB["b1"]] = inp["hy_b_f1"][0]
        v[0:64, VB["fr"]] = inp["hy_freq"][0]
        v[0:64, VB["b2"]] = inp["hy_b_f2"][0][0]
        v[0:64, VB["b3"]] = inp["hy_b_f2"][0][1]
        v[:, VB["nd"]] = -deltas[k * 128:(k + 1) * 128] / np.float32(L - 1)
        v[:, VB["ndc"]] = -deltas[k * 128:(k + 1) * 128] / np.float32(CTX - 1)
        v[:, VB["negpi"]] = -math.pi
        hws = np.zeros((128, 3, 4), np.float32)
        for s in range(3):
            cs = s * D + k * 128
            hws[:, s, 0:3] = wsh[:, cs:cs + 128].T
            hws[:, s, 3] = bsh[cs:cs + 128]
        hbias = np.ascontiguousarray(hb[:, k * 128:(k + 1) * 128].T)
        w3 = np.concatenate([wf3[:, o * 2 * D + dr * D + k * 128:o * 2 * D + dr * D + (k + 1) * 128]
                             for o in range(2) for dr in range(2)], axis=1)
        maps.append({
            "consts": cst, "hu": hu, "hc": hc, "zT": zT, "zcT": zcT, "atau": atau, "atauc": atauc,
            "wf1": np.asarray(inp["hy_w_f1"][0], np.float32),
            "wf2": np.ascontiguousarray(np.concatenate([inp["hy_w_f2"][0][0], inp["hy_w_f2"][0][1]], axis=1), dtype=np.float32),
            "wf3": np.ascontiguousarray(w3), "vecs": v,
            "hws": np.ascontiguousarray(np.broadcast_to(hws.reshape(1, -1), (128, 128 * 12))),
            "hbias": np.ascontiguousarray(np.broadcast_to(hbias.reshape(1, -1), (128, 256))),
        })
    return maps


def stage_post(E, tiles, xin, rin, sc, w_out, bias, xout):
    P = E.P
    P.mark()
    w = P.alloc([8, 1024], BF16)
    wr = P.R()
    P.dma("gpsimd", w, w_out.rearrange("(k p) n -> p k n", p=128), writes=[wr])
    x = [P.alloc([8, 512]) for _ in range(2)]
    xr = P.R(2)
    rb = [P.alloc([8, 512], BF16) for _ in range(2)]
    rbr = P.R(2)
    g1b = None
    if bias is not None:
        bvt, bcol, bvr = bias
        g1b = P.alloc([8, 2])
        g1br = P.R()
        for j in range(2):
            P.V(lambda e, j=j: e.tensor_tensor(out=g1b[:, :, j], in0=sc["g1"][:, :, j], in1=bvt[:, bcol:bcol + 8], op=ALU.mult),
                [sc["modr"], bvr], [g1br])
    cnt = [0]

    def tile_body(t):
        W, j, col = t["W"], t["j"], t["col"]
        i = cnt[0] % 2
        cnt[0] += 1
        xx, xxr, rr, rrr = x[i], xr[i], rb[i], rbr[i]
        P.dma("sync", xx[:, :, 0:W], xin[:, :, col:col + W], reads=t["in_regions"], writes=[xxr])
        P.dma("gpsimd", rr[:, :, 0:W], rin[:, :, col:col + W], reads=t.get("r_regions", []), writes=[rrr])
        for c in range(8):
            ps, psr = P.bank()
            P.mm(ps[:, 0:W], psr, [(w[:, k, c * 128:(c + 1) * 128], rr[:, k, 0:W]) for k in range(8)], [wr, rrr])
            P.V(lambda e, c=c, ps=ps: e.scalar_tensor_tensor(out=xx[:, c, 0:W], in0=ps[:, 0:W], scalar=sc["g1"][:, c, j:j + 1],
                                                             in1=xx[:, c, 0:W], op0=ALU.mult, op1=ALU.add), [psr, sc["modr"], xxr], [xxr])
            if g1b is not None:
                P.S(lambda e, c=c: e.activation(out=xx[:, c, 0:W], in_=xx[:, c, 0:W], func=AF.Identity,
                                                bias=g1b[:, c, j:j + 1], scale=1.0), [xxr, g1br], [xxr])
        P.dma("sync", xout[:, :, col:col + W], xx[:, :, 0:W], reads=[xxr], writes=t["out_regions"])
    for t in tiles:
        tile_body(t)
    P.release()


def host_sel():
    s = np.zeros((8, 8 * 128), np.float32)
    for e in range(8):
        s[e, e * 128:(e + 1) * 128] = 1.0
    return s


def load_sel(E):
    P = E.P
    d = E.inp("selc", [8, 1024])
    t = P.alloc([1024], parts=8)
    r = P.R()
    P.dma("sync", t, d, writes=[r])
    return t, r


VC = dict(c=0, adab1=16, adab2=64, n2_1=112, n1_2=120, bout=128, NV=136)


def build_C():
    E = Env("C")
    P = E.P
    E.consts()
    x0 = E.inp("x0", [8, 128, NCA]).rearrange("c p w -> p c w")
    zT = E.inp("zT", [8, 128, NCA]).rearrange("c p w -> p c w")
    adaw = E.inp("ada_w", [2, 1024, 6144])
    wout = E.inp("w_out", [1024, 1024])
    wrt = E.inp("w_router", [1024, 8])
    wg = E.inp("w_gate", [NE, 1024, DFF])
    wu = E.inp("w_up", [NE, 1024, DFF])
    wd = E.inp("w_down", [NE, DFF, 1024])
    win = E.inp("w_in", [1024, 5120])
    vt, vr = load_small(E, "vecs", VC["NV"])
    selc, selr = load_sel(E)
    xm = E.scratch("xm", [8, 128, NCA]).rearrange("c p w -> p c w")
    x1 = E.out("x1", [8, 128, NCA]).rearrange("c p w -> p c w")
    uo = E.out("u2", [40, 128, NCA]).rearrange("c p w -> p c w")
    nreg = (NCA + 511) // 512
    xmr, x1r = P.R(nreg), P.R(nreg)
    modT = P.alloc([48, 2])
    modr = P.R()
    compute_mod(E, adaw[0], vt, vr, VC["c"], VC["adab1"], modT, modr)
    sc1 = layer_scalars(E, modT, modr, vt, vr, VC["n2_1"], VC["n2_1"])
    tiles = []
    for t in tile_list():
        t = dict(t)
        t["in_regions"] = []
        t["out_regions"] = col_regions(xmr, t["col"], t["W"])
        tiles.append(t)
    stage_post(E, tiles, x0, zT, sc1, wout, (vt, VC["bout"], vr), xm)
    groups = []
    for g in tile_list(step=1024):
        g = dict(g)
        g["in_regions"] = col_regions(xmr, g["col"], g["W"])
        g["out_regions"] = col_regions(x1r, g["col"], g["W"])
        g["is_out"] = True
        groups.append(g)
    stage_ffn(E, groups, xm, sc1, wg, wu, wd, x1, moe=dict(wr=wrt, selc=selc, selr=selr))
    modT2 = P.alloc([48, 2])
    modr2 = P.R()
    compute_mod(E, adaw[1], vt, vr, VC["c"], VC["adab2"], modT2, modr2)
    sc2 = layer_scalars(E, modT2, modr2, vt, vr, VC["n1_2"], VC["n1_2"])
    ptiles = []
    for t in tile_list():
        t = dict(t)
        t["in_regions"] = col_regions(x1r, t["col"], t["W"])
        ptiles.append(t)
    stage_pre(E, ptiles, x1, sc2, win, 40, None, uo)
    P.finish()
    P.emit()
    return E


def cvec(inp, b):
    cc = np.stack([fm(inp["c"][b]), fm(inp["c_ctx"])], axis=-1)
    return cc.reshape(128, 16)


def gather_tok(chan_lat, chan_ctx, k):
    b, seg = k // 4, k % 4
    out = np.empty((8, 128, NCA), np.float32)
    for kk in range(8):
        out[kk, :, 0:TPC] = chan_lat[kk][:, b, seg * TPC:(seg + 1) * TPC]
        out[kk, :, TPC:] = chan_ctx[kk][:, b, :]
    return out


def prep_C(inp, x0s, hz, hzc):
    cst = host_consts()
    sel = host_sel()
    maps = []
    for k in range(NCORE):
        b = k // 4
        v = np.zeros((128, VC["NV"]), np.float32)
        v[:, VC["c"]:VC["c"] + 16] = cvec(inp, b)
        v[:, VC["adab1"]:VC["adab1"] + 48] = fm(inp["ada_b"][1])
        v[:, VC["adab2"]:VC["adab2"] + 48] = fm(inp["ada_b"][2])
        v[:, VC["n2_1"]:VC["n2_1"] + 8] = fm(inp["norm2_g"][1])
        v[:, VC["n1_2"]:VC["n1_2"] + 8] = fm(inp["norm1_g"][2])
        v[:, VC["bout"]:VC["bout"] + 8] = fm(inp["hy_b_out"][0])
        maps.append({
            "consts": cst, "selc": sel, "x0": x0s[k], "zT": gather_tok(hz, hzc, k),
            "ada_w": np.ascontiguousarray(inp["ada_w"][1:3]),
            "w_out": np.asarray(inp["hy_w_out"][0]), "w_router": np.asarray(inp["moe_w_router"][0]),
            "w_gate": np.asarray(inp["moe_w_gate"][0]), "w_up": np.asarray(inp["moe_w_up"][0]),
            "w_down": np.asarray(inp["moe_w_down"][0]), "w_in": np.asarray(inp["hg_w_in"][0]),
            "vecs": v,
        })
    return maps


HG_T = CTX + SEQ
HG_NCH = HG_T // 64
VD = dict(lbl=0, gn=4, NV=8)


def build_D(nblk=32):
    E = Env("D")
    P = E.P
    E.consts()
    qT = E.inp("qT", [128, 2, HG_T])
    ogT = E.inp("ogT", [128, 2, HG_T])
    zfT = E.inp("zfT", [128, 2, HG_T])
    zbT = E.inp("zbT", [128, 2, HG_T])
    vTM = E.inp("vTM", [64, 2, HG_NCH, 128])
    tri_d = E.inp("tri", [64, 128])
    rT = E.out("rT", [128, 2, SEQ])
    osc = E.scratch("osc", [128, 2, SEQ])
    oscr = [P.R(32) for _ in range(2)]
    vt, vr = load_small(E, "vecs", VD["NV"])
    tri = P.alloc([128], parts=64)
    trir = P.R()
    P.dma("sync", tri, tri_d, writes=[trir])
    triF, triB = tri[:, 0:64], tri[:, 64:128]
    lbt = P.alloc([8])
    lbr = P.R()
    P.S(lambda e: e.activation(out=lbt[:, 0:4], in_=vt[:, VD["lbl"]:VD["lbl"] + 4], func=AF.Exp), [vr], [lbr])
    P.V(lambda e: e.reduce_sum(out=lbt[:, 4:5], in_=lbt[:, 0:4], axis=AX.X), [lbr], [lbr])
    P.V(lambda e: e.reciprocal(out=lbt[:, 4:5], in_=lbt[:, 4:5]), [lbr], [lbr])
    P.V(lambda e: e.tensor_tensor(out=lbt[:, 5:6], in0=lbt[:, 1:2], in1=lbt[:, 2:3], op=ALU.add), [lbr], [lbr])
    P.V(lambda e: e.tensor_tensor(out=lbt[:, 5:6], in0=lbt[:, 5:6], in1=lbt[:, 4:5], op=ALU.mult), [lbr], [lbr])
    P.V(lambda e: e.tensor_scalar(out=lbt[:, 6:7], in0=lbt[:, 5:6], scalar1=-1.0, scalar2=1.0, op0=ALU.mult, op1=ALU.add),
        [lbr], [lbr])
    lb, oml = lbt[:, 5:6], lbt[:, 6:7]

    def mkbuf():
        d = {}
        for nm in ("qt", "zt", "f", "g", "kk", "qs", "bsb", "eb", "enb", "og", "of", "os", "sq"):
            d[nm] = P.alloc([512])
            d[nm + "_r"] = P.R()
        for nm in ("qd", "ki", "ke"):
            d[nm] = P.alloc([512], BF16)
            d[nm + "_r"] = P.R()
        d["gTM"] = P.alloc([8, 128], parts=64)
        d["gTM_r"] = P.R()
        for nm in ("v", "keTM"):
            d[nm] = P.alloc([8, 128], BF16, parts=64)
            d[nm + "_r"] = P.R()
        d["att"] = P.alloc([64], BF16, parts=64)
        d["att_r"] = P.R()
        d["S"] = P.alloc([128])
        d["S_r"] = P.R()
        d["Sb"] = P.alloc([128], BF16)
        d["Sb_r"] = P.R()
        return d
    bufs = [mkbuf() for _ in range(2)]

    def block(b, fwd, blk):
        B = bufs[b]
        ctxb = blk < 0
        nch = 4 if ctxb else 8
        W = nch * 64
        col0 = 0 if ctxb else CTX + blk * 512
        ch0 = col0 // 64
        zsrc = zfT if fwd else zbT
        tr = triF if fwd else triB
        R = lambda nm: B[nm + "_r"]
        P.dma("sync", B["qt"][:, 0:W], qT[:, b, col0:col0 + W], writes=[R("qt")])
        P.dma("sync", B["zt"][:, 0:W], zsrc[:, b, col0:col0 + W], writes=[R("zt")])
        P.dma("gpsimd", B["v"][:, 0:nch, :], vTM[:, b, ch0:ch0 + nch, :], writes=[R("v")])
        P.S(lambda e: e.activation(out=B["f"][:, 0:W], in_=B["zt"][:, 0:W], func=AF.Sigmoid), [R("zt")], [R("f")])
        P.V(lambda e: e.tensor_scalar(out=B["f"][:, 0:W], in0=B["f"][:, 0:W], scalar1=oml, scalar2=lb, op0=ALU.mult, op1=ALU.add),
            [R("f"), lbr], [R("f")])
        P.S(lambda e: e.activation(out=B["g"][:, 0:W], in_=B["f"][:, 0:W], func=AF.Ln), [R("f")], [R("g")])
        P.V(lambda e: e.tensor_scalar(out=B["kk"][:, 0:W], in0=B["f"][:, 0:W], scalar1=-1.0, scalar2=1.0, op0=ALU.mult, op1=ALU.add),
            [R("f")], [R("kk")])
        P.S(lambda e: e.activation(out=B["qs"][:, 0:W], in_=B["qt"][:, 0:W], func=AF.Silu), [R("qt")], [R("qs")])
        pts = [(P.psum[0], P.psr[0]), (P.psum[1], P.psr[1])]
        for c in range(nch):
            pp, ppr = pts[c // 4]
            P.mm(pp[0:64, (c % 4) * 128:(c % 4 + 1) * 128], ppr, [(B["g"][:, c * 64:(c + 1) * 64], E.ident)], [R("g"), E.cr])
        for hf in range((nch + 3) // 4):
            pp, ppr = pts[hf]
            n4 = min(4, nch - hf * 4)
            P.V(lambda e, pp=pp, hf=hf, n4=n4: e.tensor_copy(
                out=B["gTM"][:, hf * 4:hf * 4 + n4, :].rearrange("p a b -> p (a b)"), in_=pp[0:64, 0:n4 * 128]), [ppr], [R("gTM")])
        pb, pbr = P.psum[2], P.psr[2]
        for c in range(nch):
            P.mm(pb[:, c * 64:(c + 1) * 64], pbr, [(B["gTM"][:, c, :], tr)], [R("gTM"), trir])
        P.V(lambda e: e.tensor_copy(out=B["bsb"][:, 0:W], in_=pb[:, 0:W]), [pbr], [R("bsb")])
        P.S(lambda e: e.activation(out=B["eb"][:, 0:W], in_=B["bsb"][:, 0:W], func=AF.Exp), [R("bsb")], [R("eb")])
        P.S(lambda e: e.activation(out=B["enb"][:, 0:W], in_=B["bsb"][:, 0:W], func=AF.Exp, scale=-1.0), [R("bsb")], [R("enb")])
        P.V(lambda e: e.tensor_tensor(out=B["qd"][:, 0:W], in0=B["qs"][:, 0:W], in1=B["eb"][:, 0:W], op=ALU.mult),
            [R("qs"), R("eb")], [R("qd")])
        P.V(lambda e: e.tensor_tensor(out=B["ki"][:, 0:W], in0=B["kk"][:, 0:W], in1=B["enb"][:, 0:W], op=ALU.mult),
            [R("kk"), R("enb")], [R("ki")])
        lastcol = lambda c: (c * 64 + 63) if fwd else (c * 64)
        for c in range(nch):
            P.V(lambda e, c=c: e.tensor_scalar(out=B["ke"][:, c * 64:(c + 1) * 64], in0=B["ki"][:, c * 64:(c + 1) * 64],
                                               scalar1=B["eb"][:, lastcol(c):lastcol(c) + 1], scalar2=None, op0=ALU.mult),
                [R("ki"), R("eb")], [R("ke")])
        pts2 = [(P.psum[0], P.psr[0]), (P.psum[1], P.psr[1])]
        for c in range(nch):
            pp, ppr = pts2[c // 4]
            P.mm(pp[0:64, (c % 4) * 128:(c % 4 + 1) * 128], ppr, [(B["ke"][:, c * 64:(c + 1) * 64], E.ident_b)], [R("ke"), E.cr])
        for hf in range((nch + 3) // 4):
            pp, ppr = pts2[hf]
            n4 = min(4, nch - hf * 4)
            P.S(lambda e, pp=pp, hf=hf, n4=n4: e.copy(
                out=B["keTM"][:, hf * 4:hf * 4 + n4, :].rearrange("p a b -> p (a b)"), in_=pp[0:64, 0:n4 * 128]), [ppr], [R("keTM")])
        po, por = (None, None) if ctxb else (P.psum[3 + b], P.psr[3 + b])
        order = range(nch) if fwd else range(nch - 1, -1, -1)
        yield
        for c in order:
            cs = slice(c * 64, (c + 1) * 64)
            if not ctxb:
                pa, par = P.psum[5], P.psr[5]
                P.mm(pa[0:64, 0:64], par, [(B["ki"][:, cs], B["qd"][:, cs])], [R("ki"), R("qd")])
                P.V(lambda e, pa=pa: e.tensor_tensor(out=B["att"], in0=pa[0:64, 0:64], in1=tr, op=ALU.mult), [par, trir], [R("att")])
                P.mm(po[:, cs], por, [(B["v"][:, c, :], B["att"]), (B["Sb"], B["qd"][:, cs])], [R("v"), R("att"), R("Sb"), R("qd")])
            pd, pdr = P.psum[6], P.psr[6]
            P.mm(pd[:, 0:128], pdr, [(B["keTM"][:, c, :], B["v"][:, c, :])], [R("keTM"), R("v")])
            P.V(lambda e, pd=pd, c=c: e.scalar_tensor_tensor(out=B["S"], in0=B["S"], scalar=B["eb"][:, lastcol(c):lastcol(c) + 1],
                                                             in1=pd[:, 0:128], op0=ALU.mult, op1=ALU.add), [R("S"), R("eb"), pdr], [R("S")])
            P.S(lambda e: e.copy(out=B["Sb"], in_=B["S"]), [R("S")], [R("Sb")])
            yield
        if ctxb:
            return
        t0 = blk * 512
        if fwd:
            P.S(lambda e: e.copy(out=B["os"], in_=po[:, :]), [por], [R("os")])
            P.dma("sync", osc[:, b, t0:t0 + 512], B["os"], reads=[R("os")], writes=[oscr[b][blk]])
            return
        P.dma("sync", B["of"], osc[:, b, t0:t0 + 512], reads=[oscr[b][blk]], writes=[R("of")])
        P.dma("sync", B["og"], ogT[:, b, CTX + t0:CTX + t0 + 512], writes=[R("og")])
        P.V(lambda e: e.tensor_tensor(out=B["os"], in0=po[:, :], in1=B["of"], op=ALU.add), [por, R("of")], [R("os")])
        P.V(lambda e: e.tensor_tensor(out=B["sq"], in0=B["os"], in1=B["os"], op=ALU.mult), [R("os")], [R("sq")])
        pn, pnr = P.psum[7], P.psr[7]
        P.mm(pn[:, :], pnr, [(E.ones, B["sq"])], [E.cr, R("sq")])
        P.V(lambda e: e.tensor_scalar(out=B["sq"], in0=pn[:, :], scalar1=1.0 / 128.0, scalar2=float(EPS), op0=ALU.mult, op1=ALU.add),
            [pnr], [R("sq")])
        P.S(lambda e: e.sqrt(out=B["sq"], in_=B["sq"]), [R("sq")], [R("sq")])
        P.V(lambda e: e.reciprocal(out=B["sq"], in_=B["sq"]), [R("sq")], [R("sq")])
        P.V(lambda e: e.scalar_tensor_tensor(out=B["os"], in0=B["os"], scalar=vt[:, VD["gn"]:VD["gn"] + 1], in1=B["sq"],
                                             op0=ALU.mult, op1=ALU.mult), [R("os"), vr, R("sq")], [R("os")])
        P.S(lambda e: e.activation(out=B["og"], in_=B["og"], func=AF.Silu), [R("og")], [R("og")])
        P.V(lambda e: e.tensor_tensor(out=B["os"], in0=B["os"], in1=B["og"], op=ALU.mult), [R("os"), R("og")], [R("os")])
        P.dma("sync", rT[:, b, t0:t0 + 512], B["os"], reads=[R("os")], writes=[], is_out=True)

    for fwd in (True, False):
        for b in range(2):
            P.V(lambda e, b=b: e.memset(bufs[b]["S"], 0.0), [], [bufs[b]["S_r"]])
            P.S(lambda e, b=b: e.copy(out=bufs[b]["Sb"], in_=bufs[b]["S"]), [bufs[b]["S_r"]], [bufs[b]["Sb_r"]])
        blks = [-1] + (list(range(nblk)) if fwd else list(range(nblk - 1, -1, -1)))
        for blk in blks:
            alive = [block(b, fwd, blk) for b in range(2)]
            while alive:
                for g_ in list(alive):
                    try:
                        next(g_)
                    except StopIteration:
                        alive.remove(g_)
    P.finish()
    P.emit()
    return E


def host_tri():
    s = np.arange(64)
    f = (s[:, None] <= s[None, :]).astype(np.float32)
    bk = (s[:, None] >= s[None, :]).astype(np.float32)
    return np.concatenate([f, bk], axis=1)


def prep_D(inp, u2s):
    cst = host_consts()
    tri = host_tri()
    maps = []
    lbl = np.asarray(inp["hg_lb_logits"], np.float32)
    for k in range(NCORE):
        def gat(s):
            out = np.empty((128, 2, HG_T), np.float32)
            for b in range(2):
                out[:, b, 0:CTX] = u2s[4 * b][s * 8 + k][:, TPC:TPC + CTX]
                for seg in range(4):
                    out[:, b, CTX + seg * TPC:CTX + (seg + 1) * TPC] = u2s[4 * b + seg][s * 8 + k][:, 0:TPC]
            return out
        iT = gat(1)
        vTM = np.ascontiguousarray(iT.transpose(1, 2, 0).reshape(2, HG_NCH, 64, 128).transpose(2, 0, 1, 3))
        v = np.zeros((128, VD["NV"]), np.float32)
        v[:, VD["lbl"]:VD["lbl"] + 4] = lbl[:, k * 128:(k + 1) * 128].T
        v[:, VD["gn"]] = inp["hg_gn_g"][0][k * 128:(k + 1) * 128]
        maps.append({"consts": cst, "tri": tri, "qT": gat(0), "ogT": gat(2), "zfT": gat(3), "zbT": gat(4), "vTM": vTM, "vecs": v})
    return maps


VE = dict(c=0, adab2=16, n2_2=64, NV=72)


def build_E():
    E = Env("E")
    P = E.P
    E.consts()
    x1 = E.inp("x1", [8, 128, NCA]).rearrange("c p w -> p c w")
    rT = E.inp("rT", [8, 128, TPC]).rearrange("c p w -> p c w")
    adaw = E.inp("ada_w", [1024, 6144])
    wout = E.inp("w_out", [1024, 1024])
    wg = E.inp("w_gate", [1024, DFF])
    wu = E.inp("w_up", [1024, DFF])
    wd = E.inp("w_down", [DFF, 1024])
    vt, vr = load_small(E, "vecs", VE["NV"])
    xm = E.scratch("xm", [8, 128, TPC]).rearrange("c p w -> p c w")
    x2 = E.out("x2", [8, 128, TPC]).rearrange("c p w -> p c w")
    nreg = TPC // 512
    xmr, x2r = P.R(nreg), P.R(nreg)
    modT = P.alloc([48, 2])
    modr = P.R()
    compute_mod(E, adaw, vt, vr, VE["c"], VE["adab2"], modT, modr)
    sc = layer_scalars(E, modT, modr, vt, vr, VE["n2_2"], VE["n2_2"])
    tiles = []
    for t in tile_list(with_ctx=False):
        t = dict(t)
        t["in_regions"] = []
        t["out_regions"] = col_regions(xmr, t["col"], t["W"])
        tiles.append(t)
    stage_post(E, tiles, x1, rT, sc, wout, None, xm)
    groups = []
    for g in tile_list(with_ctx=False, step=1024):
        g = dict(g)
        g["in_regions"] = col_regions(xmr, g["col"], g["W"])
        g["out_regions"] = col_regions(x2r, g["col"], g["W"])
        g["is_out"] = True
        groups.append(g)
    stage_ffn(E, groups, xm, sc, wg, wu, wd, x2)
    P.finish()
    P.emit()
    return E


def prep_E(inp, x1s, rTs):
    cst = host_consts()
    maps = []
    for k in range(NCORE):
        b, seg = k // 4, k % 4
        v = np.zeros((128, VE["NV"]), np.float32)
        v[:, VE["c"]:VE["c"] + 16] = cvec(inp, b)
        v[:, VE["adab2"]:VE["adab2"] + 48] = fm(inp["ada_b"][2])
        v[:, VE["n2_2"]:VE["n2_2"] + 8] = fm(inp["norm2_g"][2])
        r = np.stack([rTs[kk][:, b, seg * TPC:(seg + 1) * TPC] for kk in range(8)], axis=0)
        maps.append({"consts": cst, "x1": x1s[k], "rT": np.ascontiguousarray(r), "ada_w": np.asarray(inp["ada_w"][2]),
                     "w_out": np.asarray(inp["hg_w_out"][0]), "w_gate": np.asarray(inp["ffn_w_gate"][1]),
                     "w_up": np.asarray(inp["ffn_w_up"][1]), "w_down": np.asarray(inp["ffn_w_down"][1]), "vecs": v})
    return maps


VF = dict(c=0, adab3=16, n1_3=64, n2_3=72, bpw1=80, bdw=96, lng=104, lnb=112, bpw2=120, wdw=128, edge=376, nf=378, NV=386)


def stage_final(E, tiles, xin, vt, vr, col_nf, yout):
    P = E.P
    P.mark()
    rc = rms_ctx(E, 512)
    gs = P.alloc([8, 2])
    sh = P.alloc([8, 2])
    gr = P.R()
    P.V(lambda e: e.memset(sh, 0.0), [], [gr])
    P.V(lambda e: e.memset(gs, 0.0), [], [gr])
    P.V(lambda e: e.tensor_scalar(out=gs[:, :, 0], in0=vt[:, col_nf:col_nf + 8], scalar1=float(math.sqrt(D)), scalar2=None,
                                  op0=ALU.mult), [vr, gr], [gr])
    x = P.alloc([8, 512])
    xr = P.R()
    hb = P.alloc([8, 512], BF16)
    hbr = P.R()
    y = P.alloc([8, 512])
    yr = P.R()

    def tile_body(t):
        W, col = t["W"], t["col"]
        P.dma("sync", x[:, :, 0:W], xin[:, :, col:col + W], reads=t["in_regions"], writes=[xr])
        rms_mod(E, rc, x, xr, W, gs, sh, 0, gr, hb, hbr, out_f32=y, out_f32_r=yr)
        P.dma("sync", yout[:, :, col:col + W], y[:, :, 0:W], reads=[yr], writes=[], is_out=True)
    for t in tiles:
        tile_body(t)
    P.release()


def build_F():
    E = Env("F")
    P = E.P
    E.consts()
    xe_d = E.inp("xe", [8, 128, TPC + 32]).rearrange("c p w -> p c w")
    adaw = E.inp("ada_w", [1024, 6144])
    wpw1 = E.inp("w_pw1", [1024, 2048])
    wpw2 = E.inp("w_pw2", [1024, 1024])
    wrt = E.inp("w_router", [1024, 8])
    wg = E.inp("w_gate", [NE, 1024, DFF])
    wu = E.inp("w_up", [NE, 1024, DFF])
    wd = E.inp("w_down", [NE, DFF, 1024])
    vt, vr = load_small(E, "vecs", VF["NV"])
    E.edger = vr
    selc, selr = load_sel(E)
    xm = E.scratch("xm", [8, 128, TPC]).rearrange("c p w -> p c w")
    x3 = E.scratch("x3", [8, 128, TPC]).rearrange("c p w -> p c w")
    yo = E.out("y", [8, 128, TPC]).rearrange("c p w -> p c w")
    nreg = TPC // 512
    xmr, x3r = P.R(nreg), P.R(nreg)
    modT = P.alloc([48, 2])
    modr = P.R()
    compute_mod(E, adaw, vt, vr, VF["c"], VF["adab3"], modT, modr)
    sc = layer_scalars(E, modT, modr, vt, vr, VF["n1_3"], VF["n2_3"])
    tiles = []
    for t in tile_list(with_ctx=False):
        t = dict(t)
        c0 = t["col"] + 1
        t.update(src=xe_d[:, :, c0:c0 + 542], pos=None, in_regions=[], ocol=t["col"])
        if t["col"] == 0:
            t["ledge"] = vt[:, VF["edge"]:VF["edge"] + 1]
        if t["col"] == TPC - 512:
            t["redge"] = vt[:, VF["edge"] + 1:VF["edge"] + 2]
        t["out_regions"] = col_regions(xmr, t["col"], t["W"])
        tiles.append(t)
    stage_conformer(E, tiles, wpw1, wpw2, vt, vr,
                    (VF["bpw1"], VF["bdw"], VF["lng"], VF["lnb"], VF["bpw2"], VF["wdw"]), sc, xm, None)
    groups = []
    for g in tile_list(with_ctx=False, step=1024):
        g = dict(g)
        g["in_regions"] = col_regions(xmr, g["col"], g["W"])
        g["out_regions"] = col_regions(x3r, g["col"], g["W"])
        groups.append(g)
    stage_ffn(E, groups, xm, sc, wg, wu, wd, x3, moe=dict(wr=wrt, selc=selc, selr=selr))
    ftiles = []
    for t in tile_list(with_ctx=False):
        t = dict(t)
        t["in_regions"] = col_regions(x3r, t["col"], t["W"])
        ftiles.append(t)
    stage_final(E, ftiles, x3, vt, vr, VF["nf"], yo)
    P.finish()
    P.emit()
    return E


def prep_F(inp, x2s):
    cst = host_consts()
    sel = host_sel()
    maps = []
    full = None if x2s is None else [np.concatenate([unchunk_T(x2s[4 * b + seg]) for seg in range(4)], axis=0) for b in range(BATCH)]
    for k in range(NCORE):
        b, seg = k // 4, k % 4
        v = np.zeros((128, VF["NV"]), np.float32)
        v[:, VF["c"]:VF["c"] + 16] = cvec(inp, b)
        v[:, VF["adab3"]:VF["adab3"] + 48] = fm(inp["ada_b"][3])
        v[:, VF["n1_3"]:VF["n1_3"] + 8] = fm(inp["norm1_g"][3])
        v[:, VF["n2_3"]:VF["n2_3"] + 8] = fm(inp["norm2_g"][3])
        v[:, VF["bpw1"]:VF["bpw1"] + 16] = fm(inp["cf_b_pw1"][1])
        v[:, VF["bdw"]:VF["bdw"] + 8] = fm(inp["cf_b_dw"][1])
        v[:, VF["lng"]:VF["lng"] + 8] = fm(inp["cf_ln_g"][1])
        v[:, VF["lnb"]:VF["lnb"] + 8] = fm(inp["cf_ln_b"][1])
        v[:, VF["bpw2"]:VF["bpw2"] + 8] = fm(inp["cf_b_pw2"][1])
        wdw = np.asarray(inp["cf_w_dw"][1], np.float32)
        v[:, VF["wdw"]:VF["wdw"] + 248] = wdw.T.reshape(8, 128, 31).transpose(1, 0, 2).reshape(128, 248)
        v[:, VF["edge"]] = 0.0 if seg == 0 else 1.0
        v[:, VF["edge"] + 1] = 0.0 if seg == 3 else 1.0
        v[:, VF["nf"]:VF["nf"] + 8] = fm(inp["normf_g"])
        maps.append({"consts": cst, "selc": sel, "xe": None if full is None else window_T(full[b], seg * TPC, -16, TPC + 16),
                     "ada_w": np.asarray(inp["ada_w"][3]), "w_pw1": np.asarray(inp["cf_w_pw1"][1]),
                     "w_pw2": np.asarray(inp["cf_w_pw2"][1]), "w_router": np.asarray(inp["moe_w_router"][1]),
                     "w_gate": np.asarray(inp["moe_w_gate"][1]), "w_up": np.asarray(inp["moe_w_up"][1]),
                     "w_down": np.asarray(inp["moe_w_down"][1]), "vecs": v})
    return maps


NCE = TPC + 32
VG = dict(VF)
VG.update(c=0, adab2=402, n2_2=450, NV=458)


def build_EF():
    E = Env("EF")
    P = E.P
    E.consts()
    x1 = E.inp("x1", [8, 128, NCE]).rearrange("c p w -> p c w")
    rT = E.inp("rT", [8, 128, NCE]).rearrange("c p w -> p c w")
    adaw2 = E.inp("ada_w2", [1024, 6144])
    wout = E.inp("w_out", [1024, 1024])
    wg2 = E.inp("w_gate2", [1024, DFF])
    wu2 = E.inp("w_up2", [1024, DFF])
    wd2 = E.inp("w_down2", [DFF, 1024])
    adaw = E.inp("ada_w", [1024, 6144])
    wpw1 = E.inp("w_pw1", [1024, 2048])
    wpw2 = E.inp("w_pw2", [1024, 1024])
    wrt = E.inp("w_router", [1024, 8])
    wg = E.inp("w_gate", [NE, 1024, DFF])
    wu = E.inp("w_up", [NE, 1024, DFF])
    wd = E.inp("w_down", [NE, DFF, 1024])
    vt, vr = load_small(E, "vecs", VG["NV"])
    E.edger = vr
    selc, selr = load_sel(E)
    xm2 = E.scratch("xm2", [8, 128, NCE]).rearrange("c p w -> p c w")
    x2 = E.scratch("x2", [8, 128, NCE]).rearrange("c p w -> p c w")
    xm = E.scratch("xm", [8, 128, TPC]).rearrange("c p w -> p c w")
    x3 = E.scratch("x3", [8, 128, TPC]).rearrange("c p w -> p c w")
    yo = E.out("y", [8, 128, TPC]).rearrange("c p w -> p c w")
    nreg = (NCE + 511) // 512
    xm2r, x2r, xmr, x3r = P.R(nreg), P.R(nreg), P.R(nreg), P.R(nreg)
    modT2 = P.alloc([48, 2])
    modr2 = P.R()
    compute_mod(E, adaw2, vt, vr, VG["c"], VG["adab2"], modT2, modr2)
    sc2 = layer_scalars(E, modT2, modr2, vt, vr, VG["n2_2"], VG["n2_2"])
    tl2 = tile_list(with_ctx=False) + [dict(col=TPC, W=32, j=0)]
    tiles = []
    for t in tl2:
        t = dict(t)
        t["in_regions"] = []
        t["out_regions"] = col_regions(xm2r, t["col"], t["W"])
        tiles.append(t)
    stage_post(E, tiles, x1, rT, sc2, wout, None, xm2)
    groups = []
    for g in [dict(col=0, W=1024, j=0), dict(col=1024, W=1024, j=0), dict(col=2048, W=1024, j=0), dict(col=3072, W=1024 + 32, j=0)]:
        g = dict(g)
        g["in_regions"] = col_regions(xm2r, g["col"], g["W"])
        g["out_regions"] = col_regions(x2r, g["col"], g["W"])
        groups.append(g)
    stage_ffn(E, groups, xm2, sc2, wg2, wu2, wd2, x2)
    modT = P.alloc([48, 2])
    modr = P.R()
    compute_mod(E, adaw, vt, vr, VG["c"], VG["adab3"], modT, modr)
    sc = layer_scalars(E, modT, modr, vt, vr, VG["n1_3"], VG["n2_3"])
    tiles = []
    for t in tile_list(with_ctx=False):
        t = dict(t)
        c = t["col"]
        if c == 0:
            src = [(0, 15, x2[:, :, TPC + 1:TPC + 16]), (15, 527, x2[:, :, 0:527])]
            t["ledge"] = vt[:, VG["edge"]:VG["edge"] + 1]
        elif c == TPC - 512:
            src = [(0, 527, x2[:, :, c - 15:TPC]), (527, 15, x2[:, :, TPC + 16:TPC + 31])]
            t["redge"] = vt[:, VG["edge"] + 1:VG["edge"] + 2]
        else:
            src = x2[:, :, c - 15:c + 527]
        t.update(src=src, pos=None, in_regions=list(x2r), ocol=c)
        t["out_regions"] = col_regions(xmr, c, t["W"])
        tiles.append(t)
    stage_conformer(E, tiles, wpw1, wpw2, vt, vr,
                    (VG["bpw1"], VG["bdw"], VG["lng"], VG["lnb"], VG["bpw2"], VG["wdw"]), sc, xm, None)
    groups = []
    for g in tile_list(with_ctx=False, step=1024):
        g = dict(g)
        g["in_regions"] = col_regions(xmr, g["col"], g["W"])
        g["out_regions"] = col_regions(x3r, g["col"], g["W"])
        groups.append(g)
    stage_ffn(E, groups, xm, sc, wg, wu, wd, x3, moe=dict(wr=wrt, selc=selc, selr=selr))
    ftiles = []
    for t in tile_list(with_ctx=False):
        t = dict(t)
        t["in_regions"] = col_regions(x3r, t["col"], t["W"])
        ftiles.append(t)
    stage_final(E, ftiles, x3, vt, vr, VG["nf"], yo)
    P.finish()
    P.emit()
    return E


def prep_EF(inp, x1s, rTs):
    mF = prep_F(inp, None)
    maps = []
    x1full = [np.concatenate([unchunk_T(x1s[4 * b + seg])[0:TPC] for seg in range(4)], axis=0) for b in range(BATCH)]
    for k in range(NCORE):
        b, seg = k // 4, k % 4
        t0 = seg * TPC
        v = np.zeros((128, VG["NV"]), np.float32)
        v[:, 0:VF["NV"]] = mF[k]["vecs"]
        v[:, VG["adab2"]:VG["adab2"] + 48] = fm(inp["ada_b"][2])
        v[:, VG["n2_2"]:VG["n2_2"] + 8] = fm(inp["norm2_g"][2])
        x1w = window_T(x1full[b], t0, -16, TPC + 16)
        x1e = np.concatenate([x1w[:, :, 16:16 + TPC], x1w[:, :, 0:16], x1w[:, :, 16 + TPC:]], axis=2)
        rw = np.zeros((8, 128, TPC + 32), np.float32)
        lo, hi = max(0, t0 - 16), min(SEQ, t0 + TPC + 16)
        for kk in range(8):
            rw[kk][:, lo - (t0 - 16):hi - (t0 - 16)] = rTs[kk][:, b, lo:hi]
        re_ = np.concatenate([rw[:, :, 16:16 + TPC], rw[:, :, 0:16], rw[:, :, 16 + TPC:]], axis=2)
        m = dict(mF[k])
        m.pop("xe")
        m.update({"x1": np.ascontiguousarray(x1e), "rT": np.ascontiguousarray(re_), "ada_w2": np.asarray(inp["ada_w"][2]),
                  "w_out": np.asarray(inp["hg_w_out"][0]), "w_gate2": np.asarray(inp["ffn_w_gate"][1]),
                  "w_up2": np.asarray(inp["ffn_w_up"][1]), "w_down2": np.asarray(inp["ffn_w_down"][1]), "vecs": v})
        maps.append(m)
    return maps


def _run(E, maps):
    res = run_bass_kernel_spmd(E.nc, maps, core_ids=list(range(NCORE)))
    return res.results


def kernel(**inp):
    inp = {k: np.asarray(v) for k, v in inp.items()}
    ra = _run(build_A(), prep_A(inp))
    x0s = [r["x0"] for r in ra]
    rb = _run(build_B(), prep_B(inp, [r["u_pre"] for r in ra]))
    del ra
    rc_ = _run(build_C(), prep_C(inp, x0s, [r["hz"] for r in rb], [r["hzc"] for r in rb]))
    del rb, x0s
    x1s = [r["x1"] for r in rc_]
    rd = _run(build_D(), prep_D(inp, [r["u2"] for r in rc_]))
    del rc_
    rf = _run(build_EF(), prep_EF(inp, x1s, [r["rT"] for r in rd]))
    del rd, x1s
    out = np.empty((BATCH, SEQ, D), np.float32)
    for k in range(NCORE):
        b, seg = k // 4, k % 4
        out[b, seg * TPC:(seg + 1) * TPC] = unchunk_T(rf[k]["y"])
    return out
```
